# Optimizing a Trainium2 kernel written in Bass

```python
import jax, jax.numpy as jnp
from jax import lax
import numpy as np

D_MODEL = 1024
BATCH = 1
SEQ = 16384
DEPTH = 2
DEC_BATCH = 8
DEC_SEQ = 8192
PAST_LEN = 128

N_MIXERS = 2
N_RET_LAYERS = (DEPTH + 1) // 2
N_GM_LAYERS = DEPTH // 2
RET_HEADS = 4
RET_DK = D_MODEL // RET_HEADS
RET_DV = 2 * RET_DK
RET_QK = RET_HEADS * RET_DK
RET_VDIM = RET_HEADS * RET_DV
RET_IN = 2 * RET_QK + 2 * RET_VDIM
RET_CHUNK = 128
ROPE_BASE = 10000.0
GM_GROUPS = 4
GM_WIDTH = 2 * D_MODEL
GM_GDIM = GM_WIDTH // GM_GROUPS
GM_CHUNK = 128
N_EXPERTS = 32
TOP_K = 4
D_EXPERT = D_MODEL
SWIGLU_LIMIT = 7.0
SWIGLU_ALPHA = 1.702
MOE_BLOCK = 256
N_MOD = 6
EPS = 1e-6

kernel_name = "hybrid_retention_gmlp_moe_encoder"


def _rmsnorm(x, g):
    xf = x.astype(jnp.float32)
    y = xf * lax.rsqrt(jnp.mean(xf * xf, axis=-1, keepdims=True) + EPS) * g.astype(jnp.float32)
    return y.astype(x.dtype)


def _layernorm(x, g, b):
    xf = x.astype(jnp.float32)
    mu = jnp.mean(xf, axis=-1, keepdims=True)
    var = jnp.mean(jnp.square(xf - mu), axis=-1, keepdims=True)
    y = (xf - mu) * lax.rsqrt(var + EPS) * g.astype(jnp.float32) + b.astype(jnp.float32)
    return y.astype(x.dtype)


def _rope(x):
    S, D = x.shape[1], x.shape[-1]
    half = D // 2
    inv = ROPE_BASE ** (-jnp.arange(half, dtype=jnp.float32) / half)
    ang = jnp.arange(S, dtype=jnp.float32)[:, None] * inv[None, :]
    cos = jnp.cos(ang)[None, :, None, :]
    sin = jnp.sin(ang)[None, :, None, :]
    x1, x2 = x[..., :half], x[..., half:]
    return jnp.concatenate([x1 * cos - x2 * sin, x2 * cos + x1 * sin], axis=-1)


def _retention_direction(q, k, v, log_gamma, include_diag):
    B, S, H, DK = q.shape
    DV = v.shape[-1]
    nc = S // RET_CHUNK
    idx = jnp.arange(RET_CHUNK, dtype=jnp.float32)
    diff = idx[:, None] - idx[None, :]
    tri = (diff >= 0) if include_diag else (diff > 0)
    decay_intra = jnp.where(tri[None], jnp.exp(log_gamma[:, None, None] * jnp.where(tri, diff, 0.0)[None]), 0.0)
    q_decay = jnp.exp(log_gamma[None, :] * (idx + 1.0)[:, None])
    k_decay = jnp.exp(log_gamma[None, :] * (RET_CHUNK - 1.0 - idx)[:, None])
    chunk_decay = jnp.exp(log_gamma * RET_CHUNK)

    def to_chunks(t):
        return jnp.moveaxis(t.reshape(B, nc, RET_CHUNK, H, t.shape[-1]), 1, 0)

    def step(state, blk):
        qc, kc, vc = blk
        s = jnp.einsum('bihd,bjhd->bhij', qc, kc) * decay_intra
        o = (jnp.einsum('bhij,bjhe->bihe', s, vc)
             + jnp.einsum('bihd,bhde->bihe', qc, state) * q_decay[None, :, :, None])
        state = (state * chunk_decay[None, :, None, None]
                 + jnp.einsum('bjhd,bjhe->bhde', kc * k_decay[None, :, :, None], vc))
        return state, o

    state0 = jnp.zeros((B, H, DK, DV), jnp.float32)
    _, o = lax.scan(step, state0, (to_chunks(q), to_chunks(k), to_chunks(v)))
    return jnp.moveaxis(o, 0, 1).reshape(B, S, H, DV)


def _retention_mixer(h, w_in, log_decay, gn_g, w_out):
    B, S, _ = h.shape
    proj = h @ w_in
    q, k, v, g = jnp.split(proj, [RET_QK, 2 * RET_QK, 2 * RET_QK + RET_VDIM], axis=-1)
    q = _rope(q.reshape(B, S, RET_HEADS, RET_DK).astype(jnp.float32)) * (RET_DK ** -0.5)
    k = _rope(k.reshape(B, S, RET_HEADS, RET_DK).astype(jnp.float32))
    v = v.reshape(B, S, RET_HEADS, RET_DV).astype(jnp.float32)
    ld = log_decay.astype(jnp.float32)
    fwd = _retention_direction(q, k, v, ld[0], True)
    bwd = _retention_direction(q[:, ::-1], k[:, ::-1], v[:, ::-1], ld[1], False)[:, ::-1]
    o = fwd + bwd
    mu = jnp.mean(o, axis=-1, keepdims=True)
    var = jnp.mean(jnp.square(o - mu), axis=-1, keepdims=True)
    o = ((o - mu) * lax.rsqrt(var + EPS)).reshape(B, S, RET_VDIM) * gn_g.astype(jnp.float32)
    o = o.astype(h.dtype) * jax.nn.silu(g)
    return o @ w_out


def _gmlp_mixer(h, w_in, b_in, vn_g, vn_b, w_s, b_s, w_out, b_out):
    B, S, _ = h.shape
    z = jax.nn.gelu(h @ w_in + b_in, approximate=False)
    u, v = jnp.split(z, 2, axis=-1)
    v = _layernorm(v, vn_g, vn_b)
    nc = S // GM_CHUNK
    vc = v.reshape(B, nc, GM_CHUNK, GM_GROUPS, GM_GDIM)
    mixed = jnp.einsum('gij,bnjgc->bnigc', w_s, vc) + b_s.T[None, None, :, :, None]
    return (u * mixed.reshape(B, S, GM_WIDTH)) @ w_out + b_out


def _moe(h, w_r, b_r, w_gu, b_gu, w_dn, b_dn):
    T, D = h.shape
    logits = (h @ w_r + b_r).astype(jnp.float32)
    top_v, top_i = lax.top_k(logits, TOP_K)
    gates = jax.nn.softmax(top_v, axis=-1).astype(h.dtype)
    A = T * TOP_K
    flat_e = top_i.reshape(A)
    flat_tok = jnp.repeat(jnp.arange(T, dtype=jnp.int32), TOP_K)
    flat_g = gates.reshape(A)
    order = jnp.argsort(flat_e)
    se = flat_e[order]
    counts = jnp.bincount(flat_e, length=N_EXPERTS)
    padded = (counts + MOE_BLOCK - 1) // MOE_BLOCK * MOE_BLOCK
    start = jnp.cumsum(counts) - counts
    pend = jnp.cumsum(padded)
    pstart = pend - padded
    dest = pstart[se] + (jnp.arange(A, dtype=jnp.int32) - start[se])
    n_blocks = -(-A // MOE_BLOCK) + N_EXPERTS
    P = n_blocks * MOE_BLOCK
    row_tok = jnp.full((P,), T, jnp.int32).at[dest].set(flat_tok[order])
    row_gate = jnp.zeros((P,), h.dtype).at[dest].set(flat_g[order])
    block_e = jnp.minimum(jnp.searchsorted(pend, jnp.arange(n_blocks, dtype=jnp.int32) * MOE_BLOCK, side='right'),
                          N_EXPERTS - 1).astype(jnp.int32)
    h_pad = jnp.concatenate([h, jnp.zeros((1, D), h.dtype)], axis=0)
    xb = h_pad[row_tok].reshape(n_blocks, MOE_BLOCK, D)

    def expert_block(args):
        xblk, e = args
        hg = xblk @ w_gu[e] + b_gu[e]
        glu = jnp.minimum(hg[:, ::2], SWIGLU_LIMIT)
        lin = jnp.clip(hg[:, 1::2], -SWIGLU_LIMIT, SWIGLU_LIMIT)
        act = glu * jax.nn.sigmoid(SWIGLU_ALPHA * glu) * (lin + 1.0)
        return act @ w_dn[e] + b_dn[e]

    yb = lax.map(expert_block, (xb, block_e)).reshape(P, D) * row_gate[:, None]
    return jax.ops.segment_sum(yb, row_tok, num_segments=T + 1)[:T]


def _trunk(x, c, ada_w, ada_b, norm1_g, norm2_g,
           ret_w_in, ret_log_decay, ret_gn_g, ret_w_out,
           gm_w_in, gm_b_in, gm_vn_g, gm_vn_b, gm_w_s, gm_b_s, gm_w_out, gm_b_out,
           moe_w_r, moe_b_r, moe_w_gu, moe_b_gu, moe_w_dn, moe_b_dn, final_g):
    B, S, D = x.shape
    c_act = jax.nn.silu(c)
    for i in range(DEPTH):
        mod = c_act @ ada_w[i] + ada_b[i]
        sh1, sc1, g1, sh2, sc2, g2 = [m[:, None, :] for m in jnp.split(mod, N_MOD, axis=-1)]
        h = _rmsnorm(x, norm1_g[i]) * (1.0 + sc1) + sh1
        j = i // N_MIXERS
        if i % N_MIXERS == 0:
            mix = _retention_mixer(h, ret_w_in[j], ret_log_decay[j], ret_gn_g[j], ret_w_out[j])
        else:
            mix = _gmlp_mixer(h, gm_w_in[j], gm_b_in[j], gm_vn_g[j], gm_vn_b[j],
                              gm_w_s[j], gm_b_s[j], gm_w_out[j], gm_b_out[j])
        x = x + g1 * mix
        h = _rmsnorm(x, norm2_g[i]) * (1.0 + sc2) + sh2
        y = _moe(h.reshape(B * S, D), moe_w_r[i], moe_b_r[i], moe_w_gu[i], moe_b_gu[i],
                 moe_w_dn[i], moe_b_dn[i]).reshape(B, S, D)
        x = x + g2 * y
    return _rmsnorm(x, final_g)


def setup_inputs(seed: int = 0) -> dict:
    key = jax.random.key(seed)
    ks = jax.random.split(key, 32)
    f32 = jnp.float32

    def nrm(k, shape, scale):
        return jax.random.normal(k, shape, f32) * scale

    D = D_MODEL
    return {
        "x_prompt": nrm(ks[0], (BATCH, SEQ, D), 1.0),
        "x_sample": nrm(ks[1], (DEC_BATCH, DEC_SEQ, D), 1.0),
        "c_prompt": nrm(ks[2], (BATCH, D), 1.0),
        "c_sample": nrm(ks[3], (DEC_BATCH, D), 1.0),
        "ada_w": nrm(ks[4], (DEPTH, D, N_MOD * D), 0.5 * D ** -0.5),
        "ada_b": nrm(ks[5], (DEPTH, N_MOD * D), 0.02),
        "norm1_g": 1.0 + nrm(ks[6], (DEPTH, D), 0.02),
        "norm2_g": 1.0 + nrm(ks[7], (DEPTH, D), 0.02),
        "ret_w_in": nrm(ks[8], (N_RET_LAYERS, D, RET_IN), D ** -0.5),
        "ret_log_decay": jnp.log1p(-(2.0 ** (-5.0 - jnp.arange(RET_HEADS, dtype=f32)
                                             + nrm(ks[9], (N_RET_LAYERS, 2, RET_HEADS), 0.1)))),
        "ret_gn_g": 1.0 + nrm(ks[10], (N_RET_LAYERS, RET_VDIM), 0.02),
        "ret_w_out": nrm(ks[11], (N_RET_LAYERS, RET_VDIM, D), RET_VDIM ** -0.5),
        "gm_w_in": nrm(ks[12], (N_GM_LAYERS, D, 2 * GM_WIDTH), D ** -0.5),
        "gm_b_in": nrm(ks[13], (N_GM_LAYERS, 2 * GM_WIDTH), 0.02),
        "gm_vn_g": 1.0 + nrm(ks[14], (N_GM_LAYERS, GM_WIDTH), 0.02),
        "gm_vn_b": nrm(ks[15], (N_GM_LAYERS, GM_WIDTH), 0.02),
        "gm_w_s": nrm(ks[16], (N_GM_LAYERS, GM_GROUPS, GM_CHUNK, GM_CHUNK), GM_CHUNK ** -0.5),
        "gm_b_s": 1.0 + nrm(ks[17], (N_GM_LAYERS, GM_GROUPS, GM_CHUNK), 0.02),
        "gm_w_out": nrm(ks[18], (N_GM_LAYERS, GM_WIDTH, D), GM_WIDTH ** -0.5),
        "gm_b_out": nrm(ks[19], (N_GM_LAYERS, D), 0.02),
        "moe_w_r": nrm(ks[20], (DEPTH, D, N_EXPERTS), D ** -0.5),
        "moe_b_r": nrm(ks[21], (DEPTH, N_EXPERTS), 0.01),
        "moe_w_gu": nrm(ks[22], (DEPTH, N_EXPERTS, D, 2 * D_EXPERT), D ** -0.5),
        "moe_b_gu": nrm(ks[23], (DEPTH, N_EXPERTS, 2 * D_EXPERT), 0.02),
        "moe_w_dn": nrm(ks[24], (DEPTH, N_EXPERTS, D_EXPERT, D), D_EXPERT ** -0.5),
        "moe_b_dn": nrm(ks[25], (DEPTH, N_EXPERTS, D), 0.02),
        "final_g": 1.0 + nrm(ks[26], (D,), 0.02),
    }


def reference(x_prompt, x_sample, c_prompt, c_sample, ada_w, ada_b, norm1_g, norm2_g,
              ret_w_in, ret_log_decay, ret_gn_g, ret_w_out,
              gm_w_in, gm_b_in, gm_vn_g, gm_vn_b, gm_w_s, gm_b_s, gm_w_out, gm_b_out,
              moe_w_r, moe_b_r, moe_w_gu, moe_b_gu, moe_w_dn, moe_b_dn, final_g):
    y_prompt = _trunk(x_prompt, c_prompt, ada_w, ada_b, norm1_g, norm2_g,
                      ret_w_in, ret_log_decay, ret_gn_g, ret_w_out,
                      gm_w_in, gm_b_in, gm_vn_g, gm_vn_b, gm_w_s, gm_b_s, gm_w_out, gm_b_out,
                      moe_w_r, moe_b_r, moe_w_gu, moe_b_gu, moe_w_dn, moe_b_dn, final_g)
    y_sample = _trunk(x_sample, c_sample, ada_w, ada_b, norm1_g, norm2_g,
                      ret_w_in, ret_log_decay, ret_gn_g, ret_w_out,
                      gm_w_in, gm_b_in, gm_vn_g, gm_vn_b, gm_w_s, gm_b_s, gm_w_out, gm_b_out,
                      moe_w_r, moe_b_r, moe_w_gu, moe_b_gu, moe_w_dn, moe_b_dn, final_g)
    return (y_prompt, y_sample)
```

```python
import numpy as np
from contextlib import ExitStack
import concourse.bass as bass
import concourse.mybir as mybir
from concourse.bass_utils import run_bass_kernel_spmd

F32 = mybir.dt.float32
BF16 = mybir.dt.bfloat16
ALU = mybir.AluOpType
AF = mybir.ActivationFunctionType
EPS = 1e-6


class Buf:
    __slots__ = ("name", "w", "r", "sem", "cnt", "dram_tokens", "scope")

    def __init__(self, name, dram=False):
        self.name = name
        self.scope = 0
        self.w = None
        self.r = {}
        self.sem = None
        self.cnt = 0
        self.dram_tokens = {} if dram else None


class Eng:
    def __init__(self, name, inst, sem):
        self.name = name
        self.inst = inst
        self.sem = sem
        self.cnt = 0
        self.seen = {}
        self.pend_r = []
        self.pend_w = []


class MK:
    def __init__(self, nc, stack):
        self.nc = nc
        self.stacks = [stack]
        self.engs = {}
        for name, inst in (("pe", nc.tensor), ("act", nc.scalar), ("dve", nc.vector),
                           ("pool", nc.gpsimd), ("sp", nc.sync)):
            sem = stack.enter_context(nc.semaphore("s_" + name))
            self.engs[name] = Eng(name, inst, sem)
        self.dma_bufs = []
        self.scope_dma = [[]]
        self.sem_pool = []
        self.dbg = False
        self.window = 6
        self.n_inst = 0
        self.n_wait = 0
        self.uid = 0

    def push(self):
        st = ExitStack()
        st.__enter__()
        self.stacks.append(st)
        self.scope_dma.append([])

    def pop(self):
        self.barrier()
        st = self.stacks.pop()
        for b in self.scope_dma.pop():
            self.dma_bufs.remove(b)
            self.sem_pool.append((b.sem, b.cnt))
        st.__exit__(None, None, None)

    def sb(self, name, shape, dtype):
        self.uid += 1
        t = self.stacks[-1].enter_context(self.nc.sbuf_tensor(f"{name}_{self.uid}", list(shape), dtype))
        b = Buf(name)
        b.scope = len(self.stacks) - 1
        return t, b

    def ps(self, name, shape, dtype=F32):
        self.uid += 1
        t = self.stacks[-1].enter_context(self.nc.psum_tensor(f"{name}_{self.uid}", list(shape), dtype))
        return t, Buf(name)

    def dram(self, name, shape, dtype):
        t = self.nc.dram_tensor(name, list(shape), dtype, kind=("ExternalOutput" if self.dbg else "Internal"))
        return t.ap(), Buf(name, dram=True)

    def _dsem(self, b):
        if b.sem is None:
            self.uid += 1
            if self.sem_pool:
                b.sem, b.cnt = self.sem_pool.pop()
            else:
                b.sem = self.stacks[0].enter_context(self.nc.semaphore(f"d_{b.name}_{self.uid}"))
            self.dma_bufs.append(b)
            self.scope_dma[b.scope].append(b)
        return b.sem

    def _wait(self, e, tokens):
        best = {}
        for tok in tokens:
            if tok is None:
                continue
            sem, val, owner = tok
            if owner == e.name:
                if e.name == "pe" or val <= e.cnt - self.window:
                    continue
            k = id(sem)
            if k not in best or best[k][1] < val:
                best[k] = (sem, val)
        for k, (sem, val) in best.items():
            if e.seen.get(k, 0) >= val:
                continue
            e.inst.wait_ge(sem, val)
            e.seen[k] = val
            self.n_wait += 1

    @staticmethod
    def _deps(reads, writes):
        toks = []
        for b in reads:
            toks.append(b.w)
            if b.dram_tokens:
                toks.extend(b.dram_tokens.values())
        for b in writes:
            toks.append(b.w)
            toks.extend(b.r.values())
            if b.dram_tokens:
                toks.extend(b.dram_tokens.values())
        return toks

    @staticmethod
    def _commit(tok, key, reads, writes):
        for b in writes:
            if b.dram_tokens is not None:
                b.dram_tokens[id(tok[0])] = tok
            else:
                b.w = tok
            b.r = {}
        for b in reads:
            b.r[key] = tok

    def op(self, eng, fn, reads=(), writes=(), inc=True):
        e = self.engs[eng]
        self._wait(e, self._deps(reads, writes))
        ins = fn(e.inst)
        self.n_inst += 1
        if not inc:
            e.pend_r.extend(reads)
            e.pend_w.extend(writes)
            return ins
        e.cnt += 1
        ins.then_inc(e.sem, 1)
        tok = (e.sem, e.cnt, e.name)
        self._commit(tok, e.name, list(reads) + e.pend_r, list(writes) + e.pend_w)
        e.pend_r = []
        e.pend_w = []
        return ins

    def dma(self, out, in_, sbuf_side, reads=(), writes=(), q="sp"):
        e = self.engs[q]
        self._wait(e, self._deps(reads, writes))
        sem = self._dsem(sbuf_side)
        ins = e.inst.dma_start(out=out, in_=in_)
        sbuf_side.cnt += 16
        ins.then_inc(sem, 16)
        tok = (sem, sbuf_side.cnt, None)
        self._commit(tok, ("dma", id(sem)), reads, writes)
        self.n_inst += 1
        return ins

    def barrier(self):
        for e in self.engs.values():
            assert not e.pend_r and not e.pend_w
        for e in self.engs.values():
            for o in self.engs.values():
                if o is e or o.cnt == 0:
                    continue
                if e.seen.get(id(o.sem), 0) < o.cnt:
                    e.inst.wait_ge(o.sem, o.cnt)
                    e.seen[id(o.sem)] = o.cnt
            for b in self.dma_bufs:
                if b.cnt and e.seen.get(id(b.sem), 0) < b.cnt:
                    e.inst.wait_ge(b.sem, b.cnt)
                    e.seen[id(b.sem)] = b.cnt

    def finish(self):
        self.barrier()


class Cfg:
    def __init__(self, NC=8, TS=64, TP=16, NE=32, DE=1024):
        self.NC, self.TS, self.TP, self.NE, self.DE = NC, TS, TP, NE, DE
        self.D = 1024
        self.NT = TS + TP
        self.NO = TP * (NC - 1)
        self.ST = 4 if self.NT % 4 == 0 else (2 if self.NT % 2 == 0 else 1)


CT_EF, CT_MF, CT_EB, CT_MB, CT_IP1, CT_CMI, CT_C127, CT_PIDX, CT_W = 0, 128, 256, 384, 512, 640, 768, 769, 770


def make_ctab():
    i = np.arange(128, dtype=np.float32)
    j = i[:, None]
    ii = i[None, :]
    t = np.zeros((128, CT_W), np.float32)
    t[:, CT_EF:CT_EF + 128] = np.maximum(ii - j, 0)
    t[:, CT_MF:CT_MF + 128] = (ii >= j)
    t[:, CT_EB:CT_EB + 128] = np.maximum(j - ii, 0)
    t[:, CT_MB:CT_MB + 128] = (j > ii)
    t[:, CT_IP1:CT_IP1 + 128] = ii + 1
    t[:, CT_CMI:CT_CMI + 128] = 128 - ii
    t[:, CT_C127] = 127 - i
    t[:, CT_PIDX] = i
    return t


def rope_table(pos):
    half = 128
    inv = (np.float32(10000.0) ** (-np.arange(half, dtype=np.float32) / np.float32(half))).astype(np.float32)
    ang = (pos.astype(np.float32)[:, None] * inv[None, :]).astype(np.float32)
    return np.concatenate([np.cos(ang), np.sin(ang)], axis=1).astype(np.float32)


class StopBuild(Exception):
    pass


def build(cfg, dbg=False, stop_after=None):
    nc = bass.Bass("TRN2", target_bir_lowering=False)

    def check(name):
        if stop_after == name:
            raise StopBuild()
    D, NT, NO, NE, DE, TS, TP = cfg.D, cfg.NT, cfg.NO, cfg.NE, cfg.DE, cfg.TS, cfg.TP

    def IN(name, shape):
        return nc.dram_tensor(name, list(shape), F32, kind="ExternalInput").ap()

    xs = IN("xs", [NT * 128, D])
    xo = IN("xo", [max(NO, 1) * 128, D])
    ccol = IN("ccol", [128, 16])
    rope_own = IN("rope_own", [NT * 128, 256])
    rope_oth = IN("rope_oth", [max(NO, 1) * 128, 256])
    oth_meta = IN("oth_meta", [max(NO, 1) * 128, 4])
    ctab_d = IN("ctab", [128, CT_W])
    ada_w = IN("ada_w", [2 * 1024, 6144])
    ada_b = IN("ada_b", [2, 6144])
    norm1_g = IN("norm1_g", [2, 1024])
    norm2_g = IN("norm2_g", [2, 1024])
    ret_w_in = IN("ret_w_in", [1024, 6144])
    ret_ld = IN("ret_ld", [1, 8])
    ret_gn_g = IN("ret_gn_g", [1, 2048])
    ret_w_out = IN("ret_w_out", [2048, 1024])
    gm_w_in = IN("gm_w_in", [1024, 4096])
    gm_b_in = IN("gm_b_in", [1, 4096])
    gm_vn_g = IN("gm_vn_g", [1, 2048])
    gm_vn_b = IN("gm_vn_b", [1, 2048])
    gm_w_sT = IN("gm_w_sT", [4 * 128, 128])
    gm_b_sT = IN("gm_b_sT", [128, 4])
    gm_w_out = IN("gm_w_out", [2048, 1024])
    gm_b_out = IN("gm_b_out", [1, 1024])
    moe_w_r = IN("moe_w_r", [2 * 1024, NE])
    moe_b_r = IN("moe_b_r", [2, NE])
    moe_w_gu = IN("moe_w_gu", [2 * NE * 1024, 2 * DE])
    moe_b_gu = IN("moe_b_gu", [2 * NE, 2 * DE])
    moe_w_dn = IN("moe_w_dn", [2 * NE * DE, 1024])
    moe_b_dn = IN("moe_b_dn", [2 * NE, 1024])
    final_g = IN("final_g", [1, 1024])
    y_out = nc.dram_tensor("y", [NT * 128, D], F32, kind="ExternalOutput").ap()

    KE = DE // 128
    GB = (2 * DE) // 512
    GH = GB // 2

    with ExitStack() as root:
        mk = MK(nc, root)
        mk.dbg = dbg
        op, dma = mk.op, mk.dma
        try:
          if True:

            modv, modv_b = mk.dram("modv", [4, 6144], F32)
            r1, r1_b = mk.dram("r1", [NT * 128, 7168], BF16)
            osc, osc_b = mk.dram("osc", [max(NO, 1) * 128, 4096], BF16)
            sbst, sbst_b = mk.dram("sbst", [NT * 128, 4096], BF16)
            xa, xa_b = mk.dram("xa", [NT * 128, D], F32)
            xb, xb_b = mk.dram("xb", [NT * 128, D], F32)
            h2d, h2d_b = mk.dram("h2d", [NT * 128, D], BF16)
            xsb = Buf("xs_in", dram=True)
            xob = Buf("xo_in", dram=True)
            wdr = Buf("weights_in", dram=True)
            yb_ = Buf("y_out", dram=True)

            ident, ident_b = mk.sb("ident", [128, 128], BF16)
            identf, identf_b = mk.sb("identf", [128, 128], F32)
            ones1, ones1_b = mk.sb("ones1", [1, 128], BF16)
            ctab, ctab_b = mk.sb("ctab", [128, CT_W], F32)
            lg, lg_b = mk.sb("lg", [128, 8], F32)
            cdec, cdec_b = mk.sb("cdec", [128, 8], F32)

            dma(ctab[:], ctab_d[:, :], ctab_b, reads=[wdr], writes=[ctab_b])
            dma(lg[:], ret_ld[0:1, :].partition_broadcast(128), lg_b, reads=[wdr], writes=[lg_b])
            op("pool", lambda g: g.memset(identf[:], 0.0), writes=[identf_b])
            op("pool", lambda g: g.affine_select(out=identf[:], in_=identf[:], pattern=[[-1, 128]],
                                                 compare_op=ALU.not_equal, fill=1.0, base=0, channel_multiplier=1),
               reads=[identf_b], writes=[identf_b])
            op("pool", lambda g: g.tensor_copy(out=ident[:], in_=identf[:]), reads=[identf_b], writes=[ident_b])
            op("pool", lambda g: g.memset(ones1[:], 1.0), writes=[ones1_b])
            op("act", lambda a: a.activation(out=cdec[:], in_=lg[:], func=AF.Exp, scale=128.0),
               reads=[lg_b], writes=[cdec_b])

            def seq_of(t):
                return 0 if t < TS else 1

            def load_bc(dst, dst_b, src_row):
                dma(dst, src_row.partition_broadcast(128), dst_b, reads=[wdr, modv_b], writes=[dst_b])

            def make_mod(l, sub, want_gate, want_norm=True):
                ng_src = (norm1_g if sub == 0 else norm2_g)
                res = []
                if want_norm:
                    ng, ng_b = mk.sb("ng", [128, 1024], F32)
                    load_bc(ng[:], ng_b, ng_src[l:l + 1, :])
                for s in range(2):
                    row = l * 2 + s
                    gm = gm_b = sh = sh_b = None
                    if want_norm:
                        gm, gm_b = mk.sb("gm", [128, 1024], F32)
                        sh, sh_b = mk.sb("sh", [128, 1024], F32)
                        load_bc(sh[:], sh_b, modv[row:row + 1, (3 * sub) * 1024:(3 * sub + 1) * 1024])
                        load_bc(gm[:], gm_b, modv[row:row + 1, (3 * sub + 1) * 1024:(3 * sub + 2) * 1024])
                        op("dve", lambda v: v.scalar_tensor_tensor(out=gm[:], in0=gm[:], scalar=1.0, in1=ng[:],
                                                                  op0=ALU.add, op1=ALU.mult),
                           reads=[gm_b, ng_b], writes=[gm_b])
                    gt = gt_b = None
                    if want_gate:
                        gt, gt_b = mk.sb("gt", [128, 1024], F32)
                        load_bc(gt[:], gt_b, modv[row:row + 1, (3 * sub + 2) * 1024:(3 * sub + 3) * 1024])
                    res.append((gm, gm_b, sh, sh_b, gt, gt_b))
                return res

            class NormBufs:
                def __init__(self):
                    self.ss = [mk.sb("ss", [128, 1], F32) for _ in range(2)]
                    _tt = mk.sb("tt", [128, 1024], F32)
                    self.tt = [_tt, _tt]
                    self.hh = [mk.sb("hh", [128, 1024], BF16) for _ in range(2)]
                    self.hT = [mk.sb("hT", [128, 8, 128], BF16) for _ in range(2)]
                    self.hTp, self.hTp_b = mk.ps("hTp", [128, 8, 128], BF16)

            def norm_mod_T(nb, par, x_t, x_b, gm, gm_b, sh, sh_b, out_hT=None):
                ss, ss_b = nb.ss[par]
                tt, tt_b = nb.tt[par]
                hh, hh_b = nb.hh[par]
                hT, hT_b = nb.hT[par] if out_hT is None else out_hT
                op("act", lambda a: a.activation(out=tt[:], in_=x_t, func=AF.Square, accum_out=ss[:]),
                   reads=[x_b], writes=[tt_b, ss_b])
                op("dve", lambda v: v.tensor_scalar(out=ss[:], in0=ss[:], scalar1=1.0 / D, scalar2=EPS,
                                                    op0=ALU.mult, op1=ALU.add), reads=[ss_b], writes=[ss_b])
                op("act", lambda a: a.sqrt(out=ss[:], in_=ss[:]), reads=[ss_b], writes=[ss_b])
                op("dve", lambda v: v.reciprocal(out=ss[:], in_=ss[:]), reads=[ss_b], writes=[ss_b])
                op("dve", lambda v: v.scalar_tensor_tensor(out=tt[:], in0=x_t, scalar=ss[:, 0:1], in1=gm[:],
                                                          op0=ALU.mult, op1=ALU.mult),
                   reads=[x_b, ss_b, gm_b], writes=[tt_b])
                op("pool", lambda g: g.tensor_tensor(out=hh[:], in0=tt[:], in1=sh[:], op=ALU.add),
                   reads=[tt_b, sh_b], writes=[hh_b])
                for k in range(8):
                    op("pe", lambda p: p.transpose(out=nb.hTp[:, k, :], in_=hh[:, k * 128:(k + 1) * 128], identity=ident[:]),
                       reads=[hh_b, ident_b], writes=[nb.hTp_b], inc=(k == 7))
                op("act", lambda a: a.copy(out=hT[:], in_=nb.hTp[:]), reads=[nb.hTp_b], writes=[hT_b])
                return hT, hT_b, hh, hh_b

            def load_weight_bf16(dst, dst_b, src, rows, cols, stg_list, col_chunk):
                i = 0
                for kc in range(rows // 128):
                    for c0 in range(0, cols, col_chunk):
                        stg, stg_b = stg_list[i % len(stg_list)]
                        i += 1
                        cw = min(col_chunk, cols - c0)
                        dma(stg[:, 0:cw], src[kc * 128:(kc + 1) * 128, c0:c0 + cw], stg_b, reads=[wdr], writes=[stg_b])
                        op("pool", lambda g: g.tensor_copy(out=dst[:, kc, c0:c0 + cw], in_=stg[:, 0:cw]),
                           reads=[stg_b], writes=[dst_b])

            mk.push()
            cact, cact_b = mk.sb("cact", [128, 16], F32)
            adab, adab_b = mk.sb("adab", [2, 6144], F32)
            modsb, modsb_b = mk.sb("modsb", [2, 6144], F32)
            wst = [mk.sb("wst", [128, 3072], F32) for _ in range(2)]
            mps = [mk.ps("mps", [128, 512], F32) for _ in range(6)]
            dma(cact[:], ccol[:, :], cact_b, reads=[wdr], writes=[cact_b])
            op("act", lambda a: a.activation(out=cact[:], in_=cact[:], func=AF.Silu), reads=[cact_b], writes=[cact_b])
            for l in range(2):
                for s in range(2):
                    dma(adab[s:s + 1, :], ada_b[l:l + 1, :], adab_b, reads=[wdr], writes=[adab_b])
                for half in range(2):
                    for k in range(8):
                        w, w_b = wst[k % 2]
                        dma(w[:], ada_w[l * 1024 + k * 128: l * 1024 + (k + 1) * 128, half * 3072:(half + 1) * 3072],
                            w_b, reads=[wdr], writes=[w_b])
                        for b in range(6):
                            op("pe", lambda p: p.matmul(mps[b][0][0:2, :], lhsT=cact[:, 2 * k:2 * k + 2],
                                                        rhs=w[:, b * 512:(b + 1) * 512], start=(k == 0), stop=(k == 7)),
                               reads=[cact_b, w_b], writes=[mps[b][1]], inc=(b == 5))
                    for b in range(6):
                        c0 = half * 3072 + b * 512
                        op("dve", lambda v: v.tensor_tensor(out=modsb[:, c0:c0 + 512], in0=mps[b][0][0:2, :],
                                                            in1=adab[:, c0:c0 + 512], op=ALU.add),
                           reads=[mps[b][1], adab_b], writes=[modsb_b])
                dma(modv[2 * l:2 * l + 2, :], modsb[:], modsb_b, reads=[modsb_b], writes=[modv_b])
            mk.pop()
            check("M")

            mk.push()
            Win, Win_b = mk.sb("Win", [128, 8, 6144], BF16)
            mk.push()
            stg = [mk.sb("stg", [128, 3072], F32) for _ in range(2)]
            load_weight_bf16(Win, Win_b, ret_w_in, 1024, 6144, stg, 3072)
            mk.pop()
            mods = make_mod(0, 0, False)
            nb = NormBufs()
            xt = [mk.sb("xt", [128, 1024], F32) for _ in range(2)]
            cst = [mk.sb("cst", [128, 256], F32) for _ in range(2)]
            ra, ra_b = mk.sb("ra", [128, 256], F32)
            rb, rb_b = mk.sb("rb", [128, 256], F32)
            rc, rc_b = mk.sb("rc", [128, 256], F32)
            rd, rd_b = mk.sb("rd", [128, 256], F32)
            qr, qr_b = mk.sb("qr", [128, 1024], BF16)
            pj = [mk.ps("pj", [128, 512], F32) for _ in range(4)]
            qTp, qTp_b = mk.ps("qTp", [128, 8, 128], BF16)
            kTp, kTp_b = mk.ps("kTp", [128, 8, 128], BF16)
            pjn = [0]

            def proj_block(hT, hT_b, cb):
                ps, ps_b = pj[pjn[0] % 4]
                pjn[0] += 1
                for k in range(8):
                    op("pe", lambda p: p.matmul(ps[:], lhsT=hT[:, k, :], rhs=Win[:, k, cb * 512:(cb + 1) * 512],
                                                start=(k == 0), stop=(k == 7)),
                       reads=[hT_b, Win_b], writes=[ps_b], inc=(k == 7))
                return ps, ps_b

            def rope_block(ps, ps_b, cs, cs_b, dst, dst_b, c0):
                pv = ps[:].rearrange("p (h t d) -> p h t d", h=2, t=2)
                dv = dst[:, c0:c0 + 512].rearrange("p (h t d) -> p h t d", h=2, t=2)
                cosb = cs[:, 0:128].unsqueeze(1).to_broadcast([128, 2, 128])
                sinb = cs[:, 128:256].unsqueeze(1).to_broadcast([128, 2, 128])
                v3 = lambda t_: t_[:].rearrange("p (h d) -> p h d", h=2)
                op("dve", lambda v: v.tensor_tensor(out=v3(ra), in0=pv[:, :, 0, :], in1=cosb, op=ALU.mult),
                   reads=[ps_b, cs_b], writes=[ra_b])
                op("dve", lambda v: v.tensor_tensor(out=v3(rb), in0=pv[:, :, 1, :], in1=sinb, op=ALU.mult),
                   reads=[ps_b, cs_b], writes=[rb_b])
                op("dve", lambda v: v.tensor_tensor(out=v3(rc), in0=pv[:, :, 1, :], in1=cosb, op=ALU.mult),
                   reads=[ps_b, cs_b], writes=[rc_b])
                op("dve", lambda v: v.tensor_tensor(out=v3(rd), in0=pv[:, :, 0, :], in1=sinb, op=ALU.mult),
                   reads=[ps_b, cs_b], writes=[rd_b])
                op("pool", lambda g: g.tensor_tensor(out=dv[:, :, 0, :], in0=v3(ra), in1=v3(rb), op=ALU.subtract),
                   reads=[ra_b, rb_b], writes=[dst_b])
                op("pool", lambda g: g.tensor_tensor(out=dv[:, :, 1, :], in0=v3(rc), in1=v3(rd), op=ALU.add),
                   reads=[rc_b, rd_b], writes=[dst_b])

            mk.push()
            so = [mk.sb("so", [128, 7168], BF16) for _ in range(2)]
            dma(xt[0][0][:], xs[0:128, :], xt[0][1], reads=[xsb], writes=[xt[0][1]])
            dma(cst[0][0][:], rope_own[0:128, :], cst[0][1], reads=[wdr], writes=[cst[0][1]])
            for t in range(NT):
                par = t % 2
                if t + 1 < NT:
                    dma(xt[1 - par][0][:], xs[(t + 1) * 128:(t + 2) * 128, :], xt[1 - par][1], reads=[xsb], writes=[xt[1 - par][1]])
                    dma(cst[1 - par][0][:], rope_own[(t + 1) * 128:(t + 2) * 128, :], cst[1 - par][1], reads=[wdr],
                        writes=[cst[1 - par][1]])
                s = seq_of(t)
                gm, gm_b, sh, sh_b, _, _ = mods[s]
                x_t, x_b = xt[par]
                cs, cs_b = cst[par]
                o, o_b = so[par]
                hT, hT_b, _, _ = norm_mod_T(nb, par, x_t[:], x_b, gm, gm_b, sh, sh_b)
                for cb in range(2):
                    ps, ps_b = proj_block(hT, hT_b, cb)
                    rope_block(ps, ps_b, cs, cs_b, qr, qr_b, cb * 512)
                for k in range(8):
                    op("pe", lambda p: p.transpose(out=qTp[:, k, :], in_=qr[:, k * 128:(k + 1) * 128], identity=ident[:]),
                       reads=[qr_b, ident_b], writes=[qTp_b], inc=(k == 7))
                op("act", lambda a: a.copy(out=o[:, 0:1024].rearrange("p (k i) -> p k i", k=8), in_=qTp[:]),
                   reads=[qTp_b], writes=[o_b])
                for cb in range(2):
                    ps, ps_b = proj_block(hT, hT_b, 2 + cb)
                    rope_block(ps, ps_b, cs, cs_b, o, o_b, 2048 + cb * 512)
                for k in range(8):
                    op("pe", lambda p: p.transpose(out=kTp[:, k, :], in_=o[:, 2048 + k * 128:2048 + (k + 1) * 128],
                                                   identity=ident[:]),
                       reads=[o_b, ident_b], writes=[kTp_b], inc=(k == 7))
                op("act", lambda a: a.copy(out=o[:, 1024:2048].rearrange("p (k i) -> p k i", k=8), in_=kTp[:]),
                   reads=[kTp_b], writes=[o_b])
                for cb in range(4):
                    ps, ps_b = proj_block(hT, hT_b, 4 + cb)
                    op("act", lambda a: a.copy(out=o[:, 3072 + cb * 512:3072 + (cb + 1) * 512], in_=ps[:]),
                       reads=[ps_b], writes=[o_b])
                for cb in range(4):
                    ps, ps_b = proj_block(hT, hT_b, 8 + cb)
                    op("act", lambda a: a.activation(out=o[:, 5120 + cb * 512:5120 + (cb + 1) * 512], in_=ps[:], func=AF.Silu),
                       reads=[ps_b], writes=[o_b])
                dma(r1[t * 128:(t + 1) * 128, :], o[:], o_b, reads=[o_b], writes=[r1_b])
            mk.pop()

            check("R1")
            if NO > 0:
                mk.push()
                so2 = [mk.sb("so2", [128, 4096], BF16) for _ in range(2)]
                mt = [mk.sb("mt", [128, 4], F32) for _ in range(2)]
                sc8 = [mk.sb("sc8", [128, 8], F32) for _ in range(2)]
                kr, kr_b = mk.sb("kr", [128, 1024], F32)
                dma(xt[0][0][:], xo[0:128, :], xt[0][1], reads=[xob], writes=[xt[0][1]])
                dma(cst[0][0][:], rope_oth[0:128, :], cst[0][1], reads=[wdr], writes=[cst[0][1]])
                dma(mt[0][0][:], oth_meta[0:128, :], mt[0][1], reads=[wdr], writes=[mt[0][1]])
                gm, gm_b, sh, sh_b, _, _ = mods[1]
                for t in range(NO):
                    par = t % 2
                    if t + 1 < NO:
                        dma(xt[1 - par][0][:], xo[(t + 1) * 128:(t + 2) * 128, :], xt[1 - par][1], reads=[xob],
                            writes=[xt[1 - par][1]])
                        dma(cst[1 - par][0][:], rope_oth[(t + 1) * 128:(t + 2) * 128, :], cst[1 - par][1], reads=[wdr],
                            writes=[cst[1 - par][1]])
                        dma(mt[1 - par][0][:], oth_meta[(t + 1) * 128:(t + 2) * 128, :], mt[1 - par][1], reads=[wdr],
                            writes=[mt[1 - par][1]])
                    x_t, x_b = xt[par]
                    cs, cs_b = cst[par]
                    o, o_b = so2[par]
                    m, m_b = mt[par]
                    sc, sc_b = sc8[par]
                    hT, hT_b, _, _ = norm_mod_T(nb, par, x_t[:], x_b, gm, gm_b, sh, sh_b)
                    op("act", lambda a: a.activation(out=sc[:, 0:4], in_=lg[:, 0:4], func=AF.Exp, scale=m[:, 0:1]),
                       reads=[lg_b, m_b], writes=[sc_b])
                    op("act", lambda a: a.activation(out=sc[:, 4:8], in_=lg[:, 4:8], func=AF.Exp, scale=m[:, 2:3]),
                       reads=[lg_b, m_b], writes=[sc_b])
                    op("dve", lambda v: v.tensor_scalar(out=sc[:, 0:4], in0=sc[:, 0:4], scalar1=m[:, 1:2], scalar2=None,
                                                        op0=ALU.mult), reads=[sc_b, m_b], writes=[sc_b])
                    op("dve", lambda v: v.tensor_scalar(out=sc[:, 4:8], in0=sc[:, 4:8], scalar1=m[:, 3:4], scalar2=None,
                                                        op0=ALU.mult), reads=[sc_b, m_b], writes=[sc_b])
                    for cb in range(2):
                        ps, ps_b = proj_block(hT, hT_b, 2 + cb)
                        rope_block(ps, ps_b, cs, cs_b, kr, kr_b, cb * 512)
                    for h in range(4):
                        op("dve", lambda v: v.tensor_scalar(out=o[:, h * 256:(h + 1) * 256], in0=kr[:, h * 256:(h + 1) * 256],
                                                            scalar1=sc[:, h:h + 1], scalar2=None, op0=ALU.mult),
                           reads=[kr_b, sc_b], writes=[o_b])
                        op("pool", lambda g: g.tensor_scalar(out=o[:, 1024 + h * 256:1024 + (h + 1) * 256],
                                                             in0=kr[:, h * 256:(h + 1) * 256],
                                                             scalar1=sc[:, 4 + h:5 + h], scalar2=None, op0=ALU.mult),
                           reads=[kr_b, sc_b], writes=[o_b])
                    for cb in range(4):
                        ps, ps_b = proj_block(hT, hT_b, 4 + cb)
                        op("act", lambda a: a.copy(out=o[:, 2048 + cb * 512:2048 + (cb + 1) * 512], in_=ps[:]),
                           reads=[ps_b], writes=[o_b])
                    dma(osc[t * 128:(t + 1) * 128, :], o[:], o_b, reads=[o_b], writes=[osc_b])
                mk.pop()
            mk.pop()
            check("O")

            mk.push()
            DT, DT_b = mk.sb("DT", [128, 512], F32)
            qdF, qdF_b = mk.sb("qdF", [128, 8, 128], F32)
            qdB, qdB_b = mk.sb("qdB", [128, 8, 128], F32)
            kdec, kdec_b = mk.sb("kdec", [128, 8], F32)
            tmpd, tmpd_b = mk.sb("tmpd", [128, 128], F32)
            for h in range(4):
                op("act", lambda a: a.activation(out=tmpd[:], in_=ctab[:, CT_EF:CT_EF + 128], func=AF.Exp, scale=lg[:, h:h + 1]),
                   reads=[ctab_b, lg_b], writes=[tmpd_b])
                op("dve", lambda v: v.scalar_tensor_tensor(out=DT[:, h * 128:(h + 1) * 128], in0=tmpd[:], scalar=1.0 / 16,
                                                          in1=ctab[:, CT_MF:CT_MF + 128], op0=ALU.mult, op1=ALU.mult),
                   reads=[tmpd_b, ctab_b], writes=[DT_b])
                op("act", lambda a: a.activation(out=tmpd[:], in_=ctab[:, CT_EB:CT_EB + 128], func=AF.Exp,
                                                 scale=lg[:, 4 + h:5 + h]),
                   reads=[ctab_b, lg_b], writes=[tmpd_b])
                op("dve", lambda v: v.scalar_tensor_tensor(out=tmpd[:], in0=tmpd[:], scalar=1.0 / 16,
                                                          in1=ctab[:, CT_MB:CT_MB + 128], op0=ALU.mult, op1=ALU.mult),
                   reads=[tmpd_b, ctab_b], writes=[tmpd_b])
                op("dve", lambda v: v.tensor_tensor(out=DT[:, h * 128:(h + 1) * 128], in0=DT[:, h * 128:(h + 1) * 128],
                                                    in1=tmpd[:], op=ALU.add), reads=[tmpd_b, DT_b], writes=[DT_b])
                for dc in range(2):
                    op("act", lambda a: a.activation(out=qdF[:, h * 2 + dc, :], in_=ctab[:, CT_IP1:CT_IP1 + 128], func=AF.Exp,
                                                     scale=lg[:, h:h + 1]), reads=[ctab_b, lg_b], writes=[qdF_b])
                    op("act", lambda a: a.activation(out=qdB[:, h * 2 + dc, :], in_=ctab[:, CT_CMI:CT_CMI + 128], func=AF.Exp,
                                                     scale=lg[:, 4 + h:5 + h]), reads=[ctab_b, lg_b], writes=[qdB_b])
            op("dve", lambda v: v.tensor_scalar(out=qdF[:], in0=qdF[:], scalar1=1.0 / 16, scalar2=None, op0=ALU.mult),
               reads=[qdF_b], writes=[qdF_b])
            op("dve", lambda v: v.tensor_scalar(out=qdB[:], in0=qdB[:], scalar1=1.0 / 16, scalar2=None, op0=ALU.mult),
               reads=[qdB_b], writes=[qdB_b])
            op("act", lambda a: a.activation(out=kdec[:, 0:4], in_=lg[:, 0:4], func=AF.Exp, scale=ctab[:, CT_C127:CT_C127 + 1]),
               reads=[ctab_b, lg_b], writes=[kdec_b])
            op("act", lambda a: a.activation(out=kdec[:, 4:8], in_=lg[:, 4:8], func=AF.Exp, scale=ctab[:, CT_PIDX:CT_PIDX + 1]),
               reads=[ctab_b, lg_b], writes=[kdec_b])

            S32 = [mk.sb("S32", [128, 8, 512], F32) for _ in range(2)]
            Sbf, Sbf_b = mk.sb("Sbf", [128, 8, 512], BF16)
            Kt, Kt_b = mk.sb("Kt", [128, 1024], BF16)
            sps = [mk.ps("sps", [128, 512], F32) for _ in range(4)]
            spn = [0]

            sin_d, sin_d_b = mk.dram("sin_d", [256, 4096], F32)
            if NO > 0:
                mk.push()
                SinF, SinF_b = mk.sb("SinF", [128, 8, 512], F32)
                SinB, SinB_b = mk.sb("SinB", [128, 8, 512], F32)
                op("pool", lambda g: g.memset(SinF[:], 0.0), writes=[SinF_b])
                op("pool", lambda g: g.memset(SinB[:], 0.0), writes=[SinB_b])
                GSZ = 4 if NO % 4 == 0 else (2 if NO % 2 == 0 else 1)
                og = [mk.sb("og", [128, GSZ, 4096], BF16) for _ in range(2)]
                ngr = NO // GSZ
                def load_grp(gi):
                    g_, g_b = og[gi % 2]
                    dma(g_[:], osc[gi * GSZ * 128:(gi + 1) * GSZ * 128, :].rearrange("(i p) c -> p i c", p=128), g_b,
                        reads=[osc_b], writes=[g_b])
                load_grp(0)
                for gi in range(ngr):
                    if gi + 1 < ngr:
                        load_grp(gi + 1)
                    g_, g_b = og[gi % 2]
                    for d in range(2):
                        Sacc, Sacc_b = (SinF, SinF_b) if d == 0 else (SinB, SinB_b)
                        for h in range(4):
                            for dc in range(2):
                                ps, ps_b = sps[spn[0] % 4]
                                spn[0] += 1
                                for i in range(GSZ):
                                    kc0 = d * 1024 + h * 256 + dc * 128
                                    op("pe", lambda p: p.matmul(ps[:], lhsT=g_[:, i, kc0:kc0 + 128],
                                                                rhs=g_[:, i, 2048 + h * 512:2048 + (h + 1) * 512],
                                                                start=(i == 0), stop=(i == GSZ - 1)),
                                       reads=[g_b], writes=[ps_b], inc=(i == GSZ - 1))
                                op("dve", lambda v: v.tensor_tensor(out=Sacc[:, h * 2 + dc, :], in0=Sacc[:, h * 2 + dc, :],
                                                                    in1=ps[:], op=ALU.add),
                                   reads=[ps_b, Sacc_b], writes=[Sacc_b])
                dma(sin_d[0:128, :], SinF[:].rearrange("p a b -> p (a b)"), SinF_b, reads=[SinF_b], writes=[sin_d_b])
                dma(sin_d[128:256, :], SinB[:].rearrange("p a b -> p (a b)"), SinB_b, reads=[SinB_b], writes=[sin_d_b])
                mk.pop()

            def state_update(d, K_ap, K_b, V_ap, V_b):
                S, S_b = S32[d]
                for h in range(4):
                    eng = "dve" if h % 2 == 0 else "pool"
                    op(eng, lambda v: v.tensor_scalar(out=Kt[:, h * 256:(h + 1) * 256], in0=K_ap[:, h * 256:(h + 1) * 256],
                                                      scalar1=kdec[:, d * 4 + h:d * 4 + h + 1], scalar2=None, op0=ALU.mult),
                       reads=[K_b, kdec_b], writes=[Kt_b])
                for h in range(4):
                    for dc in range(2):
                        ps, ps_b = sps[spn[0] % 4]
                        spn[0] += 1
                        op("pe", lambda p: p.matmul(ps[:], lhsT=Kt[:, h * 256 + dc * 128:h * 256 + (dc + 1) * 128],
                                                    rhs=V_ap[:, h * 512:(h + 1) * 512], start=True, stop=True),
                           reads=[Kt_b, V_b], writes=[ps_b])
                        op("dve", lambda v: v.scalar_tensor_tensor(out=S[:, h * 2 + dc, :], in0=S[:, h * 2 + dc, :],
                                                                  scalar=cdec[:, d * 4 + h:d * 4 + h + 1], in1=ps[:],
                                                                  op0=ALU.mult, op1=ALU.add),
                           reads=[ps_b, S_b, cdec_b], writes=[S_b])

            seqs = [(0, TS), (TS, NT)]

            check("O2")
            mk.push()
            kv = [mk.sb("kv", [128, 3072], BF16) for _ in range(2)]
            sst = [mk.sb("sst", [128, 8, 512], BF16) for _ in range(2)]
            order = []
            for si, (t0, t1) in enumerate(seqs):
                order += [(si, t) for t in range(t1 - 1, t0 - 1, -1)]
            def load_kv(i):
                _, t = order[i]
                b_, b_b = kv[i % 2]
                dma(b_[:], r1[t * 128:(t + 1) * 128, 2048:5120], b_b, reads=[r1_b], writes=[b_b])
            if order:
                load_kv(0)
            for i, (si, t) in enumerate(order):
                if i + 1 < len(order):
                    load_kv(i + 1)
                S, S_b = S32[1]
                if t == seqs[si][1] - 1:
                    if si == 0 or NO == 0:
                        op("pool", lambda g: g.memset(S[:], 0.0), writes=[S_b])
                    else:
                        dma(S[:].rearrange("p a b -> p (a b)"), sin_d[128:256, :], S_b, reads=[sin_d_b], writes=[S_b])
                st_, st_b = sst[i % 2]
                op("act", lambda a: a.copy(out=st_[:], in_=S[:]), reads=[S_b], writes=[st_b])
                dma(sbst[t * 128:(t + 1) * 128, :], st_[:].rearrange("p a b -> p (a b)"), st_b, reads=[st_b], writes=[sbst_b])
                b_, b_b = kv[i % 2]
                state_update(1, b_[:, 0:1024], b_b, b_[:, 1024:3072], b_b)
            mk.pop()

            check("R2")
            mk.push()
            Wo, Wo_b = mk.sb("Wo", [128, 16, 1024], BF16)
            mk.push()
            stg = [mk.sb("stg", [128, 1024], F32) for _ in range(2)]
            load_weight_bf16(Wo, Wo_b, ret_w_out, 2048, 1024, stg, 1024)
            mk.pop()
            mods = make_mod(0, 0, True, want_norm=False)
            gng, gng_b = mk.sb("gng", [128, 2048], F32)
            load_bc(gng[:], gng_b, ret_gn_g[0:1, :])
            rt = [mk.sb("rt", [128, 7168], BF16) for _ in range(2)]
            sbt = [mk.sb("sbt", [128, 8, 512], BF16) for _ in range(2)]
            xt = [mk.sb("xt", [128, 1024], F32) for _ in range(2)]
            QfT, QfT_b = mk.sb("QfT", [128, 8, 128], BF16)
            QbT, QbT_b = mk.sb("QbT", [128, 8, 128], BF16)
            sTm, sTm_b = mk.sb("sTm", [128, 512], BF16)
            on, on_b = mk.sb("on", [128, 2048], F32)
            ogb, ogb_b = mk.sb("ogb", [128, 2048], BF16)
            ogT, ogT_b = mk.sb("ogT", [128, 16, 128], BF16)
            st6, st6_b = mk.sb("st6", [128, 4, 6], F32)
            mv, mv_b = mk.sb("mv", [128, 4, 2], F32)
            rstd, rstd_b = mk.sb("rstd", [128, 4], F32)
            xo_t = [mk.sb("xo_t", [128, 1024], F32) for _ in range(1)]
            psS, psS_b = mk.ps("psS", [128, 512], F32)
            psO = [mk.ps("psO", [128, 512], F32) for _ in range(2)]
            psT, psT_b = mk.ps("psT", [128, 8, 128], BF16)

            order = [(si, t) for si, (t0, t1) in enumerate(seqs) for t in range(t0, t1)]
            def load_r3(i):
                _, t = order[i]
                a_, a_b = rt[i % 2]
                b_, b_b = sbt[i % 2]
                c_, c_b = xt[i % 2]
                dma(a_[:], r1[t * 128:(t + 1) * 128, :], a_b, reads=[r1_b], writes=[a_b])
                dma(b_[:].rearrange("p a b -> p (a b)"), sbst[t * 128:(t + 1) * 128, :], b_b, reads=[sbst_b], writes=[b_b])
                dma(c_[:], xs[t * 128:(t + 1) * 128, :], c_b, reads=[xsb], writes=[c_b])
            if order:
                load_r3(0)
            for i, (si, t) in enumerate(order):
                if i + 1 < len(order):
                    load_r3(i + 1)
                S, S_b = S32[0]
                if t == seqs[si][0]:
                    if si == 0 or NO == 0:
                        op("pool", lambda g: g.memset(S[:], 0.0), writes=[S_b])
                    else:
                        dma(S[:].rearrange("p a b -> p (a b)"), sin_d[0:128, :], S_b, reads=[sin_d_b], writes=[S_b])
                a_, a_b = rt[i % 2]
                sb_, sb_b = sbt[i % 2]
                x_t, x_b = xt[i % 2]
                _, _, _, _, gt, gt_b = mods[si]
                QT = a_[:, 0:1024].rearrange("p (k i) -> p k i", k=8)
                KT = a_[:, 1024:2048].rearrange("p (k i) -> p k i", k=8)
                op("act", lambda a: a.copy(out=Sbf[:], in_=S[:]), reads=[S_b], writes=[Sbf_b])
                for h in range(4):
                    for dc in range(2):
                        op("pe", lambda p: p.matmul(psS[:, h * 128:(h + 1) * 128], lhsT=KT[:, h * 2 + dc, :],
                                                    rhs=QT[:, h * 2 + dc, :], start=(dc == 0), stop=(dc == 1)),
                           reads=[a_b], writes=[psS_b], inc=(h == 3 and dc == 1))
                op("dve", lambda v: v.tensor_tensor(out=sTm[:], in0=psS[:], in1=DT[:], op=ALU.mult),
                   reads=[psS_b, DT_b], writes=[sTm_b])
                op("pool", lambda g: g.tensor_tensor(out=QfT[:], in0=QT, in1=qdF[:], op=ALU.mult),
                   reads=[a_b, qdF_b], writes=[QfT_b])
                op("pool", lambda g: g.tensor_tensor(out=QbT[:], in0=QT, in1=qdB[:], op=ALU.mult),
                   reads=[a_b, qdB_b], writes=[QbT_b])
                for h in range(4):
                    po, po_b = psO[h % 2]
                    op("pe", lambda p: p.matmul(po[:], lhsT=sTm[:, h * 128:(h + 1) * 128],
                                                rhs=a_[:, 3072 + h * 512:3072 + (h + 1) * 512], start=True, stop=False),
                       reads=[sTm_b, a_b], writes=[po_b], inc=False)
                    for dc in range(2):
                        op("pe", lambda p: p.matmul(po[:], lhsT=QfT[:, h * 2 + dc, :], rhs=Sbf[:, h * 2 + dc, :],
                                                    start=False, stop=False),
                           reads=[QfT_b, Sbf_b], writes=[po_b], inc=False)
                    for dc in range(2):
                        op("pe", lambda p: p.matmul(po[:], lhsT=QbT[:, h * 2 + dc, :], rhs=sb_[:, h * 2 + dc, :],
                                                    start=False, stop=(dc == 1)),
                           reads=[QbT_b, sb_b], writes=[po_b], inc=(dc == 1))
                    op("dve", lambda v: v.bn_stats(out=st6[:, h, :], in_=po[:]), reads=[po_b], writes=[st6_b])
                    op("dve", lambda v: v.bn_aggr(out=mv[:, h, :], in_=st6[:, h, :]), reads=[st6_b], writes=[mv_b])
                    op("dve", lambda v: v.tensor_scalar(out=rstd[:, h:h + 1], in0=mv[:, h, 1:2], scalar1=EPS, scalar2=None,
                                                        op0=ALU.add), reads=[mv_b], writes=[rstd_b])
                    op("act", lambda a: a.sqrt(out=rstd[:, h:h + 1], in_=rstd[:, h:h + 1]), reads=[rstd_b], writes=[rstd_b])
                    op("dve", lambda v: v.reciprocal(out=rstd[:, h:h + 1], in_=rstd[:, h:h + 1]), reads=[rstd_b], writes=[rstd_b])
                    op("dve", lambda v: v.tensor_scalar(out=on[:, h * 512:(h + 1) * 512], in0=po[:], scalar1=mv[:, h, 0:1],
                                                        scalar2=rstd[:, h:h + 1], op0=ALU.subtract, op1=ALU.mult),
                       reads=[po_b, mv_b, rstd_b], writes=[on_b])
                op("pool", lambda g: g.tensor_tensor(out=on[:], in0=on[:], in1=gng[:], op=ALU.mult),
                   reads=[on_b, gng_b], writes=[on_b])
                op("pool", lambda g: g.tensor_tensor(out=ogb[:], in0=on[:], in1=a_[:, 5120:7168], op=ALU.mult),
                   reads=[on_b, a_b], writes=[ogb_b])
                for half in range(2):
                    for k in range(8):
                        kk = half * 8 + k
                        op("pe", lambda p: p.transpose(out=psT[:, k, :], in_=ogb[:, kk * 128:(kk + 1) * 128], identity=ident[:]),
                           reads=[ogb_b, ident_b], writes=[psT_b], inc=(k == 7))
                    op("act", lambda a: a.copy(out=ogT[:, half * 8:(half + 1) * 8, :], in_=psT[:]),
                       reads=[psT_b], writes=[ogT_b])
                xo_, xo_b = xo_t[0]
                for cb in range(2):
                    po, po_b = psO[cb]
                    for k in range(16):
                        op("pe", lambda p: p.matmul(po[:], lhsT=ogT[:, k, :], rhs=Wo[:, k, cb * 512:(cb + 1) * 512],
                                                    start=(k == 0), stop=(k == 15)),
                           reads=[ogT_b, Wo_b], writes=[po_b], inc=(k == 15))
                    op("dve", lambda v: v.tensor_tensor(out=xo_[:, cb * 512:(cb + 1) * 512], in0=po[:],
                                                        in1=gt[:, cb * 512:(cb + 1) * 512], op=ALU.mult),
                       reads=[po_b, gt_b], writes=[xo_b])
                op("pool", lambda g: g.tensor_tensor(out=x_t[:], in0=xo_[:], in1=x_t[:], op=ALU.add),
                   reads=[xo_b, x_b], writes=[x_b])
                dma(xa[t * 128:(t + 1) * 128, :], x_t[:], x_b, reads=[x_b], writes=[xa_b])
                state_update(0, a_[:, 2048:3072], a_b, a_[:, 3072:5120], a_b)
            mk.pop()
            mk.pop()
            check("R3")

            def moe_layer(l, xin, xin_b, xout, xout_b):
                ST = cfg.ST
                mk.push()
                Gall, Gall_b = mk.sb("Gall", [128, NT, NE], F32)
                mk.push()
                mods = make_mod(l, 1, False)
                nb = NormBufs()
                xt = [mk.sb("xt", [128, 1024], F32) for _ in range(2)]
                wr32, wr32_b = mk.sb("wr32", [128, 8, NE], F32)
                wr, wr_b = mk.sb("wr", [128, 8, NE], BF16)
                brb, brb_b = mk.sb("brb", [128, NE], F32)
                lgt, lgt_b = mk.sb("lgt", [128, NE], F32)
                ex, ex_b = mk.sb("ex", [128, NE], F32)
                msk, msk_b = mk.sb("msk", [128, NE], F32)
                top, top_b = mk.sb("top", [128, 8], F32)
                nm, nm_b = mk.sb("nm", [128, 1], F32)
                sm, sm_b = mk.sb("sm", [128, 1], F32)
                hTs = [mk.sb("hTs", [128, 8, 128], BF16) for _ in range(2)]
                lps, lps_b = mk.ps("lps", [128, NE], F32)
                dma(wr32[:], moe_w_r[l * 1024:(l + 1) * 1024, :].rearrange("(k p) e -> p k e", p=128), wr32_b,
                    reads=[wdr], writes=[wr32_b])
                op("pool", lambda g: g.tensor_copy(out=wr[:], in_=wr32[:]), reads=[wr32_b], writes=[wr_b])
                load_bc(brb[:], brb_b, moe_b_r[l:l + 1, :])
                dma(xt[0][0][:], xin[0:128, :], xt[0][1], reads=[xin_b], writes=[xt[0][1]])
                for t in range(NT):
                    par = t % 2
                    if t + 1 < NT:
                        dma(xt[1 - par][0][:], xin[(t + 1) * 128:(t + 2) * 128, :], xt[1 - par][1], reads=[xin_b],
                            writes=[xt[1 - par][1]])
                    gm, gm_b, sh, sh_b, _, _ = mods[seq_of(t)]
                    x_t, x_b = xt[par]
                    hT, hT_b, _, _ = norm_mod_T(nb, par, x_t[:], x_b, gm, gm_b, sh, sh_b, out_hT=hTs[par])
                    dma(h2d[t * 128:(t + 1) * 128, :], hT[:].rearrange("p k i -> p (k i)"), hT_b, reads=[hT_b], writes=[h2d_b])
                    for k in range(8):
                        op("pe", lambda p: p.matmul(lps[:], lhsT=hT[:, k, :], rhs=wr[:, k, :], start=(k == 0), stop=(k == 7)),
                           reads=[hT_b, wr_b], writes=[lps_b], inc=(k == 7))
                    op("dve", lambda v: v.tensor_tensor(out=lgt[:], in0=lps[:], in1=brb[:], op=ALU.add),
                       reads=[lps_b, brb_b], writes=[lgt_b])
                    op("dve", lambda v: v.max(out=top[:], in_=lgt[:]), reads=[lgt_b], writes=[top_b])
                    op("dve", lambda v: v.tensor_scalar(out=msk[:], in0=lgt[:], scalar1=top[:, 3:4], scalar2=None, op0=ALU.is_ge),
                       reads=[lgt_b, top_b], writes=[msk_b])
                    op("dve", lambda v: v.tensor_scalar(out=nm[:], in0=top[:, 0:1], scalar1=-1.0, scalar2=None, op0=ALU.mult),
                       reads=[top_b], writes=[nm_b])
                    op("act", lambda a: a.activation(out=ex[:], in_=lgt[:], func=AF.Exp, bias=nm[:, 0:1]),
                       reads=[lgt_b, nm_b], writes=[ex_b])
                    op("dve", lambda v: v.tensor_tensor(out=ex[:], in0=ex[:], in1=msk[:], op=ALU.mult),
                       reads=[ex_b, msk_b], writes=[ex_b])
                    op("dve", lambda v: v.reduce_sum(out=sm[:], in_=ex[:], axis=mybir.AxisListType.X),
                       reads=[ex_b], writes=[sm_b])
                    op("dve", lambda v: v.reciprocal(out=sm[:], in_=sm[:]), reads=[sm_b], writes=[sm_b])
                    op("dve", lambda v: v.tensor_scalar(out=Gall[:, t, :], in0=ex[:], scalar1=sm[:, 0:1], scalar2=None,
                                                        op0=ALU.mult), reads=[ex_b, sm_b], writes=[Gall_b])
                mk.pop()

                mk.push()
                gts = make_mod(l, 1, True, want_norm=False)
                Wgu = [mk.sb("Wgu", [128, 8, 2 * DE], BF16) for _ in range(2)]
                Wdn = [mk.sb("Wdn", [128, KE, 1024], BF16) for _ in range(2)]
                bgu = [mk.sb("bgu", [1, 2 * DE], BF16) for _ in range(2)]
                bdn32, bdn32_b = mk.sb("bdn32", [NE, 1024], F32)
                bdn, bdn_b = mk.sb("bdn", [NE, 1024], BF16)
                stg = [mk.sb("stg", [128, 1024], F32) for _ in range(2)]
                hS, hS_b = mk.sb("hS", [128, ST, 1024], BF16)
                yacc, yacc_b = mk.sb("yacc", [128, ST, 1024], F32)
                gl, gl_b = mk.sb("gl", [128, DE], F32)
                sg, sg_b = mk.sb("sg", [128, DE], F32)
                ln, ln_b = mk.sb("ln", [128, DE], F32)
                actb = [mk.sb("actb", [128, DE], BF16) for _ in range(2)]
                actT = [mk.sb("actT", [128, KE, 128], BF16) for _ in range(2)]
                Gbf, Gbf_b = mk.sb("Gbf", [128, NE], BF16)
                GT, GT_b = mk.sb("GT", [NE, 128], BF16)
                xt1, xt1_b = mk.sb("xt1", [128, 1024], F32)
                hps = [mk.ps("hps", [128, 512], F32) for _ in range(4)]
                yps = [mk.ps("yps", [128, 512], F32) for _ in range(2)]
                tps, tps_b = mk.ps("tps", [128, KE, 128], BF16)
                gps, gps_b = mk.ps("gps", [128, 128], BF16)
                dma(bdn32[:], moe_b_dn[l * NE:(l + 1) * NE, :], bdn32_b, reads=[wdr], writes=[bdn32_b])
                op("pool", lambda g: g.tensor_copy(out=bdn[:], in_=bdn32[:]), reads=[bdn32_b], writes=[bdn_b])
                n_ = [0]

                def load_expert(e, slot):
                    wg, wg_b = Wgu[slot]
                    wd, wd_b = Wdn[slot]
                    base = (l * NE + e) * 1024
                    for kc in range(8):
                        for c0 in range(0, 2 * DE, 1024):
                            s_, s_b = stg[n_[0] % 2]
                            n_[0] += 1
                            dma(s_[:], moe_w_gu[base + kc * 128: base + (kc + 1) * 128, c0:c0 + 1024], s_b,
                                reads=[wdr], writes=[s_b])
                            op("pool", lambda g: g.tensor_copy(out=wg[:, kc, c0:c0 + 1024], in_=s_[:]),
                               reads=[s_b], writes=[wg_b])
                            yield
                    based = (l * NE + e) * DE
                    for kc in range(KE):
                        s_, s_b = stg[n_[0] % 2]
                        n_[0] += 1
                        dma(s_[:], moe_w_dn[based + kc * 128: based + (kc + 1) * 128, :], s_b, reads=[wdr], writes=[s_b])
                        op("pool", lambda g: g.tensor_copy(out=wd[:, kc, :], in_=s_[:]), reads=[s_b], writes=[wd_b])
                        yield
                    b_, b_b = bgu[slot]
                    for c0 in range(0, 2 * DE, 1024):
                        s_, s_b = stg[n_[0] % 2]
                        n_[0] += 1
                        dma(s_[0:1, :], moe_b_gu[l * NE + e:l * NE + e + 1, c0:c0 + 1024], s_b, reads=[wdr], writes=[s_b])
                        op("pool", lambda g: g.tensor_copy(out=b_[0:1, c0:c0 + 1024], in_=s_[0:1, :]), reads=[s_b], writes=[b_b])
                        yield

                n_pieces = 8 * ((2 * DE) // 1024) + KE + (2 * DE) // 1024
                per_tile = -(-n_pieces // ST)

                def advance(gen, n):
                    if gen is None:
                        return
                    for _ in range(n):
                        try:
                            next(gen)
                        except StopIteration:
                            return

                nsup = NT // ST
                cnt = 0
                for su in range(nsup):
                    t0 = su * ST
                    dma(hS[:], h2d[t0 * 128:(t0 + ST) * 128, :].rearrange("(i p) c -> p i c", p=128), hS_b,
                        reads=[h2d_b], writes=[hS_b])
                    for i in range(ST):
                        t = t0 + i
                        op("pool", lambda g: g.tensor_copy(out=Gbf[:], in_=Gall[:, t, :]), reads=[Gall_b], writes=[Gbf_b])
                        op("pe", lambda p: p.transpose(out=gps[0:NE, :], in_=Gbf[:], identity=ident[:]),
                           reads=[Gbf_b, ident_b], writes=[gps_b])
                        op("act", lambda a: a.copy(out=GT[:], in_=gps[0:NE, :]), reads=[gps_b], writes=[GT_b])
                        for cb in range(2):
                            yp, yp_b = yps[cb]
                            op("pe", lambda p: p.matmul(yp[:], lhsT=GT[:], rhs=bdn[:, cb * 512:(cb + 1) * 512], start=True, stop=True),
                               reads=[GT_b, bdn_b], writes=[yp_b])
                            op("act", lambda a: a.copy(out=yacc[:, i, cb * 512:(cb + 1) * 512], in_=yp[:]),
                               reads=[yp_b], writes=[yacc_b])
                    if su == 0:
                        advance(load_expert(0, 0), 10 ** 6)
                    for e in range(NE):
                        slot = cnt % 2
                        cnt += 1
                        gen = None
                        if e + 1 < NE:
                            gen = load_expert(e + 1, cnt % 2)
                        elif su + 1 < nsup:
                            gen = load_expert(0, cnt % 2)
                        wg, wg_b = Wgu[slot]
                        wd, wd_b = Wdn[slot]
                        bg, bg_b = bgu[slot]
                        for i in range(ST):
                            advance(gen, per_tile)
                            t = t0 + i
                            hT = hS[:, i, :].rearrange("p (k j) -> p k j", k=8)
                            ab, ab_b = actb[i % 2]
                            aT, aT_b = actT[i % 2]
                            for half in range(2):
                                for q in range(GH):
                                    cb = half * GH + q
                                    hp, hp_b = hps[cb % 4]
                                    op("pe", lambda p: p.matmul(hp[:], lhsT=ones1[0:1, :], rhs=bg[0:1, cb * 512:(cb + 1) * 512],
                                                                start=True, stop=False),
                                       reads=[ones1_b, bg_b], writes=[hp_b], inc=False)
                                    for k in range(8):
                                        op("pe", lambda p: p.matmul(hp[:], lhsT=hT[:, k, :], rhs=wg[:, k, cb * 512:(cb + 1) * 512],
                                                                    start=False, stop=(k == 7)),
                                           reads=[hS_b, wg_b], writes=[hp_b], inc=(k == 7))
                                    if half == 0:
                                        op("dve", lambda v: v.tensor_scalar(out=gl[:, q * 512:(q + 1) * 512], in0=hp[:], scalar1=7.0,
                                                                            scalar2=None, op0=ALU.min),
                                           reads=[hp_b], writes=[gl_b])
                                    else:
                                        op("dve", lambda v: v.tensor_scalar(out=ln[:, q * 512:(q + 1) * 512], in0=hp[:], scalar1=7.0,
                                                                            scalar2=-7.0, op0=ALU.min, op1=ALU.max),
                                           reads=[hp_b], writes=[ln_b])
                            op("act", lambda a: a.activation(out=sg[:], in_=gl[:], func=AF.Sigmoid, scale=1.702),
                               reads=[gl_b], writes=[sg_b])
                            op("pool", lambda g: g.tensor_scalar(out=ln[:], in0=ln[:], scalar1=1.0, scalar2=None, op0=ALU.add),
                               reads=[ln_b], writes=[ln_b])
                            op("pool", lambda g: g.tensor_tensor(out=ln[:], in0=ln[:], in1=gl[:], op=ALU.mult),
                               reads=[ln_b, gl_b], writes=[ln_b])
                            op("dve", lambda v: v.scalar_tensor_tensor(out=ab[:], in0=ln[:], scalar=Gall[:, t, e:e + 1], in1=sg[:],
                                                                      op0=ALU.mult, op1=ALU.mult),
                               reads=[ln_b, sg_b, Gall_b], writes=[ab_b])
                            for k in range(KE):
                                op("pe", lambda p: p.transpose(out=tps[:, k, :], in_=ab[:, k * 128:(k + 1) * 128], identity=ident[:]),
                                   reads=[ab_b, ident_b], writes=[tps_b], inc=(k == KE - 1))
                            op("act", lambda a: a.copy(out=aT[:], in_=tps[:]), reads=[tps_b], writes=[aT_b])
                            for cb in range(2):
                                yp, yp_b = yps[cb]
                                for k in range(KE):
                                    op("pe", lambda p: p.matmul(yp[:], lhsT=aT[:, k, :], rhs=wd[:, k, cb * 512:(cb + 1) * 512],
                                                                start=(k == 0), stop=(k == KE - 1)),
                                       reads=[aT_b, wd_b], writes=[yp_b], inc=(k == KE - 1))
                                op("dve", lambda v: v.tensor_tensor(out=yacc[:, i, cb * 512:(cb + 1) * 512],
                                                                    in0=yacc[:, i, cb * 512:(cb + 1) * 512], in1=yp[:], op=ALU.add),
                                   reads=[yp_b, yacc_b], writes=[yacc_b])
                        advance(gen, 10 ** 6)
                    for i in range(ST):
                        t = t0 + i
                        _, _, _, _, gt, gt_b = gts[seq_of(t)]
                        dma(xt1[:], xin[t * 128:(t + 1) * 128, :], xt1_b, reads=[xin_b], writes=[xt1_b])
                        op("pool", lambda g: g.tensor_tensor(out=yacc[:, i, :], in0=yacc[:, i, :], in1=gt[:], op=ALU.mult),
                           reads=[yacc_b, gt_b], writes=[yacc_b])
                        op("pool", lambda g: g.tensor_tensor(out=xt1[:], in0=xt1[:], in1=yacc[:, i, :], op=ALU.add),
                           reads=[yacc_b, xt1_b], writes=[xt1_b])
                        dma(xout[t * 128:(t + 1) * 128, :], xt1[:], xt1_b, reads=[xt1_b], writes=[xout_b])
                mk.pop()
                mk.pop()

            moe_layer(0, xa, xa_b, xb, xb_b)
            check("E0")

            mk.push()
            Wg, Wg_b = mk.sb("Wg", [128, 8, 4096], BF16)
            Wgo, Wgo_b = mk.sb("Wgo", [128, 16, 1024], BF16)
            wsT, wsT_b = mk.sb("wsT", [128, 4, 128], BF16)
            bb16, bb16_b = mk.sb("bb16", [1, 5120], BF16)
            mk.push()
            stg = [mk.sb("stg", [128, 2048], F32) for _ in range(2)]
            load_weight_bf16(Wg, Wg_b, gm_w_in, 1024, 4096, stg, 2048)
            load_weight_bf16(Wgo, Wgo_b, gm_w_out, 2048, 1024, stg, 1024)
            s_, s_b = stg[0]
            dma(s_[:, 0:512].rearrange("p (g i) -> p g i", g=4), gm_w_sT[:, :].rearrange("(g p) i -> p g i", p=128), s_b,
                reads=[wdr], writes=[s_b])
            op("pool", lambda g: g.tensor_copy(out=wsT[:], in_=s_[:, 0:512].rearrange("p (g i) -> p g i", g=4)),
               reads=[s_b], writes=[wsT_b])
            b32, b32_b = mk.sb("b32", [1, 5120], F32)
            dma(b32[:, 0:4096], gm_b_in[0:1, :], b32_b, reads=[wdr], writes=[b32_b])
            dma(b32[:, 4096:5120], gm_b_out[0:1, :], b32_b, reads=[wdr], writes=[b32_b])
            op("pool", lambda g: g.tensor_copy(out=bb16[:], in_=b32[:]), reads=[b32_b], writes=[bb16_b])
            mk.barrier()
            mk.pop()
            mods = make_mod(1, 0, True)
            nb = NormBufs()
            vng, vng_b = mk.sb("vng", [128, 2048], F32)
            vnb, vnb_b = mk.sb("vnb", [128, 2048], F32)
            bsT, bsT_b = mk.sb("bsT", [128, 4], F32)
            load_bc(vng[:], vng_b, gm_vn_g[0:1, :])
            load_bc(vnb[:], vnb_b, gm_vn_b[0:1, :])
            dma(bsT[:], gm_b_sT[:, :], bsT_b, reads=[wdr], writes=[bsT_b])
            xt = [mk.sb("xt", [128, 1024], F32) for _ in range(2)]
            uu, uu_b = mk.sb("uu", [128, 2048], F32)
            vv, vv_b = mk.sb("vv", [128, 2048], F32)
            vn, vn_b = mk.sb("vn", [128, 2048], BF16)
            pp, pp_b = mk.sb("pp", [128, 2048], BF16)
            ppT, ppT_b = mk.sb("ppT", [128, 16, 128], BF16)
            st6, st6_b = mk.sb("st6", [128, 4, 6], F32)
            mv, mv_b = mk.sb("mv", [128, 2], F32)
            rstd, rstd_b = mk.sb("rstd", [128, 1], F32)
            xo_t = [mk.sb("xo_t", [128, 1024], F32) for _ in range(1)]
            zps = [mk.ps("zps", [128, 512], F32) for _ in range(4)]
            mps_ = [mk.ps("mps2", [128, 512], F32) for _ in range(2)]
            psT, psT_b = mk.ps("psT", [128, 8, 128], BF16)
            dma(xt[0][0][:], xb[0:128, :], xt[0][1], reads=[xb_b], writes=[xt[0][1]])
            for t in range(NT):
                par = t % 2
                if t + 1 < NT:
                    dma(xt[1 - par][0][:], xb[(t + 1) * 128:(t + 2) * 128, :], xt[1 - par][1], reads=[xb_b],
                        writes=[xt[1 - par][1]])
                gm, gm_b, sh, sh_b, gt, gt_b = mods[seq_of(t)]
                x_t, x_b = xt[par]
                hT, hT_b, _, _ = norm_mod_T(nb, par, x_t[:], x_b, gm, gm_b, sh, sh_b)
                for cb in range(8):
                    zp, zp_b = zps[cb % 4]
                    op("pe", lambda p: p.matmul(zp[:], lhsT=ones1[0:1, :], rhs=bb16[0:1, cb * 512:(cb + 1) * 512],
                                                start=True, stop=False), reads=[ones1_b, bb16_b], writes=[zp_b], inc=False)
                    for k in range(8):
                        op("pe", lambda p: p.matmul(zp[:], lhsT=hT[:, k, :], rhs=Wg[:, k, cb * 512:(cb + 1) * 512],
                                                    start=False, stop=(k == 7)),
                           reads=[hT_b, Wg_b], writes=[zp_b], inc=(k == 7))
                    if cb < 4:
                        op("act", lambda a: a.activation(out=uu[:, cb * 512:(cb + 1) * 512], in_=zp[:], func=AF.Gelu),
                           reads=[zp_b], writes=[uu_b])
                    else:
                        c = cb - 4
                        op("act", lambda a: a.activation(out=vv[:, c * 512:(c + 1) * 512], in_=zp[:], func=AF.Gelu),
                           reads=[zp_b], writes=[vv_b])
                        op("dve", lambda v: v.bn_stats(out=st6[:, c, :], in_=vv[:, c * 512:(c + 1) * 512]),
                           reads=[vv_b], writes=[st6_b])
                op("dve", lambda v: v.bn_aggr(out=mv[:], in_=st6[:]), reads=[st6_b], writes=[mv_b])
                op("dve", lambda v: v.tensor_scalar(out=rstd[:], in0=mv[:, 1:2], scalar1=EPS, scalar2=None, op0=ALU.add),
                   reads=[mv_b], writes=[rstd_b])
                op("act", lambda a: a.sqrt(out=rstd[:], in_=rstd[:]), reads=[rstd_b], writes=[rstd_b])
                op("dve", lambda v: v.reciprocal(out=rstd[:], in_=rstd[:]), reads=[rstd_b], writes=[rstd_b])
                op("dve", lambda v: v.tensor_scalar(out=vv[:], in0=vv[:], scalar1=mv[:, 0:1], scalar2=rstd[:, 0:1],
                                                    op0=ALU.subtract, op1=ALU.mult), reads=[vv_b, mv_b, rstd_b], writes=[vv_b])
                op("pool", lambda g: g.tensor_tensor(out=vv[:], in0=vv[:], in1=vng[:], op=ALU.mult),
                   reads=[vv_b, vng_b], writes=[vv_b])
                op("pool", lambda g: g.tensor_tensor(out=vn[:], in0=vv[:], in1=vnb[:], op=ALU.add),
                   reads=[vv_b, vnb_b], writes=[vn_b])
                for g_ in range(4):
                    mp, mp_b = mps_[g_ % 2]
                    op("pe", lambda p: p.matmul(mp[:], lhsT=wsT[:, g_, :], rhs=vn[:, g_ * 512:(g_ + 1) * 512], start=True, stop=True),
                       reads=[wsT_b, vn_b], writes=[mp_b])
                    op("dve", lambda v: v.scalar_tensor_tensor(out=pp[:, g_ * 512:(g_ + 1) * 512], in0=mp[:], scalar=bsT[:, g_:g_ + 1],
                                                              in1=uu[:, g_ * 512:(g_ + 1) * 512], op0=ALU.add, op1=ALU.mult),
                       reads=[mp_b, bsT_b, uu_b], writes=[pp_b])
                for half in range(2):
                    for k in range(8):
                        kk = half * 8 + k
                        op("pe", lambda p: p.transpose(out=psT[:, k, :], in_=pp[:, kk * 128:(kk + 1) * 128], identity=ident[:]),
                           reads=[pp_b, ident_b], writes=[psT_b], inc=(k == 7))
                    op("act", lambda a: a.copy(out=ppT[:, half * 8:(half + 1) * 8, :], in_=psT[:]), reads=[psT_b], writes=[ppT_b])
                xo_, xo_b = xo_t[0]
                for cb in range(2):
                    mp, mp_b = mps_[cb]
                    op("pe", lambda p: p.matmul(mp[:], lhsT=ones1[0:1, :], rhs=bb16[0:1, 4096 + cb * 512:4096 + (cb + 1) * 512],
                                                start=True, stop=False), reads=[ones1_b, bb16_b], writes=[mp_b], inc=False)
                    for k in range(16):
                        op("pe", lambda p: p.matmul(mp[:], lhsT=ppT[:, k, :], rhs=Wgo[:, k, cb * 512:(cb + 1) * 512],
                                                    start=False, stop=(k == 15)),
                           reads=[ppT_b, Wgo_b], writes=[mp_b], inc=(k == 15))
                    op("dve", lambda v: v.tensor_tensor(out=xo_[:, cb * 512:(cb + 1) * 512], in0=mp[:],
                                                        in1=gt[:, cb * 512:(cb + 1) * 512], op=ALU.mult),
                       reads=[mp_b, gt_b], writes=[xo_b])
                op("pool", lambda g: g.tensor_tensor(out=x_t[:], in0=xo_[:], in1=x_t[:], op=ALU.add),
                   reads=[xo_b, x_b], writes=[x_b])
                dma(xa[t * 128:(t + 1) * 128, :], x_t[:], x_b, reads=[x_b], writes=[xa_b])
            mk.pop()

            check("G")
            moe_layer(1, xa, xa_b, xb, xb_b)
            check("E1")

            mk.push()
            fg, fg_b = mk.sb("fg", [128, 1024], F32)
            load_bc(fg[:], fg_b, final_g[0:1, :])
            xt = [mk.sb("xt", [128, 1024], F32) for _ in range(2)]
            yt = [mk.sb("yt", [128, 1024], F32) for _ in range(2)]
            junk, junk_b = mk.sb("junk", [128, 1024], F32)
            ss2 = [mk.sb("ss", [128, 1], F32) for _ in range(2)]
            dma(xt[0][0][:], xb[0:128, :], xt[0][1], reads=[xb_b], writes=[xt[0][1]])
            for t in range(NT):
                par = t % 2
                if t + 1 < NT:
                    dma(xt[1 - par][0][:], xb[(t + 1) * 128:(t + 2) * 128, :], xt[1 - par][1], reads=[xb_b],
                        writes=[xt[1 - par][1]])
                x_t, x_b = xt[par]
                y_t, y_b = yt[par]
                ss, ss_b = ss2[par]
                op("act", lambda a: a.activation(out=junk[:], in_=x_t[:], func=AF.Square, accum_out=ss[:]),
                   reads=[x_b], writes=[junk_b, ss_b])
                op("dve", lambda v: v.tensor_scalar(out=ss[:], in0=ss[:], scalar1=1.0 / D, scalar2=EPS,
                                                    op0=ALU.mult, op1=ALU.add), reads=[ss_b], writes=[ss_b])
                op("act", lambda a: a.sqrt(out=ss[:], in_=ss[:]), reads=[ss_b], writes=[ss_b])
                op("dve", lambda v: v.reciprocal(out=ss[:], in_=ss[:]), reads=[ss_b], writes=[ss_b])
                op("dve", lambda v: v.scalar_tensor_tensor(out=y_t[:], in0=x_t[:], scalar=ss[:, 0:1], in1=fg[:],
                                                          op0=ALU.mult, op1=ALU.mult),
                   reads=[x_b, ss_b, fg_b], writes=[y_b])
                dma(y_out[t * 128:(t + 1) * 128, :], y_t[:], y_b, reads=[y_b], writes=[yb_])
            mk.pop()

        except StopBuild:
            while len(mk.stacks) > 1:
                mk.pop()
        mk.finish()
        build.stats = (mk.n_inst, mk.n_wait)
    return nc


def prepare(cfg, inp):
    NC, TS, TP, NE, DE = cfg.NC, cfg.TS, cfg.TP, cfg.NE, cfg.DE
    f = lambda a: np.ascontiguousarray(np.asarray(a, dtype=np.float32))
    xp = f(inp["x_prompt"])[0]
    xsm = f(inp["x_sample"])
    cp = f(inp["c_prompt"])[0]
    csm = f(inp["c_sample"])
    PT = TP * 128
    S = xp.shape[0]
    perm = np.concatenate([np.arange(0, 2 * DE, 2), np.arange(1, 2 * DE, 2)])
    shared = {
        "ctab": make_ctab(),
        "ada_w": f(inp["ada_w"]).reshape(2 * 1024, 6144),
        "ada_b": f(inp["ada_b"]),
        "norm1_g": f(inp["norm1_g"]), "norm2_g": f(inp["norm2_g"]),
        "ret_w_in": f(inp["ret_w_in"])[0],
        "ret_ld": f(inp["ret_log_decay"]).reshape(1, 8),
        "ret_gn_g": f(inp["ret_gn_g"]).reshape(1, 2048),
        "ret_w_out": f(inp["ret_w_out"])[0],
        "gm_w_in": f(inp["gm_w_in"])[0],
        "gm_b_in": f(inp["gm_b_in"]).reshape(1, 4096),
        "gm_vn_g": f(inp["gm_vn_g"]).reshape(1, 2048),
        "gm_vn_b": f(inp["gm_vn_b"]).reshape(1, 2048),
        "gm_w_sT": f(np.transpose(f(inp["gm_w_s"])[0], (0, 2, 1))).reshape(4 * 128, 128),
        "gm_b_sT": f(f(inp["gm_b_s"])[0].T),
        "gm_w_out": f(inp["gm_w_out"])[0],
        "gm_b_out": f(inp["gm_b_out"]).reshape(1, 1024),
        "moe_w_r": f(inp["moe_w_r"]).reshape(2 * 1024, NE),
        "moe_b_r": f(inp["moe_b_r"]),
        "moe_w_gu": f(f(inp["moe_w_gu"])[..., perm]).reshape(2 * NE * 1024, 2 * DE),
        "moe_b_gu": f(f(inp["moe_b_gu"])[..., perm]).reshape(2 * NE, 2 * DE),
        "moe_w_dn": f(inp["moe_w_dn"]).reshape(2 * NE * DE, 1024),
        "moe_b_dn": f(inp["moe_b_dn"]).reshape(2 * NE, 1024),
        "final_g": f(inp["final_g"]).reshape(1, 1024),
    }
    pos_s = np.arange(TS * 128)
    in_maps = []
    for c in range(NC):
        a, b = c * PT, (c + 1) * PT
        m = dict(shared)
        m["xs"] = np.concatenate([xsm[c], xp[a:b]], axis=0)
        if NC > 1:
            m["xo"] = np.concatenate([xp[:a], xp[b:]], axis=0)
            pos_o = np.concatenate([np.arange(0, a), np.arange(b, S)])
            meta = np.zeros((pos_o.shape[0], 4), np.float32)
            bef = pos_o < a
            meta[bef, 0] = (a - 1 - pos_o[bef])
            meta[bef, 1] = 1.0
            meta[~bef, 2] = (pos_o[~bef] - b)
            meta[~bef, 3] = 1.0
        else:
            m["xo"] = np.zeros((128, 1024), np.float32)
            pos_o = np.zeros(128)
            meta = np.zeros((128, 4), np.float32)
        m["oth_meta"] = meta
        m["rope_oth"] = rope_table(pos_o)
        m["rope_own"] = rope_table(np.concatenate([pos_s, np.arange(a, b)]))
        cc = np.stack([csm[c], cp], axis=-1)
        m["ccol"] = f(cc.reshape(8, 128, 2).transpose(1, 0, 2).reshape(128, 16))
        in_maps.append(m)
    return in_maps


def assemble(cfg, results):
    TS, TP = cfg.TS, cfg.TP
    ys = [np.asarray(r["y"], dtype=np.float32) for r in results]
    y_sample = np.stack([y[:TS * 128] for y in ys], axis=0)
    y_prompt = np.concatenate([y[TS * 128:] for y in ys], axis=0)[None]
    return y_prompt, y_sample


_CACHE = {}


def run(cfg, inputs):
    key = (cfg.NC, cfg.TS, cfg.TP, cfg.NE, cfg.DE)
    if key not in _CACHE:
        _CACHE[key] = build(cfg)
    nc = _CACHE[key]
    in_maps = prepare(cfg, inputs)
    res = run_bass_kernel_spmd(nc, in_maps, core_ids=list(range(cfg.NC)))
    return assemble(cfg, res.results)


def kernel(**inputs):
    cfg = Cfg()
    return run(cfg, inputs)
```

```python
import numpy as np
from contextlib import ExitStack
import concourse.bass as bass
import concourse.mybir as mybir
from concourse.bass_utils import run_bass_kernel_spmd

F32 = mybir.dt.float32
BF16 = mybir.dt.bfloat16
ALU = mybir.AluOpType
AF = mybir.ActivationFunctionType
EPS = 1e-6


class Buf:
    __slots__ = ("name", "w", "r", "sem", "cnt", "dram_tokens", "scope")

    def __init__(self, name, dram=False):
        self.name = name
        self.scope = 0
        self.w = None
        self.r = {}
        self.sem = None
        self.cnt = 0
        self.dram_tokens = {} if dram else None


class Eng:
    def __init__(self, name, inst, sem):
        self.name = name
        self.inst = inst
        self.sem = sem
        self.cnt = 0
        self.seen = {}
        self.pend_r = []
        self.pend_w = []


class MK:
    def __init__(self, nc, stack):
        self.nc = nc
        self.stacks = [stack]
        self.engs = {}
        for name, inst in (("pe", nc.tensor), ("act", nc.scalar), ("dve", nc.vector),
                           ("pool", nc.gpsimd), ("sp", nc.sync)):
            sem = stack.enter_context(nc.semaphore("s_" + name))
            self.engs[name] = Eng(name, inst, sem)
        self.dma_bufs = []
        self.scope_dma = [[]]
        self.sem_pool = []
        self.dbg = False
        self.rec = None
        self.window = 6
        self.n_inst = 0
        self.n_wait = 0
        self.uid = 0

    def push(self):
        st = ExitStack()
        st.__enter__()
        self.stacks.append(st)
        self.scope_dma.append([])

    def pop(self):
        self.barrier()
        st = self.stacks.pop()
        for b in self.scope_dma.pop():
            self.dma_bufs.remove(b)
            self.sem_pool.append((b.sem, b.cnt))
        st.__exit__(None, None, None)

    def sb(self, name, shape, dtype):
        self.uid += 1
        t = self.stacks[-1].enter_context(self.nc.sbuf_tensor(f"{name}_{self.uid}", list(shape), dtype))
        b = Buf(name)
        b.scope = len(self.stacks) - 1
        return t, b

    def ps(self, name, shape, dtype=F32):
        self.uid += 1
        t = self.stacks[-1].enter_context(self.nc.psum_tensor(f"{name}_{self.uid}", list(shape), dtype))
        return t, Buf(name)

    def dram(self, name, shape, dtype):
        t = self.nc.dram_tensor(name, list(shape), dtype, kind=("ExternalOutput" if self.dbg else "Internal"))
        return t.ap(), Buf(name, dram=True)

    def _dsem(self, b):
        if b.sem is None:
            self.uid += 1
            if self.sem_pool:
                b.sem, b.cnt = self.sem_pool.pop()
            else:
                b.sem = self.stacks[0].enter_context(self.nc.semaphore(f"d_{b.name}_{self.uid}"))
            self.dma_bufs.append(b)
            self.scope_dma[b.scope].append(b)
        return b.sem

    def _wait(self, e, tokens):
        best = {}
        for tok in tokens:
            if tok is None:
                continue
            sem, val, owner = tok
            if owner == e.name:
                if e.name == "pe" or val <= e.cnt - self.window:
                    continue
            k = id(sem)
            if k not in best or best[k][1] < val:
                best[k] = (sem, val)
        for k, (sem, val) in best.items():
            if e.seen.get(k, 0) >= val:
                continue
            e.inst.wait_ge(sem, val)
            e.seen[k] = val
            self.n_wait += 1
            if self.rec is not None:
                self.rec[e.name].append(("w", k, val))

    @staticmethod
    def _deps(reads, writes):
        toks = []
        for b in reads:
            toks.append(b.w)
            if b.dram_tokens:
                toks.extend(b.dram_tokens.values())
        for b in writes:
            toks.append(b.w)
            toks.extend(b.r.values())
            if b.dram_tokens:
                toks.extend(b.dram_tokens.values())
        return toks

    @staticmethod
    def _commit(tok, key, reads, writes):
        for b in writes:
            if b.dram_tokens is not None:
                b.dram_tokens[id(tok[0])] = tok
            else:
                b.w = tok
            b.r = {}
        for b in reads:
            b.r[key] = tok

    def op(self, eng, fn, reads=(), writes=(), inc=True):
        e = self.engs[eng]
        self._wait(e, self._deps(reads, writes))
        ins = fn(e.inst)
        self.n_inst += 1
        if not inc:
            e.pend_r.extend(reads)
            e.pend_w.extend(writes)
            return ins
        e.cnt += 1
        ins.then_inc(e.sem, 1)
        if self.rec is not None:
            self.rec[e.name].append(("i", id(e.sem), 1, self.n_inst))
        tok = (e.sem, e.cnt, e.name)
        self._commit(tok, e.name, list(reads) + e.pend_r, list(writes) + e.pend_w)
        e.pend_r = []
        e.pend_w = []
        return ins

    def dma(self, out, in_, sbuf_side, reads=(), writes=(), q="sp", fn=None):
        e = self.engs[q]
        self._wait(e, self._deps(reads, writes))
        sem = self._dsem(sbuf_side)
        ins = e.inst.dma_start(out=out, in_=in_) if fn is None else fn(e.inst)
        sbuf_side.cnt += 16
        ins.then_inc(sem, 16)
        if self.rec is not None:
            self.rec[e.name].append(("i", id(sem), 16, self.n_inst))
        tok = (sem, sbuf_side.cnt, None)
        self._commit(tok, ("dma", id(sem)), reads, writes)
        self.n_inst += 1
        return ins

    def barrier(self):
        for e in self.engs.values():
            assert not e.pend_r and not e.pend_w
        for e in self.engs.values():
            for o in self.engs.values():
                if o is e or o.cnt == 0:
                    continue
                if e.seen.get(id(o.sem), 0) < o.cnt:
                    e.inst.wait_ge(o.sem, o.cnt)
                    e.seen[id(o.sem)] = o.cnt
            for b in self.dma_bufs:
                if b.cnt and e.seen.get(id(b.sem), 0) < b.cnt:
                    e.inst.wait_ge(b.sem, b.cnt)
                    e.seen[id(b.sem)] = b.cnt

    def finish(self):
        self.barrier()


class Cfg:
    def __init__(self, NC=8, TS=64, TP=16, NE=32, DE=1024):
        self.NC, self.TS, self.TP, self.NE, self.DE = NC, TS, TP, NE, DE
        self.D = 1024
        self.NT = TS + TP
        self.NO = TP * (NC - 1)
        self.ST = 4 if self.NT % 4 == 0 else (2 if self.NT % 2 == 0 else 1)


CT_EF, CT_MF, CT_EB, CT_MB, CT_IP1, CT_CMI, CT_C127, CT_PIDX, CT_IO, CT_U, CT_W = 0, 128, 256, 384, 512, 640, 768, 769, 770, 898, 1026


def make_ctab():
    i = np.arange(128, dtype=np.float32)
    j = i[:, None]
    ii = i[None, :]
    t = np.zeros((128, CT_W), np.float32)
    t[:, CT_EF:CT_EF + 128] = np.maximum(ii - j, 0)
    t[:, CT_MF:CT_MF + 128] = (ii >= j)
    t[:, CT_EB:CT_EB + 128] = np.maximum(j - ii, 0)
    t[:, CT_MB:CT_MB + 128] = (j > ii)
    t[:, CT_IP1:CT_IP1 + 128] = ii + 1
    t[:, CT_CMI:CT_CMI + 128] = 128 - ii
    t[:, CT_C127] = 127 - i
    t[:, CT_PIDX] = i
    t[:, CT_IO:CT_IO + 128] = ii
    t[:, CT_U:CT_U + 128] = (j < ii)
    return t


def rope_table(pos):
    half = 128
    inv = (np.float32(10000.0) ** (-np.arange(half, dtype=np.float32) / np.float32(half))).astype(np.float32)
    ang = (pos.astype(np.float32)[:, None] * inv[None, :]).astype(np.float32)
    return np.concatenate([np.cos(ang), np.sin(ang)], axis=1).astype(np.float32)


class StopBuild(Exception):
    pass


def build(cfg, dbg=False, stop_after=None):
    nc = bass.Bass("TRN2", target_bir_lowering=False)

    def check(name):
        if stop_after == name:
            raise StopBuild()
    D, NT, NO, NE, DE, TS, TP = cfg.D, cfg.NT, cfg.NO, cfg.NE, cfg.DE, cfg.TS, cfg.TP

    def IN(name, shape):
        return nc.dram_tensor(name, list(shape), F32, kind="ExternalInput").ap()

    xs = IN("xs", [NT * 128, D])
    xo = IN("xo", [max(NO, 1) * 128, D])
    ccol = IN("ccol", [128, 16])
    rope_own = IN("rope_own", [NT * 128, 256])
    rope_oth = IN("rope_oth", [max(NO, 1) * 128, 256])
    oth_meta = IN("oth_meta", [max(NO, 1) * 128, 4])
    ctab_d = IN("ctab", [128, CT_W])
    ada_w = IN("ada_w", [2 * 1024, 6144])
    ada_b = IN("ada_b", [2, 6144])
    norm1_g = IN("norm1_g", [2, 1024])
    norm2_g = IN("norm2_g", [2, 1024])
    ret_w_in = IN("ret_w_in", [1024, 6144])
    ret_ld = IN("ret_ld", [1, 8])
    ret_gn_g = IN("ret_gn_g", [1, 2048])
    ret_w_out = IN("ret_w_out", [2048, 1024])
    gm_w_in = IN("gm_w_in", [1024, 4096])
    gm_b_in = IN("gm_b_in", [1, 4096])
    gm_vn_g = IN("gm_vn_g", [1, 2048])
    gm_vn_b = IN("gm_vn_b", [1, 2048])
    gm_w_sT = IN("gm_w_sT", [4 * 128, 128])
    gm_b_sT = IN("gm_b_sT", [128, 4])
    gm_w_out = IN("gm_w_out", [2048, 1024])
    gm_b_out = IN("gm_b_out", [1, 1024])
    moe_w_r = IN("moe_w_r", [2 * 1024, NE])
    moe_b_r = IN("moe_b_r", [2, NE])
    moe_w_gu = IN("moe_w_gu", [2 * NE * 128, 8 * 2 * DE])
    moe_b_gu = IN("moe_b_gu", [2 * NE, 2 * DE])
    moe_w_dn = IN("moe_w_dn", [2 * NE * 128, (DE // 128) * 1024])
    moe_b_dn = IN("moe_b_dn", [2 * NE, 1024])
    final_g = IN("final_g", [1, 1024])
    y_out = nc.dram_tensor("y", [NT * 128, D], F32, kind="ExternalOutput").ap()

    KE = DE // 128
    GB = (2 * DE) // 512
    GH = GB // 2

    with ExitStack() as root:
        mk = MK(nc, root)
        mk.dbg = dbg
        if getattr(build, "record", False):
            mk.rec = {n: [] for n in mk.engs}
            build.rec = mk.rec
        op, dma = mk.op, mk.dma
        try:
          if True:

            modv, modv_b = mk.dram("modv", [4, 6144], F32)
            r1, r1_b = mk.dram("r1", [NT * 128, 7168], BF16)
            osc, osc_b = mk.dram("osc", [max(NO, 1) * 128, 4096], BF16)
            sbst, sbst_b = mk.dram("sbst", [NT * 128, 4096], BF16)
            xa, xa_b = mk.dram("xa", [NT * 128, D], F32)
            xb, xb_b = mk.dram("xb", [NT * 128, D], F32)
            h2d, h2d_b = mk.dram("h2d", [NT * 128, D], BF16)
            xsb = Buf("xs_in", dram=True)
            xob = Buf("xo_in", dram=True)
            wdr = Buf("weights_in", dram=True)
            yb_ = Buf("y_out", dram=True)

            ident, ident_b = mk.sb("ident", [128, 128], BF16)
            identf, identf_b = mk.sb("identf", [128, 128], F32)
            ones1, ones1_b = mk.sb("ones1", [1, 128], BF16)
            ctab, ctab_b = mk.sb("ctab", [128, CT_W], F32)
            lg, lg_b = mk.sb("lg", [128, 8], F32)
            cdec, cdec_b = mk.sb("cdec", [128, 8], F32)

            dma(ctab[:], ctab_d[:, :], ctab_b, reads=[wdr], writes=[ctab_b])
            dma(lg[:], ret_ld[0:1, :].partition_broadcast(128), lg_b, reads=[wdr], writes=[lg_b])
            op("pool", lambda g: g.memset(identf[:], 0.0), writes=[identf_b])
            op("pool", lambda g: g.affine_select(out=identf[:], in_=identf[:], pattern=[[-1, 128]],
                                                 compare_op=ALU.not_equal, fill=1.0, base=0, channel_multiplier=1),
               reads=[identf_b], writes=[identf_b])
            op("pool", lambda g: g.tensor_copy(out=ident[:], in_=identf[:]), reads=[identf_b], writes=[ident_b])
            op("pool", lambda g: g.memset(ones1[:], 1.0), writes=[ones1_b])
            op("act", lambda a: a.activation(out=cdec[:], in_=lg[:], func=AF.Exp, scale=128.0),
               reads=[lg_b], writes=[cdec_b])

            def seq_of(t):
                return 0 if t < TS else 1

            def load_bc(dst, dst_b, src_row):
                dma(dst, src_row.partition_broadcast(128), dst_b, reads=[wdr, modv_b], writes=[dst_b])

            def make_mod(l, sub, want_gate, want_norm=True):
                ng_src = (norm1_g if sub == 0 else norm2_g)
                res = []
                if want_norm:
                    ng, ng_b = mk.sb("ng", [128, 1024], F32)
                    load_bc(ng[:], ng_b, ng_src[l:l + 1, :])
                for s in range(2):
                    row = l * 2 + s
                    gm = gm_b = sh = sh_b = None
                    if want_norm:
                        gm, gm_b = mk.sb("gm", [128, 1024], F32)
                        sh, sh_b = mk.sb("sh", [128, 1024], F32)
                        load_bc(sh[:], sh_b, modv[row:row + 1, (3 * sub) * 1024:(3 * sub + 1) * 1024])
                        load_bc(gm[:], gm_b, modv[row:row + 1, (3 * sub + 1) * 1024:(3 * sub + 2) * 1024])
                        op("dve", lambda v: v.scalar_tensor_tensor(out=gm[:], in0=gm[:], scalar=1.0, in1=ng[:],
                                                                  op0=ALU.add, op1=ALU.mult),
                           reads=[gm_b, ng_b], writes=[gm_b])
                    gt = gt_b = None
                    if want_gate:
                        gt, gt_b = mk.sb("gt", [128, 1024], F32)
                        load_bc(gt[:], gt_b, modv[row:row + 1, (3 * sub + 2) * 1024:(3 * sub + 3) * 1024])
                    res.append((gm, gm_b, sh, sh_b, gt, gt_b))
                return res

            class NormBufs:
                def __init__(self):
                    self.ss = [mk.sb("ss", [128, 1], F32) for _ in range(2)]
                    _tt = mk.sb("tt", [128, 1024], F32)
                    self.tt = [_tt, _tt]
                    self.hh = [mk.sb("hh", [128, 1024], BF16) for _ in range(2)]
                    self.hT = [mk.sb("hT", [128, 8, 128], BF16) for _ in range(2)]
                    self.hTp, self.hTp_b = mk.ps("hTp", [128, 8, 128], BF16)

            def norm_mod_T(nb, par, x_t, x_b, gm, gm_b, sh, sh_b, out_hT=None):
                ss, ss_b = nb.ss[par]
                tt, tt_b = nb.tt[par]
                hh, hh_b = nb.hh[par]
                hT, hT_b = nb.hT[par] if out_hT is None else out_hT
                op("act", lambda a: a.activation(out=tt[:], in_=x_t, func=AF.Square, accum_out=ss[:]),
                   reads=[x_b], writes=[tt_b, ss_b])
                op("dve", lambda v: v.tensor_scalar(out=ss[:], in0=ss[:], scalar1=1.0 / D, scalar2=EPS,
                                                    op0=ALU.mult, op1=ALU.add), reads=[ss_b], writes=[ss_b])
                op("act", lambda a: a.sqrt(out=ss[:], in_=ss[:]), reads=[ss_b], writes=[ss_b])
                op("dve", lambda v: v.reciprocal(out=ss[:], in_=ss[:]), reads=[ss_b], writes=[ss_b])
                op("dve", lambda v: v.scalar_tensor_tensor(out=tt[:], in0=x_t, scalar=ss[:, 0:1], in1=gm[:],
                                                          op0=ALU.mult, op1=ALU.mult),
                   reads=[x_b, ss_b, gm_b], writes=[tt_b])
                op("pool", lambda g: g.tensor_tensor(out=hh[:], in0=tt[:], in1=sh[:], op=ALU.add),
                   reads=[tt_b, sh_b], writes=[hh_b])
                for k in range(8):
                    op("pe", lambda p: p.transpose(out=nb.hTp[:, k, :], in_=hh[:, k * 128:(k + 1) * 128], identity=ident[:]),
                       reads=[hh_b, ident_b], writes=[nb.hTp_b], inc=(k == 7))
                op("act", lambda a: a.copy(out=hT[:], in_=nb.hTp[:]), reads=[nb.hTp_b], writes=[hT_b])
                return hT, hT_b, hh, hh_b

            def load_weight_bf16(dst, dst_b, src, rows, cols, stg_list, col_chunk):
                i = 0
                for kc in range(rows // 128):
                    for c0 in range(0, cols, col_chunk):
                        stg, stg_b = stg_list[i % len(stg_list)]
                        i += 1
                        cw = min(col_chunk, cols - c0)
                        dma(stg[:, 0:cw], src[kc * 128:(kc + 1) * 128, c0:c0 + cw], stg_b, reads=[wdr], writes=[stg_b])
                        op("pool", lambda g: g.tensor_copy(out=dst[:, kc, c0:c0 + cw], in_=stg[:, 0:cw]),
                           reads=[stg_b], writes=[dst_b])

            mk.push()
            cact, cact_b = mk.sb("cact", [128, 16], F32)
            adab, adab_b = mk.sb("adab", [2, 6144], F32)
            modsb, modsb_b = mk.sb("modsb", [2, 6144], F32)
            wst = [mk.sb("wst", [128, 3072], F32) for _ in range(2)]
            mps = [mk.ps("mps", [128, 512], F32) for _ in range(6)]
            dma(cact[:], ccol[:, :], cact_b, reads=[wdr], writes=[cact_b])
            op("act", lambda a: a.activation(out=cact[:], in_=cact[:], func=AF.Silu), reads=[cact_b], writes=[cact_b])
            for l in range(2):
                for s in range(2):
                    dma(adab[s:s + 1, :], ada_b[l:l + 1, :], adab_b, reads=[wdr], writes=[adab_b])
                for half in range(2):
                    for k in range(8):
                        w, w_b = wst[k % 2]
                        dma(w[:], ada_w[l * 1024 + k * 128: l * 1024 + (k + 1) * 128, half * 3072:(half + 1) * 3072],
                            w_b, reads=[wdr], writes=[w_b])
                        for b in range(6):
                            op("pe", lambda p: p.matmul(mps[b][0][0:2, :], lhsT=cact[:, 2 * k:2 * k + 2],
                                                        rhs=w[:, b * 512:(b + 1) * 512], start=(k == 0), stop=(k == 7)),
                               reads=[cact_b, w_b], writes=[mps[b][1]], inc=(b == 5))
                    for b in range(6):
                        c0 = half * 3072 + b * 512
                        op("dve", lambda v: v.tensor_tensor(out=modsb[:, c0:c0 + 512], in0=mps[b][0][0:2, :],
                                                            in1=adab[:, c0:c0 + 512], op=ALU.add),
                           reads=[mps[b][1], adab_b], writes=[modsb_b])
                dma(modv[2 * l:2 * l + 2, :], modsb[:], modsb_b, reads=[modsb_b], writes=[modv_b])
            mk.pop()
            check("M")

            mk.push()
            Win, Win_b = mk.sb("Win", [128, 8, 6144], BF16)
            mk.push()
            stg = [mk.sb("stg", [128, 3072], F32) for _ in range(2)]
            load_weight_bf16(Win, Win_b, ret_w_in, 1024, 6144, stg, 3072)
            mk.pop()
            mods = make_mod(0, 0, False)
            nb = NormBufs()
            xt = [mk.sb("xt", [128, 1024], F32) for _ in range(2)]
            cst = [mk.sb("cst", [128, 256], F32) for _ in range(2)]
            ra, ra_b = mk.sb("ra", [128, 256], F32)
            rb, rb_b = mk.sb("rb", [128, 256], F32)
            rc, rc_b = mk.sb("rc", [128, 256], F32)
            rd, rd_b = mk.sb("rd", [128, 256], F32)
            qr, qr_b = mk.sb("qr", [128, 1024], BF16)
            pj = [mk.ps("pj", [128, 512], F32) for _ in range(4)]
            qTp, qTp_b = mk.ps("qTp", [128, 8, 128], BF16)
            kTp, kTp_b = mk.ps("kTp", [128, 8, 128], BF16)
            pjn = [0]

            def proj_block(hT, hT_b, cb):
                ps, ps_b = pj[pjn[0] % 4]
                pjn[0] += 1
                for k in range(8):
                    op("pe", lambda p: p.matmul(ps[:], lhsT=hT[:, k, :], rhs=Win[:, k, cb * 512:(cb + 1) * 512],
                                                start=(k == 0), stop=(k == 7)),
                       reads=[hT_b, Win_b], writes=[ps_b], inc=(k == 7))
                return ps, ps_b

            def rope_block(ps, ps_b, cs, cs_b, dst, dst_b, c0):
                pv = ps[:].rearrange("p (h t d) -> p h t d", h=2, t=2)
                dv = dst[:, c0:c0 + 512].rearrange("p (h t d) -> p h t d", h=2, t=2)
                cosb = cs[:, 0:128].unsqueeze(1).to_broadcast([128, 2, 128])
                sinb = cs[:, 128:256].unsqueeze(1).to_broadcast([128, 2, 128])
                v3 = lambda t_: t_[:].rearrange("p (h d) -> p h d", h=2)
                op("dve", lambda v: v.tensor_tensor(out=v3(ra), in0=pv[:, :, 0, :], in1=cosb, op=ALU.mult),
                   reads=[ps_b, cs_b], writes=[ra_b])
                op("dve", lambda v: v.tensor_tensor(out=v3(rb), in0=pv[:, :, 1, :], in1=sinb, op=ALU.mult),
                   reads=[ps_b, cs_b], writes=[rb_b])
                op("dve", lambda v: v.tensor_tensor(out=v3(rc), in0=pv[:, :, 1, :], in1=cosb, op=ALU.mult),
                   reads=[ps_b, cs_b], writes=[rc_b])
                op("dve", lambda v: v.tensor_tensor(out=v3(rd), in0=pv[:, :, 0, :], in1=sinb, op=ALU.mult),
                   reads=[ps_b, cs_b], writes=[rd_b])
                op("pool", lambda g: g.tensor_tensor(out=dv[:, :, 0, :], in0=v3(ra), in1=v3(rb), op=ALU.subtract),
                   reads=[ra_b, rb_b], writes=[dst_b])
                op("pool", lambda g: g.tensor_tensor(out=dv[:, :, 1, :], in0=v3(rc), in1=v3(rd), op=ALU.add),
                   reads=[rc_b, rd_b], writes=[dst_b])

            mk.push()
            so = [mk.sb("so", [128, 7168], BF16) for _ in range(2)]
            dma(xt[0][0][:], xs[0:128, :], xt[0][1], reads=[xsb], writes=[xt[0][1]])
            dma(cst[0][0][:], rope_own[0:128, :], cst[0][1], reads=[wdr], writes=[cst[0][1]])
            for t in range(NT):
                par = t % 2
                if t + 1 < NT:
                    dma(xt[1 - par][0][:], xs[(t + 1) * 128:(t + 2) * 128, :], xt[1 - par][1], reads=[xsb], writes=[xt[1 - par][1]])
                    dma(cst[1 - par][0][:], rope_own[(t + 1) * 128:(t + 2) * 128, :], cst[1 - par][1], reads=[wdr],
                        writes=[cst[1 - par][1]])
                s = seq_of(t)
                gm, gm_b, sh, sh_b, _, _ = mods[s]
                x_t, x_b = xt[par]
                cs, cs_b = cst[par]
                o, o_b = so[par]
                hT, hT_b, _, _ = norm_mod_T(nb, par, x_t[:], x_b, gm, gm_b, sh, sh_b)
                for cb in range(2):
                    ps, ps_b = proj_block(hT, hT_b, cb)
                    rope_block(ps, ps_b, cs, cs_b, qr, qr_b, cb * 512)
                for k in range(8):
                    op("pe", lambda p: p.transpose(out=qTp[:, k, :], in_=qr[:, k * 128:(k + 1) * 128], identity=ident[:]),
                       reads=[qr_b, ident_b], writes=[qTp_b], inc=(k == 7))
                op("act", lambda a: a.copy(out=o[:, 0:1024].rearrange("p (k i) -> p k i", k=8), in_=qTp[:]),
                   reads=[qTp_b], writes=[o_b])
                for cb in range(2):
                    ps, ps_b = proj_block(hT, hT_b, 2 + cb)
                    rope_block(ps, ps_b, cs, cs_b, o, o_b, 2048 + cb * 512)
                for k in range(8):
                    op("pe", lambda p: p.transpose(out=kTp[:, k, :], in_=o[:, 2048 + k * 128:2048 + (k + 1) * 128],
                                                   identity=ident[:]),
                       reads=[o_b, ident_b], writes=[kTp_b], inc=(k == 7))
                op("act", lambda a: a.copy(out=o[:, 1024:2048].rearrange("p (k i) -> p k i", k=8), in_=kTp[:]),
                   reads=[kTp_b], writes=[o_b])
                for cb in range(4):
                    ps, ps_b = proj_block(hT, hT_b, 4 + cb)
                    op("act", lambda a: a.copy(out=o[:, 3072 + cb * 512:3072 + (cb + 1) * 512], in_=ps[:]),
                       reads=[ps_b], writes=[o_b])
                for cb in range(4):
                    ps, ps_b = proj_block(hT, hT_b, 8 + cb)
                    op("act", lambda a: a.activation(out=o[:, 5120 + cb * 512:5120 + (cb + 1) * 512], in_=ps[:], func=AF.Silu),
                       reads=[ps_b], writes=[o_b])
                dma(r1[t * 128:(t + 1) * 128, :], o[:], o_b, reads=[o_b], writes=[r1_b])
            mk.pop()

            check("R1")
            if NO > 0:
                mk.push()
                so2 = [mk.sb("so2", [128, 4096], BF16) for _ in range(2)]
                mt = [mk.sb("mt", [128, 4], F32) for _ in range(2)]
                sc8 = [mk.sb("sc8", [128, 8], F32) for _ in range(2)]
                kr, kr_b = mk.sb("kr", [128, 1024], F32)
                dma(xt[0][0][:], xo[0:128, :], xt[0][1], reads=[xob], writes=[xt[0][1]])
                dma(cst[0][0][:], rope_oth[0:128, :], cst[0][1], reads=[wdr], writes=[cst[0][1]])
                dma(mt[0][0][:], oth_meta[0:128, :], mt[0][1], reads=[wdr], writes=[mt[0][1]])
                gm, gm_b, sh, sh_b, _, _ = mods[1]
                for t in range(NO):
                    par = t % 2
                    if t + 1 < NO:
                        dma(xt[1 - par][0][:], xo[(t + 1) * 128:(t + 2) * 128, :], xt[1 - par][1], reads=[xob],
                            writes=[xt[1 - par][1]])
                        dma(cst[1 - par][0][:], rope_oth[(t + 1) * 128:(t + 2) * 128, :], cst[1 - par][1], reads=[wdr],
                            writes=[cst[1 - par][1]])
                        dma(mt[1 - par][0][:], oth_meta[(t + 1) * 128:(t + 2) * 128, :], mt[1 - par][1], reads=[wdr],
                            writes=[mt[1 - par][1]])
                    x_t, x_b = xt[par]
                    cs, cs_b = cst[par]
                    o, o_b = so2[par]
                    m, m_b = mt[par]
                    sc, sc_b = sc8[par]
                    hT, hT_b, _, _ = norm_mod_T(nb, par, x_t[:], x_b, gm, gm_b, sh, sh_b)
                    op("act", lambda a: a.activation(out=sc[:, 0:4], in_=lg[:, 0:4], func=AF.Exp, scale=m[:, 0:1]),
                       reads=[lg_b, m_b], writes=[sc_b])
                    op("act", lambda a: a.activation(out=sc[:, 4:8], in_=lg[:, 4:8], func=AF.Exp, scale=m[:, 2:3]),
                       reads=[lg_b, m_b], writes=[sc_b])
                    op("dve", lambda v: v.tensor_scalar(out=sc[:, 0:4], in0=sc[:, 0:4], scalar1=m[:, 1:2], scalar2=None,
                                                        op0=ALU.mult), reads=[sc_b, m_b], writes=[sc_b])
                    op("dve", lambda v: v.tensor_scalar(out=sc[:, 4:8], in0=sc[:, 4:8], scalar1=m[:, 3:4], scalar2=None,
                                                        op0=ALU.mult), reads=[sc_b, m_b], writes=[sc_b])
                    for cb in range(2):
                        ps, ps_b = proj_block(hT, hT_b, 2 + cb)
                        rope_block(ps, ps_b, cs, cs_b, kr, kr_b, cb * 512)
                    for h in range(4):
                        op("dve", lambda v: v.tensor_scalar(out=o[:, h * 256:(h + 1) * 256], in0=kr[:, h * 256:(h + 1) * 256],
                                                            scalar1=sc[:, h:h + 1], scalar2=None, op0=ALU.mult),
                           reads=[kr_b, sc_b], writes=[o_b])
                        op("pool", lambda g: g.tensor_scalar(out=o[:, 1024 + h * 256:1024 + (h + 1) * 256],
                                                             in0=kr[:, h * 256:(h + 1) * 256],
                                                             scalar1=sc[:, 4 + h:5 + h], scalar2=None, op0=ALU.mult),
                           reads=[kr_b, sc_b], writes=[o_b])
                    for cb in range(4):
                        ps, ps_b = proj_block(hT, hT_b, 4 + cb)
                        op("act", lambda a: a.copy(out=o[:, 2048 + cb * 512:2048 + (cb + 1) * 512], in_=ps[:]),
                           reads=[ps_b], writes=[o_b])
                    dma(osc[t * 128:(t + 1) * 128, :], o[:], o_b, reads=[o_b], writes=[osc_b])
                mk.pop()
            mk.pop()
            check("O")

            mk.push()
            DT, DT_b = mk.sb("DT", [128, 512], F32)
            qdF, qdF_b = mk.sb("qdF", [128, 8, 128], F32)
            qdB, qdB_b = mk.sb("qdB", [128, 8, 128], F32)
            kdec, kdec_b = mk.sb("kdec", [128, 8], F32)
            tmpd, tmpd_b = mk.sb("tmpd", [128, 128], F32)
            for h in range(4):
                op("act", lambda a: a.activation(out=tmpd[:], in_=ctab[:, CT_EF:CT_EF + 128], func=AF.Exp, scale=lg[:, h:h + 1]),
                   reads=[ctab_b, lg_b], writes=[tmpd_b])
                op("dve", lambda v: v.scalar_tensor_tensor(out=DT[:, h * 128:(h + 1) * 128], in0=tmpd[:], scalar=1.0 / 16,
                                                          in1=ctab[:, CT_MF:CT_MF + 128], op0=ALU.mult, op1=ALU.mult),
                   reads=[tmpd_b, ctab_b], writes=[DT_b])
                op("act", lambda a: a.activation(out=tmpd[:], in_=ctab[:, CT_EB:CT_EB + 128], func=AF.Exp,
                                                 scale=lg[:, 4 + h:5 + h]),
                   reads=[ctab_b, lg_b], writes=[tmpd_b])
                op("dve", lambda v: v.scalar_tensor_tensor(out=tmpd[:], in0=tmpd[:], scalar=1.0 / 16,
                                                          in1=ctab[:, CT_MB:CT_MB + 128], op0=ALU.mult, op1=ALU.mult),
                   reads=[tmpd_b, ctab_b], writes=[tmpd_b])
                op("dve", lambda v: v.tensor_tensor(out=DT[:, h * 128:(h + 1) * 128], in0=DT[:, h * 128:(h + 1) * 128],
                                                    in1=tmpd[:], op=ALU.add), reads=[tmpd_b, DT_b], writes=[DT_b])
                for dc in range(2):
                    op("act", lambda a: a.activation(out=qdF[:, h * 2 + dc, :], in_=ctab[:, CT_IP1:CT_IP1 + 128], func=AF.Exp,
                                                     scale=lg[:, h:h + 1]), reads=[ctab_b, lg_b], writes=[qdF_b])
                    op("act", lambda a: a.activation(out=qdB[:, h * 2 + dc, :], in_=ctab[:, CT_CMI:CT_CMI + 128], func=AF.Exp,
                                                     scale=lg[:, 4 + h:5 + h]), reads=[ctab_b, lg_b], writes=[qdB_b])
            op("dve", lambda v: v.tensor_scalar(out=qdF[:], in0=qdF[:], scalar1=1.0 / 16, scalar2=None, op0=ALU.mult),
               reads=[qdF_b], writes=[qdF_b])
            op("dve", lambda v: v.tensor_scalar(out=qdB[:], in0=qdB[:], scalar1=1.0 / 16, scalar2=None, op0=ALU.mult),
               reads=[qdB_b], writes=[qdB_b])
            op("act", lambda a: a.activation(out=kdec[:, 0:4], in_=lg[:, 0:4], func=AF.Exp, scale=ctab[:, CT_C127:CT_C127 + 1]),
               reads=[ctab_b, lg_b], writes=[kdec_b])
            op("act", lambda a: a.activation(out=kdec[:, 4:8], in_=lg[:, 4:8], func=AF.Exp, scale=ctab[:, CT_PIDX:CT_PIDX + 1]),
               reads=[ctab_b, lg_b], writes=[kdec_b])

            S32 = [mk.sb("S32", [128, 8, 512], F32) for _ in range(2)]
            Sbf, Sbf_b = mk.sb("Sbf", [128, 8, 512], BF16)
            Kt, Kt_b = mk.sb("Kt", [128, 1024], BF16)
            sps = [mk.ps("sps", [128, 512], F32) for _ in range(4)]
            spn = [0]

            sin_d, sin_d_b = mk.dram("sin_d", [256, 4096], F32)
            if NO > 0:
                mk.push()
                SinF, SinF_b = mk.sb("SinF", [128, 8, 512], F32)
                SinB, SinB_b = mk.sb("SinB", [128, 8, 512], F32)
                op("pool", lambda g: g.memset(SinF[:], 0.0), writes=[SinF_b])
                op("pool", lambda g: g.memset(SinB[:], 0.0), writes=[SinB_b])
                GSZ = 4 if NO % 4 == 0 else (2 if NO % 2 == 0 else 1)
                og = [mk.sb("og", [128, GSZ, 4096], BF16) for _ in range(2)]
                ngr = NO // GSZ
                def load_grp(gi):
                    g_, g_b = og[gi % 2]
                    dma(g_[:], osc[gi * GSZ * 128:(gi + 1) * GSZ * 128, :].rearrange("(i p) c -> p i c", p=128), g_b,
                        reads=[osc_b], writes=[g_b])
                load_grp(0)
                for gi in range(ngr):
                    if gi + 1 < ngr:
                        load_grp(gi + 1)
                    g_, g_b = og[gi % 2]
                    for d in range(2):
                        Sacc, Sacc_b = (SinF, SinF_b) if d == 0 else (SinB, SinB_b)
                        for h in range(4):
                            for dc in range(2):
                                ps, ps_b = sps[spn[0] % 4]
                                spn[0] += 1
                                for i in range(GSZ):
                                    kc0 = d * 1024 + h * 256 + dc * 128
                                    op("pe", lambda p: p.matmul(ps[:], lhsT=g_[:, i, kc0:kc0 + 128],
                                                                rhs=g_[:, i, 2048 + h * 512:2048 + (h + 1) * 512],
                                                                start=(i == 0), stop=(i == GSZ - 1)),
                                       reads=[g_b], writes=[ps_b], inc=(i == GSZ - 1))
                                op("dve", lambda v: v.tensor_tensor(out=Sacc[:, h * 2 + dc, :], in0=Sacc[:, h * 2 + dc, :],
                                                                    in1=ps[:], op=ALU.add),
                                   reads=[ps_b, Sacc_b], writes=[Sacc_b])
                dma(sin_d[0:128, :], SinF[:].rearrange("p a b -> p (a b)"), SinF_b, reads=[SinF_b], writes=[sin_d_b])
                dma(sin_d[128:256, :], SinB[:].rearrange("p a b -> p (a b)"), SinB_b, reads=[SinB_b], writes=[sin_d_b])
                mk.pop()

            def state_update(d, K_ap, K_b, V_ap, V_b):
                S, S_b = S32[d]
                for h in range(4):
                    eng = "dve" if h % 2 == 0 else "pool"
                    op(eng, lambda v: v.tensor_scalar(out=Kt[:, h * 256:(h + 1) * 256], in0=K_ap[:, h * 256:(h + 1) * 256],
                                                      scalar1=kdec[:, d * 4 + h:d * 4 + h + 1], scalar2=None, op0=ALU.mult),
                       reads=[K_b, kdec_b], writes=[Kt_b])
                for h in range(4):
                    for dc in range(2):
                        ps, ps_b = sps[spn[0] % 4]
                        spn[0] += 1
                        op("pe", lambda p: p.matmul(ps[:], lhsT=Kt[:, h * 256 + dc * 128:h * 256 + (dc + 1) * 128],
                                                    rhs=V_ap[:, h * 512:(h + 1) * 512], start=True, stop=True),
                           reads=[Kt_b, V_b], writes=[ps_b])
                        op("dve", lambda v: v.scalar_tensor_tensor(out=S[:, h * 2 + dc, :], in0=S[:, h * 2 + dc, :],
                                                                  scalar=cdec[:, d * 4 + h:d * 4 + h + 1], in1=ps[:],
                                                                  op0=ALU.mult, op1=ALU.add),
                           reads=[ps_b, S_b, cdec_b], writes=[S_b])

            seqs = [(0, TS), (TS, NT)]

            check("O2")
            mk.push()
            kv = [mk.sb("kv", [128, 3072], BF16) for _ in range(2)]
            sst = [mk.sb("sst", [128, 8, 512], BF16) for _ in range(2)]
            order = []
            for si, (t0, t1) in enumerate(seqs):
                order += [(si, t) for t in range(t1 - 1, t0 - 1, -1)]
            def load_kv(i):
                _, t = order[i]
                b_, b_b = kv[i % 2]
                dma(b_[:], r1[t * 128:(t + 1) * 128, 2048:5120], b_b, reads=[r1_b], writes=[b_b])
            if order:
                load_kv(0)
            for i, (si, t) in enumerate(order):
                if i + 1 < len(order):
                    load_kv(i + 1)
                S, S_b = S32[1]
                if t == seqs[si][1] - 1:
                    if si == 0 or NO == 0:
                        op("pool", lambda g: g.memset(S[:], 0.0), writes=[S_b])
                    else:
                        dma(S[:].rearrange("p a b -> p (a b)"), sin_d[128:256, :], S_b, reads=[sin_d_b], writes=[S_b])
                st_, st_b = sst[i % 2]
                op("act", lambda a: a.copy(out=st_[:], in_=S[:]), reads=[S_b], writes=[st_b])
                dma(sbst[t * 128:(t + 1) * 128, :], st_[:].rearrange("p a b -> p (a b)"), st_b, reads=[st_b], writes=[sbst_b])
                b_, b_b = kv[i % 2]
                state_update(1, b_[:, 0:1024], b_b, b_[:, 1024:3072], b_b)
            mk.pop()

            check("R2")
            mk.push()
            Wo, Wo_b = mk.sb("Wo", [128, 16, 1024], BF16)
            mk.push()
            stg = [mk.sb("stg", [128, 1024], F32) for _ in range(2)]
            load_weight_bf16(Wo, Wo_b, ret_w_out, 2048, 1024, stg, 1024)
            mk.pop()
            mods = make_mod(0, 0, True, want_norm=False)
            gng, gng_b = mk.sb("gng", [128, 2048], F32)
            load_bc(gng[:], gng_b, ret_gn_g[0:1, :])
            rt = [mk.sb("rt", [128, 7168], BF16) for _ in range(2)]
            sbt = [mk.sb("sbt", [128, 8, 512], BF16) for _ in range(2)]
            xt = [mk.sb("xt", [128, 1024], F32) for _ in range(2)]
            QfT, QfT_b = mk.sb("QfT", [128, 8, 128], BF16)
            QbT, QbT_b = mk.sb("QbT", [128, 8, 128], BF16)
            sTm, sTm_b = mk.sb("sTm", [128, 512], BF16)
            on, on_b = mk.sb("on", [128, 2048], F32)
            ogb, ogb_b = mk.sb("ogb", [128, 2048], BF16)
            ogT, ogT_b = mk.sb("ogT", [128, 16, 128], BF16)
            st6, st6_b = mk.sb("st6", [128, 4, 6], F32)
            mv, mv_b = mk.sb("mv", [128, 4, 2], F32)
            rstd, rstd_b = mk.sb("rstd", [128, 4], F32)
            xo_t = [mk.sb("xo_t", [128, 1024], F32) for _ in range(1)]
            psS, psS_b = mk.ps("psS", [128, 512], F32)
            psO = [mk.ps("psO", [128, 512], F32) for _ in range(2)]
            psT, psT_b = mk.ps("psT", [128, 8, 128], BF16)

            order = [(si, t) for si, (t0, t1) in enumerate(seqs) for t in range(t0, t1)]
            def load_r3(i):
                _, t = order[i]
                a_, a_b = rt[i % 2]
                b_, b_b = sbt[i % 2]
                c_, c_b = xt[i % 2]
                dma(a_[:], r1[t * 128:(t + 1) * 128, :], a_b, reads=[r1_b], writes=[a_b])
                dma(b_[:].rearrange("p a b -> p (a b)"), sbst[t * 128:(t + 1) * 128, :], b_b, reads=[sbst_b], writes=[b_b])
                dma(c_[:], xs[t * 128:(t + 1) * 128, :], c_b, reads=[xsb], writes=[c_b])
            if order:
                load_r3(0)
            for i, (si, t) in enumerate(order):
                if i + 1 < len(order):
                    load_r3(i + 1)
                S, S_b = S32[0]
                if t == seqs[si][0]:
                    if si == 0 or NO == 0:
                        op("pool", lambda g: g.memset(S[:], 0.0), writes=[S_b])
                    else:
                        dma(S[:].rearrange("p a b -> p (a b)"), sin_d[0:128, :], S_b, reads=[sin_d_b], writes=[S_b])
                a_, a_b = rt[i % 2]
                sb_, sb_b = sbt[i % 2]
                x_t, x_b = xt[i % 2]
                _, _, _, _, gt, gt_b = mods[si]
                QT = a_[:, 0:1024].rearrange("p (k i) -> p k i", k=8)
                KT = a_[:, 1024:2048].rearrange("p (k i) -> p k i", k=8)
                op("act", lambda a: a.copy(out=Sbf[:], in_=S[:]), reads=[S_b], writes=[Sbf_b])
                for h in range(4):
                    for dc in range(2):
                        op("pe", lambda p: p.matmul(psS[:, h * 128:(h + 1) * 128], lhsT=KT[:, h * 2 + dc, :],
                                                    rhs=QT[:, h * 2 + dc, :], start=(dc == 0), stop=(dc == 1)),
                           reads=[a_b], writes=[psS_b], inc=(h == 3 and dc == 1))
                op("dve", lambda v: v.tensor_tensor(out=sTm[:], in0=psS[:], in1=DT[:], op=ALU.mult),
                   reads=[psS_b, DT_b], writes=[sTm_b])
                op("pool", lambda g: g.tensor_tensor(out=QfT[:], in0=QT, in1=qdF[:], op=ALU.mult),
                   reads=[a_b, qdF_b], writes=[QfT_b])
                op("pool", lambda g: g.tensor_tensor(out=QbT[:], in0=QT, in1=qdB[:], op=ALU.mult),
                   reads=[a_b, qdB_b], writes=[QbT_b])
                for h in range(4):
                    po, po_b = psO[h % 2]
                    op("pe", lambda p: p.matmul(po[:], lhsT=sTm[:, h * 128:(h + 1) * 128],
                                                rhs=a_[:, 3072 + h * 512:3072 + (h + 1) * 512], start=True, stop=False),
                       reads=[sTm_b, a_b], writes=[po_b], inc=False)
                    for dc in range(2):
                        op("pe", lambda p: p.matmul(po[:], lhsT=QfT[:, h * 2 + dc, :], rhs=Sbf[:, h * 2 + dc, :],
                                                    start=False, stop=False),
                           reads=[QfT_b, Sbf_b], writes=[po_b], inc=False)
                    for dc in range(2):
                        op("pe", lambda p: p.matmul(po[:], lhsT=QbT[:, h * 2 + dc, :], rhs=sb_[:, h * 2 + dc, :],
                                                    start=False, stop=(dc == 1)),
                           reads=[QbT_b, sb_b], writes=[po_b], inc=(dc == 1))
                    op("dve", lambda v: v.bn_stats(out=st6[:, h, :], in_=po[:]), reads=[po_b], writes=[st6_b])
                    op("dve", lambda v: v.bn_aggr(out=mv[:, h, :], in_=st6[:, h, :]), reads=[st6_b], writes=[mv_b])
                    op("dve", lambda v: v.tensor_scalar(out=rstd[:, h:h + 1], in0=mv[:, h, 1:2], scalar1=EPS, scalar2=None,
                                                        op0=ALU.add), reads=[mv_b], writes=[rstd_b])
                    op("act", lambda a: a.sqrt(out=rstd[:, h:h + 1], in_=rstd[:, h:h + 1]), reads=[rstd_b], writes=[rstd_b])
                    op("dve", lambda v: v.reciprocal(out=rstd[:, h:h + 1], in_=rstd[:, h:h + 1]), reads=[rstd_b], writes=[rstd_b])
                    op("dve", lambda v: v.tensor_scalar(out=on[:, h * 512:(h + 1) * 512], in0=po[:], scalar1=mv[:, h, 0:1],
                                                        scalar2=rstd[:, h:h + 1], op0=ALU.subtract, op1=ALU.mult),
                       reads=[po_b, mv_b, rstd_b], writes=[on_b])
                op("pool", lambda g: g.tensor_tensor(out=on[:], in0=on[:], in1=gng[:], op=ALU.mult),
                   reads=[on_b, gng_b], writes=[on_b])
                op("pool", lambda g: g.tensor_tensor(out=ogb[:], in0=on[:], in1=a_[:, 5120:7168], op=ALU.mult),
                   reads=[on_b, a_b], writes=[ogb_b])
                for half in range(2):
                    for k in range(8):
                        kk = half * 8 + k
                        op("pe", lambda p: p.transpose(out=psT[:, k, :], in_=ogb[:, kk * 128:(kk + 1) * 128], identity=ident[:]),
                           reads=[ogb_b, ident_b], writes=[psT_b], inc=(k == 7))
                    op("act", lambda a: a.copy(out=ogT[:, half * 8:(half + 1) * 8, :], in_=psT[:]),
                       reads=[psT_b], writes=[ogT_b])
                xo_, xo_b = xo_t[0]
                for cb in range(2):
                    po, po_b = psO[cb]
                    for k in range(16):
                        op("pe", lambda p: p.matmul(po[:], lhsT=ogT[:, k, :], rhs=Wo[:, k, cb * 512:(cb + 1) * 512],
                                                    start=(k == 0), stop=(k == 15)),
                           reads=[ogT_b, Wo_b], writes=[po_b], inc=(k == 15))
                    op("dve", lambda v: v.tensor_tensor(out=xo_[:, cb * 512:(cb + 1) * 512], in0=po[:],
                                                        in1=gt[:, cb * 512:(cb + 1) * 512], op=ALU.mult),
                       reads=[po_b, gt_b], writes=[xo_b])
                op("pool", lambda g: g.tensor_tensor(out=x_t[:], in0=xo_[:], in1=x_t[:], op=ALU.add),
                   reads=[xo_b, x_b], writes=[x_b])
                dma(xa[t * 128:(t + 1) * 128, :], x_t[:], x_b, reads=[x_b], writes=[xa_b])
                state_update(0, a_[:, 2048:3072], a_b, a_[:, 3072:5120], a_b)
            mk.pop()
            mk.pop()
            check("R3")

            NBM = NT * 4 + NE
            U32 = mybir.dt.uint32
            I32 = mybir.dt.int32
            xg_d, xg_b = mk.dram("xg_d", [NBM * 128, 1024], BF16)
            yg_d, yg_b = mk.dram("yg_d", [NBM * 128, 1024], F32)

            bnd_reg = nc.gpsimd.alloc_register("bnd")
            nc.gpsimd.reg_mov(bnd_reg, 2 * NE * 128 - 1)

            def moe_layer(l, xin, xin_b, xout, xout_b):
                mk.push()
                Rall, Rall_b = mk.sb("Rall", [128, NT, NE], F32)
                I4all, I4all_b = mk.sb("I4all", [128, NT, 4], F32)
                G4all, G4all_b = mk.sb("G4all", [128, NT, 4], F32)
                Dall, Dall_b = mk.sb("Dall", [128, NT, 4], U32)
                off, off_b = mk.sb("off", [128, NE], F32)
                ebf, ebf_b = mk.sb("ebf", [128, NBM], F32)
                idxW, idxW_b = mk.sb("idxW", [128, NBM], U32)
                pst, pst_b = mk.sb("pst", [128, NE], F32)
                op("pool", lambda g: g.memset(off[:], 0.0), writes=[off_b])
                mk.push()
                mods = make_mod(l, 1, False)
                nb = NormBufs()
                xt = [mk.sb("xt", [128, 1024], F32) for _ in range(2)]
                wr32, wr32_b = mk.sb("wr32", [128, 8, NE], F32)
                wr, wr_b = mk.sb("wr", [128, 8, NE], BF16)
                brb, brb_b = mk.sb("brb", [128, NE], F32)
                lgt, lgt_b = mk.sb("lgt", [128, NE], F32)
                mskb, mskb_b = mk.sb("mskb", [128, NE], BF16)
                top, top_b = mk.sb("top", [128, 8], F32)
                idx8, idx8_b = mk.sb("idx8", [128, 8], U32)
                e4, e4_b = mk.sb("e4", [128, 4], F32)
                nm, nm_b = mk.sb("nm", [128, 1], F32)
                sm, sm_b = mk.sb("sm", [128, 1], F32)
                Ubf, Ubf_b = mk.sb("Ubf", [128, 128], BF16)
                onesb, onesb_b = mk.sb("onesb", [128, 128], BF16)
                lps, lps_b = mk.ps("lps", [128, NE], F32)
                rps, rps_b = mk.ps("rps", [128, NE], F32)
                cps, cps_b = mk.ps("cps", [128, NE], F32)
                op("pool", lambda g: g.tensor_copy(out=Ubf[:], in_=ctab[:, CT_U:CT_U + 128]), reads=[ctab_b], writes=[Ubf_b])
                op("pool", lambda g: g.memset(onesb[:], 1.0), writes=[onesb_b])
                dma(wr32[:], moe_w_r[l * 1024:(l + 1) * 1024, :].rearrange("(k p) e -> p k e", p=128), wr32_b,
                    reads=[wdr], writes=[wr32_b])
                op("pool", lambda g: g.tensor_copy(out=wr[:], in_=wr32[:]), reads=[wr32_b], writes=[wr_b])
                load_bc(brb[:], brb_b, moe_b_r[l:l + 1, :])
                dma(xt[0][0][:], xin[0:128, :], xt[0][1], reads=[xin_b], writes=[xt[0][1]])
                for t in range(NT):
                    par = t % 2
                    if t + 1 < NT:
                        dma(xt[1 - par][0][:], xin[(t + 1) * 128:(t + 2) * 128, :], xt[1 - par][1], reads=[xin_b],
                            writes=[xt[1 - par][1]])
                    gm, gm_b, sh, sh_b, _, _ = mods[seq_of(t)]
                    x_t, x_b = xt[par]
                    hT, hT_b, hh, hh_b = norm_mod_T(nb, par, x_t[:], x_b, gm, gm_b, sh, sh_b)
                    dma(h2d[t * 128:(t + 1) * 128, :], hh[:], hh_b, reads=[hh_b], writes=[h2d_b])
                    for k in range(8):
                        op("pe", lambda p: p.matmul(lps[:], lhsT=hT[:, k, :], rhs=wr[:, k, :], start=(k == 0), stop=(k == 7)),
                           reads=[hT_b, wr_b], writes=[lps_b], inc=(k == 7))
                    op("dve", lambda v: v.tensor_tensor(out=lgt[:], in0=lps[:], in1=brb[:], op=ALU.add),
                       reads=[lps_b, brb_b], writes=[lgt_b])
                    op("dve", lambda v: v.max(out=top[:], in_=lgt[:]), reads=[lgt_b], writes=[top_b])
                    op("dve", lambda v: v.max_index(out=idx8[:], in_max=top[:], in_values=lgt[:]),
                       reads=[lgt_b, top_b], writes=[idx8_b])
                    op("dve", lambda v: v.tensor_copy(out=I4all[:, t, :], in_=idx8[:, 0:4]), reads=[idx8_b], writes=[I4all_b])
                    op("dve", lambda v: v.tensor_scalar(out=mskb[:], in0=lgt[:], scalar1=top[:, 3:4], scalar2=None, op0=ALU.is_ge),
                       reads=[lgt_b, top_b], writes=[mskb_b])
                    op("dve", lambda v: v.tensor_scalar(out=nm[:], in0=top[:, 0:1], scalar1=-1.0, scalar2=None, op0=ALU.mult),
                       reads=[top_b], writes=[nm_b])
                    op("act", lambda a: a.activation(out=e4[:], in_=top[:, 0:4], func=AF.Exp, bias=nm[:, 0:1]),
                       reads=[top_b, nm_b], writes=[e4_b])
                    op("dve", lambda v: v.reduce_sum(out=sm[:], in_=e4[:], axis=mybir.AxisListType.X),
                       reads=[e4_b], writes=[sm_b])
                    op("dve", lambda v: v.reciprocal(out=sm[:], in_=sm[:]), reads=[sm_b], writes=[sm_b])
                    op("dve", lambda v: v.tensor_scalar(out=G4all[:, t, :], in0=e4[:], scalar1=sm[:, 0:1], scalar2=None,
                                                        op0=ALU.mult), reads=[e4_b, sm_b], writes=[G4all_b])
                    op("pe", lambda p: p.matmul(rps[:], lhsT=Ubf[:], rhs=mskb[:], start=True, stop=True),
                       reads=[Ubf_b, mskb_b], writes=[rps_b])
                    op("pe", lambda p: p.matmul(cps[:], lhsT=onesb[:], rhs=mskb[:], start=True, stop=True),
                       reads=[onesb_b, mskb_b], writes=[cps_b])
                    op("dve", lambda v: v.tensor_tensor(out=Rall[:, t, :], in0=rps[:], in1=off[:], op=ALU.add),
                       reads=[rps_b, off_b], writes=[Rall_b])
                    op("dve", lambda v: v.tensor_tensor(out=off[:], in0=cps[:], in1=off[:], op=ALU.add),
                       reads=[cps_b, off_b], writes=[off_b])
                mk.pop()

                mk.push()
                thr, thr_b = mk.sb("thr", [128, 128], F32)
                cmpt, cmpt_b = mk.sb("cmpt", [128, NE * NT], F32)
                nbk, nbk_b = mk.sb("nbk", [128, NE], F32)
                ca, ca_b = mk.sb("ca", [128, NE], F32)
                cb_, cb_b = mk.sb("cb_", [128, NE], F32)
                io_i, io_i_b = mk.sb("io_i", [128, NBM], I32)
                thb, thb_b = mk.sb("thb", [128, NBM], F32)
                CH = 64
                cmpb, cmpb_b = mk.sb("cmpb", [128, CH * NE], F32)
                sk, sk_b = mk.sb("sk", [128, NBM], F32)
                rowf, rowf_b = mk.sb("rowf", [128, NBM], F32)
                pcol, pcol_b = mk.sb("pcol", [128, 1], F32)
                op("dve", lambda v: v.tensor_scalar(out=thr[:], in0=ctab[:, CT_IO:CT_IO + 128], scalar1=128.0, scalar2=None,
                                                    op0=ALU.mult), reads=[ctab_b], writes=[thr_b])
                op("dve", lambda v: v.tensor_tensor(out=cmpt[:].rearrange("p (e m) -> p e m", e=NE),
                                                    in0=off[:].unsqueeze(2).to_broadcast([128, NE, NT]),
                                                    in1=thr[:, 0:NT].unsqueeze(1).to_broadcast([128, NE, NT]), op=ALU.is_gt),
                   reads=[off_b, thr_b], writes=[cmpt_b])
                op("dve", lambda v: v.reduce_sum(out=nbk[:], in_=cmpt[:].rearrange("p (e m) -> p e m", e=NE),
                                                 axis=mybir.AxisListType.X), reads=[cmpt_b], writes=[nbk_b])
                op("dve", lambda v: v.tensor_scalar(out=ca[:], in0=nbk[:], scalar1=128.0, scalar2=None, op0=ALU.mult),
                   reads=[nbk_b], writes=[ca_b])
                op("dve", lambda v: v.tensor_copy(out=pst[:], in_=ca[:]), reads=[ca_b], writes=[pst_b])
                cur, cur_b, nxt, nxt_b = ca, ca_b, cb_, cb_b
                s_ = 1
                while s_ < NE:
                    sft = s_
                    op("dve", lambda v: v.tensor_copy(out=nxt[:, 0:sft], in_=cur[:, 0:sft]), reads=[cur_b], writes=[nxt_b])
                    op("dve", lambda v: v.tensor_tensor(out=nxt[:, sft:NE], in0=cur[:, sft:NE], in1=cur[:, 0:NE - sft], op=ALU.add),
                       reads=[cur_b], writes=[nxt_b])
                    cur, cur_b, nxt, nxt_b = nxt, nxt_b, cur, cur_b
                    s_ *= 2
                pend, pend_b = cur, cur_b
                op("dve", lambda v: v.tensor_tensor(out=pst[:], in0=pend[:], in1=pst[:], op=ALU.subtract),
                   reads=[pend_b, pst_b], writes=[pst_b])
                op("pool", lambda g: g.iota(io_i[:], pattern=[[1, NBM]], base=0, channel_multiplier=0), writes=[io_i_b])
                op("dve", lambda v: v.tensor_copy(out=thb[:], in_=io_i[:]), reads=[io_i_b], writes=[thb_b])
                op("dve", lambda v: v.tensor_scalar(out=thb[:], in0=thb[:], scalar1=128.0, scalar2=None, op0=ALU.mult),
                   reads=[thb_b], writes=[thb_b])
                for c0 in range(0, NBM, CH):
                    cw = min(CH, NBM - c0)
                    op("dve", lambda v: v.tensor_tensor(out=cmpb[:, 0:cw * NE].rearrange("p (b e) -> p b e", e=NE),
                                                        in0=pend[:].unsqueeze(1).to_broadcast([128, cw, NE]),
                                                        in1=thb[:, c0:c0 + cw].unsqueeze(2).to_broadcast([128, cw, NE]), op=ALU.is_le),
                       reads=[pend_b, thb_b], writes=[cmpb_b])
                    op("dve", lambda v: v.reduce_sum(out=ebf[:, c0:c0 + cw], in_=cmpb[:, 0:cw * NE].rearrange("p (b e) -> p b e", e=NE),
                                                     axis=mybir.AxisListType.X), reads=[cmpb_b], writes=[ebf_b])
                op("dve", lambda v: v.tensor_scalar(out=ebf[:], in0=ebf[:], scalar1=float(NE - 1), scalar2=None, op0=ALU.min),
                   reads=[ebf_b], writes=[ebf_b])
                op("dve", lambda v: v.tensor_scalar(out=pcol[:], in0=ctab[:, CT_PIDX:CT_PIDX + 1], scalar1=float(l * NE * 128),
                                                    scalar2=None, op0=ALU.add), reads=[ctab_b], writes=[pcol_b])
                op("pool", lambda g: g.memset(sk[:], 0.0), writes=[sk_b])
                op("dve", lambda v: v.tensor_tensor(out=sk[:, 2:NBM], in0=ebf[:, 2:NBM], in1=ebf[:, 0:NBM - 2], op=ALU.is_equal),
                   reads=[ebf_b, sk_b], writes=[sk_b])
                op("dve", lambda v: v.tensor_scalar(out=rowf[:], in0=ebf[:], scalar1=128.0, scalar2=pcol[:, 0:1],
                                                    op0=ALU.mult, op1=ALU.add), reads=[ebf_b, pcol_b], writes=[rowf_b])
                op("dve", lambda v: v.scalar_tensor_tensor(out=rowf[:], in0=sk[:], scalar=float(1 << 22), in1=rowf[:],
                                                          op0=ALU.mult, op1=ALU.add), reads=[sk_b, rowf_b], writes=[rowf_b])
                op("dve", lambda v: v.tensor_copy(out=idxW[:], in_=rowf[:]), reads=[rowf_b], writes=[idxW_b])

                tmpR, tmpR_b = mk.sb("tmpR", [128, NE], F32)
                jk, jk_b = mk.sb("jk", [128, NE], F32)
                d4f, d4f_b = mk.sb("d4f", [128, 4], F32)
                hx = [mk.sb("hx", [128, 1024], BF16) for _ in range(2)]
                for t in range(NT):
                    h_, h_b = hx[t % 2]
                    dma(h_[:], h2d[t * 128:(t + 1) * 128, :], h_b, reads=[h2d_b], writes=[h_b])
                    op("dve", lambda v: v.tensor_tensor(out=tmpR[:], in0=Rall[:, t, :], in1=pst[:], op=ALU.add),
                       reads=[Rall_b, pst_b], writes=[tmpR_b])
                    for k in range(4):
                        op("dve", lambda v: v.scalar_tensor_tensor(out=jk[:], in0=ctab[:, CT_IO:CT_IO + NE], scalar=I4all[:, t, k:k + 1],
                                                                  in1=tmpR[:], op0=ALU.is_equal, op1=ALU.mult),
                           reads=[ctab_b, I4all_b, tmpR_b], writes=[jk_b])
                        op("dve", lambda v: v.reduce_sum(out=d4f[:, k:k + 1], in_=jk[:], axis=mybir.AxisListType.X),
                           reads=[jk_b], writes=[d4f_b])
                    op("dve", lambda v: v.tensor_copy(out=Dall[:, t, :], in_=d4f[:]), reads=[d4f_b], writes=[Dall_b])
                    for k in range(4):
                        dma(None, None, h_b, reads=[h_b, Dall_b], writes=[xg_b], q="pool",
                            fn=lambda g: g.indirect_dma_start(out=xg_d[:, :],
                                                              out_offset=bass.IndirectOffsetOnAxis(ap=Dall[:, t, k:k + 1], axis=0),
                                                              in_=h_[:], in_offset=None))
                mk.pop()

                mk.push()
                Wgu = [mk.sb("Wgu", [128, 8, 2 * DE], BF16) for _ in range(2)]
                Wdn = [mk.sb("Wdn", [128, KE, 1024], BF16) for _ in range(2)]
                bguA, bguA_b = mk.sb("bguA", [NE, 2 * DE], BF16)
                bdnA, bdnA_b = mk.sb("bdnA", [NE, 1024], BF16)
                ohs = [mk.sb("oh", [NE, 128], BF16) for _ in range(2)]
                xgs = [mk.sb("xgs", [128, 1024], BF16) for _ in range(2)]
                xTs = [mk.sb("xTs", [128, 8, 128], BF16) for _ in range(2)]
                gl, gl_b = mk.sb("gl", [128, DE], F32)
                sg, sg_b = mk.sb("sg", [128, DE], F32)
                ln, ln_b = mk.sb("ln", [128, DE], F32)
                actb = [mk.sb("actb", [128, DE], BF16) for _ in range(2)]
                actT = [mk.sb("actT", [128, KE, 128], BF16) for _ in range(2)]
                ygs = [mk.sb("ygs", [128, 1024], F32) for _ in range(2)]
                hps = [mk.ps("hps", [128, 512], F32) for _ in range(4)]
                yps = [mk.ps("yps", [128, 512], F32) for _ in range(2)]
                tps, tps_b = mk.ps("tps", [128, KE, 128], BF16)
                tpx, tpx_b = mk.ps("tpx", [128, 8, 128], BF16)
                dma(bguA[:], moe_b_gu[l * NE:(l + 1) * NE, :], bguA_b, reads=[wdr], writes=[bguA_b], q="pool")
                dma(bdnA[:], moe_b_dn[l * NE:(l + 1) * NE, :], bdnA_b, reads=[wdr], writes=[bdnA_b], q="pool")
                ROWS = 2 * NE * 128

                def load_wgu(b):
                    if b >= NBM:
                        return
                    wg, wg_b = Wgu[b % 2]
                    dma(None, None, wg_b, reads=[wdr, idxW_b], writes=[wg_b], q="pool",
                        fn=lambda g: g.indirect_dma_start(out=wg[:].rearrange("p k n -> p (k n)"), out_offset=None, in_=moe_w_gu[:, :],
                                                          in_offset=bass.IndirectOffsetOnAxis(ap=idxW[:, b:b + 1], axis=0),
                                                          bounds_check=bnd_reg, oob_is_err=False))

                def load_wdn(b):
                    if b >= NBM:
                        return
                    wd, wd_b = Wdn[b % 2]
                    dma(None, None, wd_b, reads=[wdr, idxW_b], writes=[wd_b], q="pool",
                        fn=lambda g: g.indirect_dma_start(out=wd[:].rearrange("p k n -> p (k n)"), out_offset=None, in_=moe_w_dn[:, :],
                                                          in_offset=bass.IndirectOffsetOnAxis(ap=idxW[:, b:b + 1], axis=0),
                                                          bounds_check=bnd_reg, oob_is_err=False))

                def load_x(b):
                    if b >= NBM:
                        return
                    x_, x_b_ = xgs[b % 2]
                    dma(x_[:], xg_d[b * 128:(b + 1) * 128, :], x_b_, reads=[xg_b], writes=[x_b_])

                def front(b):
                    par = b % 2
                    wg, wg_b = Wgu[par]
                    oh, oh_b = ohs[par]
                    x_, x_b_ = xgs[par]
                    xT, xT_b = xTs[par]
                    ab, ab_b = actb[par]
                    op("dve", lambda v: v.tensor_scalar(out=oh[:], in0=ebf[0:NE, b:b + 1].to_broadcast([NE, 128]),
                                                        scalar1=ctab[0:NE, CT_PIDX:CT_PIDX + 1], scalar2=None, op0=ALU.is_equal),
                       reads=[ebf_b, ctab_b], writes=[oh_b])
                    for k in range(8):
                        op("pe", lambda p: p.transpose(out=tpx[:, k, :], in_=x_[:, k * 128:(k + 1) * 128], identity=ident[:]),
                           reads=[x_b_, ident_b], writes=[tpx_b], inc=(k == 7))
                    op("act", lambda a: a.copy(out=xT[:], in_=tpx[:]), reads=[tpx_b], writes=[xT_b])
                    for half in range(2):
                        for q in range(GH):
                            cb = half * GH + q
                            hp, hp_b = hps[cb % 4]
                            op("pe", lambda p: p.matmul(hp[:], lhsT=oh[:], rhs=bguA[:, cb * 512:(cb + 1) * 512], start=True, stop=False),
                               reads=[oh_b, bguA_b], writes=[hp_b], inc=False)
                            for k in range(8):
                                op("pe", lambda p: p.matmul(hp[:], lhsT=xT[:, k, :], rhs=wg[:, k, cb * 512:(cb + 1) * 512],
                                                            start=False, stop=(k == 7)),
                                   reads=[xT_b, wg_b], writes=[hp_b], inc=(k == 7))
                            if half == 0:
                                op("dve", lambda v: v.tensor_scalar(out=gl[:, q * 512:(q + 1) * 512], in0=hp[:], scalar1=7.0,
                                                                    scalar2=None, op0=ALU.min), reads=[hp_b], writes=[gl_b])
                            else:
                                op("dve", lambda v: v.tensor_scalar(out=ln[:, q * 512:(q + 1) * 512], in0=hp[:], scalar1=7.0,
                                                                    scalar2=-7.0, op0=ALU.min, op1=ALU.max),
                                   reads=[hp_b], writes=[ln_b])
                    op("act", lambda a: a.activation(out=sg[:], in_=gl[:], func=AF.Sigmoid, scale=1.702),
                       reads=[gl_b], writes=[sg_b])
                    op("dve", lambda v: v.scalar_tensor_tensor(out=ln[:], in0=ln[:], scalar=1.0, in1=gl[:],
                                                              op0=ALU.add, op1=ALU.mult),
                       reads=[ln_b, gl_b], writes=[ln_b])
                    op("dve", lambda v: v.tensor_tensor(out=ab[:], in0=ln[:], in1=sg[:], op=ALU.mult),
                       reads=[ln_b, sg_b], writes=[ab_b])

                def back(b):
                    par = b % 2
                    wd, wd_b = Wdn[par]
                    oh, oh_b = ohs[par]
                    ab, ab_b = actb[par]
                    aT, aT_b = actT[par]
                    yg, yg_bb = ygs[par]
                    for k in range(KE):
                        op("pe", lambda p: p.transpose(out=tps[:, k, :], in_=ab[:, k * 128:(k + 1) * 128], identity=ident[:]),
                           reads=[ab_b, ident_b], writes=[tps_b], inc=(k == KE - 1))
                    op("act", lambda a: a.copy(out=aT[:], in_=tps[:]), reads=[tps_b], writes=[aT_b])
                    for cb in range(2):
                        yp, yp_b = yps[cb]
                        op("pe", lambda p: p.matmul(yp[:], lhsT=oh[:], rhs=bdnA[:, cb * 512:(cb + 1) * 512], start=True, stop=False),
                           reads=[oh_b, bdnA_b], writes=[yp_b], inc=False)
                        for k in range(KE):
                            op("pe", lambda p: p.matmul(yp[:], lhsT=aT[:, k, :], rhs=wd[:, k, cb * 512:(cb + 1) * 512],
                                                        start=False, stop=(k == KE - 1)),
                               reads=[aT_b, wd_b], writes=[yp_b], inc=(k == KE - 1))
                        op("act", lambda a: a.copy(out=yg[:, cb * 512:(cb + 1) * 512], in_=yp[:]), reads=[yp_b], writes=[yg_bb])
                    dma(yg_d[b * 128:(b + 1) * 128, :], yg[:], yg_bb, reads=[yg_bb], writes=[yg_b])

                for b0 in (0, 1):
                    load_wgu(b0)
                    load_wdn(b0)
                    load_x(b0)
                front(0)
                for b in range(NBM):
                    if b + 1 < NBM:
                        front(b + 1)
                    back(b)
                    load_wgu(b + 2)
                    load_wdn(b + 2)
                    load_x(b + 2)
                mk.pop()

                mk.push()
                gts = make_mod(l, 1, True, want_norm=False)
                yk = [[mk.sb("yk", [128, 1024], F32) for _ in range(4)] for _ in range(2)]
                acc, acc_b = mk.sb("acc", [128, 1024], F32)
                xt = [mk.sb("xt", [128, 1024], F32) for _ in range(2)]

                def load_c(t):
                    for k in range(4):
                        y_, y_b = yk[t % 2][k]
                        dma(None, None, y_b, reads=[yg_b, Dall_b], writes=[y_b], q="pool",
                            fn=lambda g: g.indirect_dma_start(out=y_[:], out_offset=None, in_=yg_d[:, :],
                                                              in_offset=bass.IndirectOffsetOnAxis(ap=Dall[:, t, k:k + 1], axis=0)))
                    x_t, x_b = xt[t % 2]
                    dma(x_t[:], xin[t * 128:(t + 1) * 128, :], x_b, reads=[xin_b], writes=[x_b])

                load_c(0)
                for t in range(NT):
                    if t + 1 < NT:
                        load_c(t + 1)
                    x_t, x_b = xt[t % 2]
                    _, _, _, _, gt, gt_b = gts[seq_of(t)]
                    y0, y0_b = yk[t % 2][0]
                    op("dve", lambda v: v.tensor_scalar(out=acc[:], in0=y0[:], scalar1=G4all[:, t, 0:1], scalar2=None, op0=ALU.mult),
                       reads=[y0_b, G4all_b], writes=[acc_b])
                    for k in range(1, 4):
                        y_, y_b = yk[t % 2][k]
                        op("dve", lambda v: v.scalar_tensor_tensor(out=acc[:], in0=y_[:], scalar=G4all[:, t, k:k + 1], in1=acc[:],
                                                                  op0=ALU.mult, op1=ALU.add),
                           reads=[y_b, G4all_b, acc_b], writes=[acc_b])
                    op("pool", lambda g: g.tensor_tensor(out=acc[:], in0=acc[:], in1=gt[:], op=ALU.mult),
                       reads=[acc_b, gt_b], writes=[acc_b])
                    op("pool", lambda g: g.tensor_tensor(out=x_t[:], in0=x_t[:], in1=acc[:], op=ALU.add),
                       reads=[acc_b, x_b], writes=[x_b])
                    dma(xout[t * 128:(t + 1) * 128, :], x_t[:], x_b, reads=[x_b], writes=[xout_b])
                mk.pop()
                mk.pop()

            moe_layer(0, xa, xa_b, xb, xb_b)
            check("E0")

            mk.push()
            Wg, Wg_b = mk.sb("Wg", [128, 8, 4096], BF16)
            Wgo, Wgo_b = mk.sb("Wgo", [128, 16, 1024], BF16)
            wsT, wsT_b = mk.sb("wsT", [128, 4, 128], BF16)
            bb16, bb16_b = mk.sb("bb16", [1, 5120], BF16)
            mk.push()
            stg = [mk.sb("stg", [128, 2048], F32) for _ in range(2)]
            load_weight_bf16(Wg, Wg_b, gm_w_in, 1024, 4096, stg, 2048)
            load_weight_bf16(Wgo, Wgo_b, gm_w_out, 2048, 1024, stg, 1024)
            s_, s_b = stg[0]
            dma(s_[:, 0:512].rearrange("p (g i) -> p g i", g=4), gm_w_sT[:, :].rearrange("(g p) i -> p g i", p=128), s_b,
                reads=[wdr], writes=[s_b])
            op("pool", lambda g: g.tensor_copy(out=wsT[:], in_=s_[:, 0:512].rearrange("p (g i) -> p g i", g=4)),
               reads=[s_b], writes=[wsT_b])
            b32, b32_b = mk.sb("b32", [1, 5120], F32)
            dma(b32[:, 0:4096], gm_b_in[0:1, :], b32_b, reads=[wdr], writes=[b32_b])
            dma(b32[:, 4096:5120], gm_b_out[0:1, :], b32_b, reads=[wdr], writes=[b32_b])
            op("pool", lambda g: g.tensor_copy(out=bb16[:], in_=b32[:]), reads=[b32_b], writes=[bb16_b])
            mk.barrier()
            mk.pop()
            mods = make_mod(1, 0, True)
            nb = NormBufs()
            vng, vng_b = mk.sb("vng", [128, 2048], F32)
            vnb, vnb_b = mk.sb("vnb", [128, 2048], F32)
            bsT, bsT_b = mk.sb("bsT", [128, 4], F32)
            load_bc(vng[:], vng_b, gm_vn_g[0:1, :])
            load_bc(vnb[:], vnb_b, gm_vn_b[0:1, :])
            dma(bsT[:], gm_b_sT[:, :], bsT_b, reads=[wdr], writes=[bsT_b])
            xt = [mk.sb("xt", [128, 1024], F32) for _ in range(2)]
            uu, uu_b = mk.sb("uu", [128, 2048], BF16)
            vv, vv_b = mk.sb("vv", [128, 2048], F32)
            vn, vn_b = mk.sb("vn", [128, 2048], BF16)
            pp, pp_b = mk.sb("pp", [128, 2048], BF16)
            ppT, ppT_b = mk.sb("ppT", [128, 16, 128], BF16)
            st6, st6_b = mk.sb("st6", [128, 4, 6], F32)
            mv, mv_b = mk.sb("mv", [128, 2], F32)
            rstd, rstd_b = mk.sb("rstd", [128, 1], F32)
            xo_t = [mk.sb("xo_t", [128, 1024], F32) for _ in range(1)]
            zps = [mk.ps("zps", [128, 512], F32) for _ in range(4)]
            mps_ = [mk.ps("mps2", [128, 512], F32) for _ in range(2)]
            psT, psT_b = mk.ps("psT", [128, 8, 128], BF16)
            dma(xt[0][0][:], xb[0:128, :], xt[0][1], reads=[xb_b], writes=[xt[0][1]])
            for t in range(NT):
                par = t % 2
                if t + 1 < NT:
                    dma(xt[1 - par][0][:], xb[(t + 1) * 128:(t + 2) * 128, :], xt[1 - par][1], reads=[xb_b],
                        writes=[xt[1 - par][1]])
                gm, gm_b, sh, sh_b, gt, gt_b = mods[seq_of(t)]
                x_t, x_b = xt[par]
                hT, hT_b, _, _ = norm_mod_T(nb, par, x_t[:], x_b, gm, gm_b, sh, sh_b)
                for cb in range(8):
                    zp, zp_b = zps[cb % 4]
                    op("pe", lambda p: p.matmul(zp[:], lhsT=ones1[0:1, :], rhs=bb16[0:1, cb * 512:(cb + 1) * 512],
                                                start=True, stop=False), reads=[ones1_b, bb16_b], writes=[zp_b], inc=False)
                    for k in range(8):
                        op("pe", lambda p: p.matmul(zp[:], lhsT=hT[:, k, :], rhs=Wg[:, k, cb * 512:(cb + 1) * 512],
                                                    start=False, stop=(k == 7)),
                           reads=[hT_b, Wg_b], writes=[zp_b], inc=(k == 7))
                    if cb < 4:
                        op("act", lambda a: a.activation(out=uu[:, cb * 512:(cb + 1) * 512], in_=zp[:], func=AF.Gelu),
                           reads=[zp_b], writes=[uu_b])
                    else:
                        c = cb - 4
                        op("act", lambda a: a.activation(out=vv[:, c * 512:(c + 1) * 512], in_=zp[:], func=AF.Gelu),
                           reads=[zp_b], writes=[vv_b])
                        op("dve", lambda v: v.bn_stats(out=st6[:, c, :], in_=vv[:, c * 512:(c + 1) * 512]),
                           reads=[vv_b], writes=[st6_b])
                op("dve", lambda v: v.bn_aggr(out=mv[:], in_=st6[:]), reads=[st6_b], writes=[mv_b])
                op("dve", lambda v: v.tensor_scalar(out=rstd[:], in0=mv[:, 1:2], scalar1=EPS, scalar2=None, op0=ALU.add),
                   reads=[mv_b], writes=[rstd_b])
                op("act", lambda a: a.sqrt(out=rstd[:], in_=rstd[:]), reads=[rstd_b], writes=[rstd_b])
                op("dve", lambda v: v.reciprocal(out=rstd[:], in_=rstd[:]), reads=[rstd_b], writes=[rstd_b])
                op("dve", lambda v: v.tensor_scalar(out=vv[:], in0=vv[:], scalar1=mv[:, 0:1], scalar2=rstd[:, 0:1],
                                                    op0=ALU.subtract, op1=ALU.mult), reads=[vv_b, mv_b, rstd_b], writes=[vv_b])
                op("pool", lambda g: g.tensor_tensor(out=vv[:], in0=vv[:], in1=vng[:], op=ALU.mult),
                   reads=[vv_b, vng_b], writes=[vv_b])
                op("pool", lambda g: g.tensor_tensor(out=vn[:], in0=vv[:], in1=vnb[:], op=ALU.add),
                   reads=[vv_b, vnb_b], writes=[vn_b])
                for g_ in range(4):
                    mp, mp_b = mps_[g_ % 2]
                    op("pe", lambda p: p.matmul(mp[:], lhsT=wsT[:, g_, :], rhs=vn[:, g_ * 512:(g_ + 1) * 512], start=True, stop=True),
                       reads=[wsT_b, vn_b], writes=[mp_b])
                    op("dve", lambda v: v.scalar_tensor_tensor(out=pp[:, g_ * 512:(g_ + 1) * 512], in0=mp[:], scalar=bsT[:, g_:g_ + 1],
                                                              in1=uu[:, g_ * 512:(g_ + 1) * 512], op0=ALU.add, op1=ALU.mult),
                       reads=[mp_b, bsT_b, uu_b], writes=[pp_b])
                for half in range(2):
                    for k in range(8):
                        kk = half * 8 + k
                        op("pe", lambda p: p.transpose(out=psT[:, k, :], in_=pp[:, kk * 128:(kk + 1) * 128], identity=ident[:]),
                           reads=[pp_b, ident_b], writes=[psT_b], inc=(k == 7))
                    op("act", lambda a: a.copy(out=ppT[:, half * 8:(half + 1) * 8, :], in_=psT[:]), reads=[psT_b], writes=[ppT_b])
                xo_, xo_b = xo_t[0]
                for cb in range(2):
                    mp, mp_b = mps_[cb]
                    op("pe", lambda p: p.matmul(mp[:], lhsT=ones1[0:1, :], rhs=bb16[0:1, 4096 + cb * 512:4096 + (cb + 1) * 512],
                                                start=True, stop=False), reads=[ones1_b, bb16_b], writes=[mp_b], inc=False)
                    for k in range(16):
                        op("pe", lambda p: p.matmul(mp[:], lhsT=ppT[:, k, :], rhs=Wgo[:, k, cb * 512:(cb + 1) * 512],
                                                    start=False, stop=(k == 15)),
                           reads=[ppT_b, Wgo_b], writes=[mp_b], inc=(k == 15))
                    op("dve", lambda v: v.tensor_tensor(out=xo_[:, cb * 512:(cb + 1) * 512], in0=mp[:],
                                                        in1=gt[:, cb * 512:(cb + 1) * 512], op=ALU.mult),
                       reads=[mp_b, gt_b], writes=[xo_b])
                op("pool", lambda g: g.tensor_tensor(out=x_t[:], in0=xo_[:], in1=x_t[:], op=ALU.add),
                   reads=[xo_b, x_b], writes=[x_b])
                dma(xa[t * 128:(t + 1) * 128, :], x_t[:], x_b, reads=[x_b], writes=[xa_b])
            mk.pop()

            check("G")
            moe_layer(1, xa, xa_b, xb, xb_b)
            check("E1")

            mk.push()
            fg, fg_b = mk.sb("fg", [128, 1024], F32)
            load_bc(fg[:], fg_b, final_g[0:1, :])
            xt = [mk.sb("xt", [128, 1024], F32) for _ in range(2)]
            yt = [mk.sb("yt", [128, 1024], F32) for _ in range(2)]
            junk, junk_b = mk.sb("junk", [128, 1024], F32)
            ss2 = [mk.sb("ss", [128, 1], F32) for _ in range(2)]
            dma(xt[0][0][:], xb[0:128, :], xt[0][1], reads=[xb_b], writes=[xt[0][1]])
            for t in range(NT):
                par = t % 2
                if t + 1 < NT:
                    dma(xt[1 - par][0][:], xb[(t + 1) * 128:(t + 2) * 128, :], xt[1 - par][1], reads=[xb_b],
                        writes=[xt[1 - par][1]])
                x_t, x_b = xt[par]
                y_t, y_b = yt[par]
                ss, ss_b = ss2[par]
                op("act", lambda a: a.activation(out=junk[:], in_=x_t[:], func=AF.Square, accum_out=ss[:]),
                   reads=[x_b], writes=[junk_b, ss_b])
                op("dve", lambda v: v.tensor_scalar(out=ss[:], in0=ss[:], scalar1=1.0 / D, scalar2=EPS,
                                                    op0=ALU.mult, op1=ALU.add), reads=[ss_b], writes=[ss_b])
                op("act", lambda a: a.sqrt(out=ss[:], in_=ss[:]), reads=[ss_b], writes=[ss_b])
                op("dve", lambda v: v.reciprocal(out=ss[:], in_=ss[:]), reads=[ss_b], writes=[ss_b])
                op("dve", lambda v: v.scalar_tensor_tensor(out=y_t[:], in0=x_t[:], scalar=ss[:, 0:1], in1=fg[:],
                                                          op0=ALU.mult, op1=ALU.mult),
                   reads=[x_b, ss_b, fg_b], writes=[y_b])
                dma(y_out[t * 128:(t + 1) * 128, :], y_t[:], y_b, reads=[y_b], writes=[yb_])
            mk.pop()

        except StopBuild:
            while len(mk.stacks) > 1:
                mk.pop()
        mk.finish()
        build.stats = (mk.n_inst, mk.n_wait)
    return nc


def prepare(cfg, inp):
    NC, TS, TP, NE, DE = cfg.NC, cfg.TS, cfg.TP, cfg.NE, cfg.DE
    f = lambda a: np.ascontiguousarray(np.asarray(a, dtype=np.float32))
    xp = f(inp["x_prompt"])[0]
    xsm = f(inp["x_sample"])
    cp = f(inp["c_prompt"])[0]
    csm = f(inp["c_sample"])
    PT = TP * 128
    S = xp.shape[0]
    perm = np.concatenate([np.arange(0, 2 * DE, 2), np.arange(1, 2 * DE, 2)])
    shared = {
        "ctab": make_ctab(),
        "ada_w": f(inp["ada_w"]).reshape(2 * 1024, 6144),
        "ada_b": f(inp["ada_b"]),
        "norm1_g": f(inp["norm1_g"]), "norm2_g": f(inp["norm2_g"]),
        "ret_w_in": f(inp["ret_w_in"])[0],
        "ret_ld": f(inp["ret_log_decay"]).reshape(1, 8),
        "ret_gn_g": f(inp["ret_gn_g"]).reshape(1, 2048),
        "ret_w_out": f(inp["ret_w_out"])[0],
        "gm_w_in": f(inp["gm_w_in"])[0],
        "gm_b_in": f(inp["gm_b_in"]).reshape(1, 4096),
        "gm_vn_g": f(inp["gm_vn_g"]).reshape(1, 2048),
        "gm_vn_b": f(inp["gm_vn_b"]).reshape(1, 2048),
        "gm_w_sT": f(np.transpose(f(inp["gm_w_s"])[0], (0, 2, 1))).reshape(4 * 128, 128),
        "gm_b_sT": f(f(inp["gm_b_s"])[0].T),
        "gm_w_out": f(inp["gm_w_out"])[0],
        "gm_b_out": f(inp["gm_b_out"]).reshape(1, 1024),
        "moe_w_r": f(inp["moe_w_r"]).reshape(2 * 1024, NE),
        "moe_b_r": f(inp["moe_b_r"]),
        "moe_w_gu": f(f(inp["moe_w_gu"])[..., perm].reshape(2, NE, 8, 128, 2 * DE).transpose(0, 1, 3, 2, 4)).reshape(2 * NE * 128, 8 * 2 * DE),
        "moe_b_gu": f(f(inp["moe_b_gu"])[..., perm]).reshape(2 * NE, 2 * DE),
        "moe_w_dn": f(f(inp["moe_w_dn"]).reshape(2, NE, DE // 128, 128, 1024).transpose(0, 1, 3, 2, 4)).reshape(2 * NE * 128, (DE // 128) * 1024),
        "moe_b_dn": f(inp["moe_b_dn"]).reshape(2 * NE, 1024),
        "final_g": f(inp["final_g"]).reshape(1, 1024),
    }
    pos_s = np.arange(TS * 128)
    in_maps = []
    for c in range(NC):
        a, b = c * PT, (c + 1) * PT
        m = dict(shared)
        m["xs"] = np.concatenate([xsm[c], xp[a:b]], axis=0)
        if NC > 1:
            m["xo"] = np.concatenate([xp[:a], xp[b:]], axis=0)
            pos_o = np.concatenate([np.arange(0, a), np.arange(b, S)])
            meta = np.zeros((pos_o.shape[0], 4), np.float32)
            bef = pos_o < a
            meta[bef, 0] = (a - 1 - pos_o[bef])
            meta[bef, 1] = 1.0
            meta[~bef, 2] = (pos_o[~bef] - b)
            meta[~bef, 3] = 1.0
        else:
            m["xo"] = np.zeros((128, 1024), np.float32)
            pos_o = np.zeros(128)
            meta = np.zeros((128, 4), np.float32)
        m["oth_meta"] = meta
        m["rope_oth"] = rope_table(pos_o)
        m["rope_own"] = rope_table(np.concatenate([pos_s, np.arange(a, b)]))
        cc = np.stack([csm[c], cp], axis=-1)
        m["ccol"] = f(cc.reshape(8, 128, 2).transpose(1, 0, 2).reshape(128, 16))
        in_maps.append(m)
    return in_maps


def assemble(cfg, results):
    TS, TP = cfg.TS, cfg.TP
    ys = [np.asarray(r["y"], dtype=np.float32) for r in results]
    y_sample = np.stack([y[:TS * 128] for y in ys], axis=0)
    y_prompt = np.concatenate([y[TS * 128:] for y in ys], axis=0)[None]
    return y_prompt, y_sample


_CACHE = {}


def run(cfg, inputs):
    key = (cfg.NC, cfg.TS, cfg.TP, cfg.NE, cfg.DE)
    if key not in _CACHE:
        _CACHE[key] = build(cfg)
    nc = _CACHE[key]
    in_maps = prepare(cfg, inputs)
    res = run_bass_kernel_spmd(nc, in_maps, core_ids=list(range(cfg.NC)))
    return assemble(cfg, res.results)


def kernel(**inputs):
    cfg = Cfg()
    return run(cfg, inputs)
```

```python
import numpy as np
from contextlib import ExitStack
import concourse.bass as bass
import concourse.mybir as mybir
from concourse.bass_utils import run_bass_kernel_spmd

F32 = mybir.dt.float32
BF16 = mybir.dt.bfloat16
ALU = mybir.AluOpType
AF = mybir.ActivationFunctionType
EPS = 1e-6


class Buf:
    __slots__ = ("name", "w", "r", "sem", "cnt", "dram_tokens", "scope")

    def __init__(self, name, dram=False):
        self.name = name
        self.scope = 0
        self.w = None
        self.r = {}
        self.sem = None
        self.cnt = 0
        self.dram_tokens = {} if dram else None


class Eng:
    def __init__(self, name, inst, sem):
        self.name = name
        self.inst = inst
        self.sem = sem
        self.cnt = 0
        self.seen = {}
        self.pend_r = []
        self.pend_w = []


class MK:
    def __init__(self, nc, stack):
        self.nc = nc
        self.stacks = [stack]
        self.engs = {}
        for name, inst in (("pe", nc.tensor), ("act", nc.scalar), ("dve", nc.vector),
                           ("pool", nc.gpsimd), ("sp", nc.sync)):
            sem = stack.enter_context(nc.semaphore("s_" + name))
            self.engs[name] = Eng(name, inst, sem)
        self.dma_bufs = []
        self.scope_dma = [[]]
        self.sem_pool = []
        self.dbg = False
        self.rec = None
        self.window = 6
        self.n_inst = 0
        self.n_wait = 0
        self.uid = 0

    def push(self):
        st = ExitStack()
        st.__enter__()
        self.stacks.append(st)
        self.scope_dma.append([])

    def pop(self):
        self.barrier()
        st = self.stacks.pop()
        for b in self.scope_dma.pop():
            self.dma_bufs.remove(b)
            self.sem_pool.append((b.sem, b.cnt))
        st.__exit__(None, None, None)

    def sb(self, name, shape, dtype):
        self.uid += 1
        t = self.stacks[-1].enter_context(self.nc.sbuf_tensor(f"{name}_{self.uid}", list(shape), dtype))
        b = Buf(name)
        b.scope = len(self.stacks) - 1
        return t, b

    def ps(self, name, shape, dtype=F32):
        self.uid += 1
        t = self.stacks[-1].enter_context(self.nc.psum_tensor(f"{name}_{self.uid}", list(shape), dtype))
        return t, Buf(name)

    def dram(self, name, shape, dtype):
        t = self.nc.dram_tensor(name, list(shape), dtype, kind=("ExternalOutput" if self.dbg else "Internal"))
        return t.ap(), Buf(name, dram=True)

    def _dsem(self, b):
        if b.sem is None:
            self.uid += 1
            if self.sem_pool:
                b.sem, b.cnt = self.sem_pool.pop()
            else:
                b.sem = self.stacks[0].enter_context(self.nc.semaphore(f"d_{b.name}_{self.uid}"))
            self.dma_bufs.append(b)
            self.scope_dma[b.scope].append(b)
        return b.sem

    def _wait(self, e, tokens):
        best = {}
        for tok in tokens:
            if tok is None:
                continue
            sem, val, owner = tok
            if owner == e.name:
                if e.name == "pe" or val <= e.cnt - self.window:
                    continue
            k = id(sem)
            if k not in best or best[k][1] < val:
                best[k] = (sem, val)
        for k, (sem, val) in best.items():
            if e.seen.get(k, 0) >= val:
                continue
            e.inst.wait_ge(sem, val)
            e.seen[k] = val
            self.n_wait += 1
            if self.rec is not None:
                self.rec[e.name].append(("w", k, val))

    @staticmethod
    def _deps(reads, writes):
        toks = []
        for b in reads:
            toks.append(b.w)
            if b.dram_tokens:
                toks.extend(b.dram_tokens.values())
        for b in writes:
            toks.append(b.w)
            toks.extend(b.r.values())
            if b.dram_tokens:
                toks.extend(b.dram_tokens.values())
        return toks

    @staticmethod
    def _commit(tok, key, reads, writes):
        for b in writes:
            if b.dram_tokens is not None:
                b.dram_tokens[id(tok[0])] = tok
            else:
                b.w = tok
            b.r = {}
        for b in reads:
            b.r[key] = tok

    def op(self, eng, fn, reads=(), writes=(), inc=True):
        e = self.engs[eng]
        self._wait(e, self._deps(reads, writes))
        ins = fn(e.inst)
        self.n_inst += 1
        if not inc:
            e.pend_r.extend(reads)
            e.pend_w.extend(writes)
            return ins
        e.cnt += 1
        ins.then_inc(e.sem, 1)
        if self.rec is not None:
            self.rec[e.name].append(("i", id(e.sem), 1, self.n_inst))
        tok = (e.sem, e.cnt, e.name)
        self._commit(tok, e.name, list(reads) + e.pend_r, list(writes) + e.pend_w)
        e.pend_r = []
        e.pend_w = []
        return ins

    def dma(self, out, in_, sbuf_side, reads=(), writes=(), q="sp", fn=None):
        e = self.engs[q]
        self._wait(e, self._deps(reads, writes))
        sem = self._dsem(sbuf_side)
        ins = e.inst.dma_start(out=out, in_=in_) if fn is None else fn(e.inst)
        sbuf_side.cnt += 16
        ins.then_inc(sem, 16)
        if self.rec is not None:
            self.rec[e.name].append(("i", id(sem), 16, self.n_inst))
        tok = (sem, sbuf_side.cnt, None)
        self._commit(tok, ("dma", id(sem)), reads, writes)
        self.n_inst += 1
        return ins

    def barrier(self):
        for e in self.engs.values():
            assert not e.pend_r and not e.pend_w
        for e in self.engs.values():
            for o in self.engs.values():
                if o is e or o.cnt == 0:
                    continue
                if e.seen.get(id(o.sem), 0) < o.cnt:
                    e.inst.wait_ge(o.sem, o.cnt)
                    e.seen[id(o.sem)] = o.cnt
            for b in self.dma_bufs:
                if b.cnt and e.seen.get(id(b.sem), 0) < b.cnt:
                    e.inst.wait_ge(b.sem, b.cnt)
                    e.seen[id(b.sem)] = b.cnt

    def finish(self):
        self.barrier()


class Cfg:
    def __init__(self, NC=8, TS=64, TP=16, NE=32, DE=1024):
        self.NC, self.TS, self.TP, self.NE, self.DE = NC, TS, TP, NE, DE
        self.D = 1024
        self.NT = TS + TP
        self.NO = TP * (NC - 1)
        self.ST = 4 if self.NT % 4 == 0 else (2 if self.NT % 2 == 0 else 1)


CT_EF, CT_MF, CT_EB, CT_MB, CT_IP1, CT_CMI, CT_C127, CT_PIDX, CT_IO, CT_U, CT_W = 0, 128, 256, 384, 512, 640, 768, 769, 770, 898, 1026


def make_ctab():
    i = np.arange(128, dtype=np.float32)
    j = i[:, None]
    ii = i[None, :]
    t = np.zeros((128, CT_W), np.float32)
    t[:, CT_EF:CT_EF + 128] = np.maximum(ii - j, 0)
    t[:, CT_MF:CT_MF + 128] = (ii >= j)
    t[:, CT_EB:CT_EB + 128] = np.maximum(j - ii, 0)
    t[:, CT_MB:CT_MB + 128] = (j > ii)
    t[:, CT_IP1:CT_IP1 + 128] = ii + 1
    t[:, CT_CMI:CT_CMI + 128] = 128 - ii
    t[:, CT_C127] = 127 - i
    t[:, CT_PIDX] = i
    t[:, CT_IO:CT_IO + 128] = ii
    t[:, CT_U:CT_U + 128] = (j < ii)
    return t


def rope_table(pos):
    half = 128
    inv = (np.float32(10000.0) ** (-np.arange(half, dtype=np.float32) / np.float32(half))).astype(np.float32)
    ang = (pos.astype(np.float32)[:, None] * inv[None, :]).astype(np.float32)
    return np.concatenate([np.cos(ang), np.sin(ang)], axis=1).astype(np.float32)


class StopBuild(Exception):
    pass


def build(cfg, dbg=False, stop_after=None):
    nc = bass.Bass("TRN2", target_bir_lowering=False)

    def check(name):
        if stop_after == name:
            raise StopBuild()
    D, NT, NO, NE, DE, TS, TP = cfg.D, cfg.NT, cfg.NO, cfg.NE, cfg.DE, cfg.TS, cfg.TP

    def IN(name, shape):
        return nc.dram_tensor(name, list(shape), F32, kind="ExternalInput").ap()

    xs = IN("xs", [NT * 128, D])
    xo = IN("xo", [max(NO, 1) * 128, D])
    ccol = IN("ccol", [128, 16])
    rope_own = IN("rope_own", [NT * 128, 256])
    rope_oth = IN("rope_oth", [max(NO, 1) * 128, 256])
    oth_meta = IN("oth_meta", [max(NO, 1) * 128, 4])
    ctab_d = IN("ctab", [128, CT_W])
    ada_w = IN("ada_w", [2 * 1024, 6144])
    ada_b = IN("ada_b", [2, 6144])
    norm1_g = IN("norm1_g", [2, 1024])
    norm2_g = IN("norm2_g", [2, 1024])
    ret_w_in = IN("ret_w_in", [1024, 6144])
    ret_ld = IN("ret_ld", [1, 8])
    ret_gn_g = IN("ret_gn_g", [1, 2048])
    ret_w_out = IN("ret_w_out", [2048, 1024])
    gm_w_in = IN("gm_w_in", [1024, 4096])
    gm_b_in = IN("gm_b_in", [1, 4096])
    gm_vn_g = IN("gm_vn_g", [1, 2048])
    gm_vn_b = IN("gm_vn_b", [1, 2048])
    gm_w_sT = IN("gm_w_sT", [4 * 128, 128])
    gm_b_sT = IN("gm_b_sT", [128, 4])
    gm_w_out = IN("gm_w_out", [2048, 1024])
    gm_b_out = IN("gm_b_out", [1, 1024])
    moe_w_r = IN("moe_w_r", [2 * 1024, NE])
    moe_b_r = IN("moe_b_r", [2, NE])
    moe_w_gu = IN("moe_w_gu", [2 * NE * 128, 8 * 2 * DE])
    moe_b_gu = IN("moe_b_gu", [2 * NE, 2 * DE])
    moe_w_dn = IN("moe_w_dn", [2 * NE * 128, (DE // 128) * 1024])
    moe_b_dn = IN("moe_b_dn", [2 * NE, 1024])
    final_g = IN("final_g", [1, 1024])
    y_out = nc.dram_tensor("y", [NT * 128, D], F32, kind="ExternalOutput").ap()

    KE = DE // 128
    GB = (2 * DE) // 512
    GH = GB // 2

    with ExitStack() as root:
        mk = MK(nc, root)
        mk.dbg = dbg
        if getattr(build, "record", False):
            mk.rec = {n: [] for n in mk.engs}
            build.rec = mk.rec
        op, dma = mk.op, mk.dma
        try:
          if True:

            modv, modv_b = mk.dram("modv", [4, 6144], F32)
            r1, r1_b = mk.dram("r1", [NT * 128, 7168], BF16)
            osc, osc_b = mk.dram("osc", [max(NO, 1) * 128, 4096], BF16)
            sbst, sbst_b = mk.dram("sbst", [NT * 128, 4096], BF16)
            xa, xa_b = mk.dram("xa", [NT * 128, D], F32)
            xb, xb_b = mk.dram("xb", [NT * 128, D], F32)
            h2d, h2d_b = mk.dram("h2d", [NT * 128, D], BF16)
            xsb = Buf("xs_in", dram=True)
            xob = Buf("xo_in", dram=True)
            wdr = Buf("weights_in", dram=True)
            yb_ = Buf("y_out", dram=True)

            ident, ident_b = mk.sb("ident", [128, 128], BF16)
            identf, identf_b = mk.sb("identf", [128, 128], F32)
            ones1, ones1_b = mk.sb("ones1", [1, 128], BF16)
            ctab, ctab_b = mk.sb("ctab", [128, CT_W], F32)
            lg, lg_b = mk.sb("lg", [128, 8], F32)
            cdec, cdec_b = mk.sb("cdec", [128, 8], F32)

            dma(ctab[:], ctab_d[:, :], ctab_b, reads=[wdr], writes=[ctab_b])
            dma(lg[:], ret_ld[0:1, :].partition_broadcast(128), lg_b, reads=[wdr], writes=[lg_b])
            op("pool", lambda g: g.memset(identf[:], 0.0), writes=[identf_b])
            op("pool", lambda g: g.affine_select(out=identf[:], in_=identf[:], pattern=[[-1, 128]],
                                                 compare_op=ALU.not_equal, fill=1.0, base=0, channel_multiplier=1),
               reads=[identf_b], writes=[identf_b])
            op("pool", lambda g: g.tensor_copy(out=ident[:], in_=identf[:]), reads=[identf_b], writes=[ident_b])
            op("pool", lambda g: g.memset(ones1[:], 1.0), writes=[ones1_b])
            op("act", lambda a: a.activation(out=cdec[:], in_=lg[:], func=AF.Exp, scale=128.0),
               reads=[lg_b], writes=[cdec_b])

            def seq_of(t):
                return 0 if t < TS else 1

            def load_bc(dst, dst_b, src_row):
                dma(dst, src_row.partition_broadcast(128), dst_b, reads=[wdr, modv_b], writes=[dst_b])

            def make_mod(l, sub, want_gate, want_norm=True):
                ng_src = (norm1_g if sub == 0 else norm2_g)
                res = []
                if want_norm:
                    ng, ng_b = mk.sb("ng", [128, 1024], F32)
                    load_bc(ng[:], ng_b, ng_src[l:l + 1, :])
                for s in range(2):
                    row = l * 2 + s
                    gm = gm_b = sh = sh_b = None
                    if want_norm:
                        gm, gm_b = mk.sb("gm", [128, 1024], F32)
                        sh, sh_b = mk.sb("sh", [128, 1024], F32)
                        load_bc(sh[:], sh_b, modv[row:row + 1, (3 * sub) * 1024:(3 * sub + 1) * 1024])
                        load_bc(gm[:], gm_b, modv[row:row + 1, (3 * sub + 1) * 1024:(3 * sub + 2) * 1024])
                        op("dve", lambda v: v.scalar_tensor_tensor(out=gm[:], in0=gm[:], scalar=1.0, in1=ng[:],
                                                                  op0=ALU.add, op1=ALU.mult),
                           reads=[gm_b, ng_b], writes=[gm_b])
                    gt = gt_b = None
                    if want_gate:
                        gt, gt_b = mk.sb("gt", [128, 1024], F32)
                        load_bc(gt[:], gt_b, modv[row:row + 1, (3 * sub + 2) * 1024:(3 * sub + 3) * 1024])
                    res.append((gm, gm_b, sh, sh_b, gt, gt_b))
                return res

            class NormBufs:
                def __init__(self):
                    self.ss = [mk.sb("ss", [128, 1], F32) for _ in range(2)]
                    _tt = mk.sb("tt", [128, 1024], F32)
                    self.tt = [_tt, _tt]
                    self.hh = [mk.sb("hh", [128, 1024], BF16) for _ in range(2)]
                    self.hT = [mk.sb("hT", [128, 8, 128], BF16) for _ in range(2)]
                    self.hTp, self.hTp_b = mk.ps("hTp", [128, 8, 128], BF16)

            def norm_mod_T(nb, par, x_t, x_b, gm, gm_b, sh, sh_b, out_hT=None):
                ss, ss_b = nb.ss[par]
                tt, tt_b = nb.tt[par]
                hh, hh_b = nb.hh[par]
                hT, hT_b = nb.hT[par] if out_hT is None else out_hT
                op("act", lambda a: a.activation(out=tt[:], in_=x_t, func=AF.Square, accum_out=ss[:]),
                   reads=[x_b], writes=[tt_b, ss_b])
                op("dve", lambda v: v.tensor_scalar(out=ss[:], in0=ss[:], scalar1=1.0 / D, scalar2=EPS,
                                                    op0=ALU.mult, op1=ALU.add), reads=[ss_b], writes=[ss_b])
                op("act", lambda a: a.sqrt(out=ss[:], in_=ss[:]), reads=[ss_b], writes=[ss_b])
                op("dve", lambda v: v.reciprocal(out=ss[:], in_=ss[:]), reads=[ss_b], writes=[ss_b])
                op("dve", lambda v: v.scalar_tensor_tensor(out=tt[:], in0=x_t, scalar=ss[:, 0:1], in1=gm[:],
                                                          op0=ALU.mult, op1=ALU.mult),
                   reads=[x_b, ss_b, gm_b], writes=[tt_b])
                op("dve", lambda g: g.tensor_tensor(out=hh[:], in0=tt[:], in1=sh[:], op=ALU.add),
                   reads=[tt_b, sh_b], writes=[hh_b])
                for k in range(8):
                    op("pe", lambda p: p.transpose(out=nb.hTp[:, k, :], in_=hh[:, k * 128:(k + 1) * 128], identity=ident[:]),
                       reads=[hh_b, ident_b], writes=[nb.hTp_b], inc=(k == 7))
                op("act", lambda a: a.copy(out=hT[:], in_=nb.hTp[:]), reads=[nb.hTp_b], writes=[hT_b])
                return hT, hT_b, hh, hh_b

            def load_weight_bf16(dst, dst_b, src, rows, cols, stg_list, col_chunk):
                i = 0
                for kc in range(rows // 128):
                    for c0 in range(0, cols, col_chunk):
                        stg, stg_b = stg_list[i % len(stg_list)]
                        i += 1
                        cw = min(col_chunk, cols - c0)
                        dma(stg[:, 0:cw], src[kc * 128:(kc + 1) * 128, c0:c0 + cw], stg_b, reads=[wdr], writes=[stg_b])
                        op("pool", lambda g: g.tensor_copy(out=dst[:, kc, c0:c0 + cw], in_=stg[:, 0:cw]),
                           reads=[stg_b], writes=[dst_b])

            mk.push()
            cact, cact_b = mk.sb("cact", [128, 16], F32)
            adab, adab_b = mk.sb("adab", [2, 6144], F32)
            modsb, modsb_b = mk.sb("modsb", [2, 6144], F32)
            wst = [mk.sb("wst", [128, 3072], F32) for _ in range(2)]
            mps = [mk.ps("mps", [128, 512], F32) for _ in range(6)]
            dma(cact[:], ccol[:, :], cact_b, reads=[wdr], writes=[cact_b])
            op("act", lambda a: a.activation(out=cact[:], in_=cact[:], func=AF.Silu), reads=[cact_b], writes=[cact_b])
            for l in range(2):
                for s in range(2):
                    dma(adab[s:s + 1, :], ada_b[l:l + 1, :], adab_b, reads=[wdr], writes=[adab_b])
                for half in range(2):
                    for k in range(8):
                        w, w_b = wst[k % 2]
                        dma(w[:], ada_w[l * 1024 + k * 128: l * 1024 + (k + 1) * 128, half * 3072:(half + 1) * 3072],
                            w_b, reads=[wdr], writes=[w_b])
                        for b in range(6):
                            op("pe", lambda p: p.matmul(mps[b][0][0:2, :], lhsT=cact[:, 2 * k:2 * k + 2],
                                                        rhs=w[:, b * 512:(b + 1) * 512], start=(k == 0), stop=(k == 7)),
                               reads=[cact_b, w_b], writes=[mps[b][1]], inc=(b == 5))
                    for b in range(6):
                        c0 = half * 3072 + b * 512
                        op("dve", lambda v: v.tensor_tensor(out=modsb[:, c0:c0 + 512], in0=mps[b][0][0:2, :],
                                                            in1=adab[:, c0:c0 + 512], op=ALU.add),
                           reads=[mps[b][1], adab_b], writes=[modsb_b])
                dma(modv[2 * l:2 * l + 2, :], modsb[:], modsb_b, reads=[modsb_b], writes=[modv_b])
            mk.pop()
            check("M")

            mk.push()
            Win, Win_b = mk.sb("Win", [128, 8, 6144], BF16)
            mk.push()
            stg = [mk.sb("stg", [128, 3072], F32) for _ in range(2)]
            load_weight_bf16(Win, Win_b, ret_w_in, 1024, 6144, stg, 3072)
            mk.pop()
            mods = make_mod(0, 0, False)
            nb = NormBufs()
            xt = [mk.sb("xt", [128, 1024], F32) for _ in range(2)]
            cst = [mk.sb("cst", [128, 256], F32) for _ in range(2)]
            ra, ra_b = mk.sb("ra", [128, 256], F32)
            rb, rb_b = mk.sb("rb", [128, 256], F32)
            rc, rc_b = mk.sb("rc", [128, 256], F32)
            rd, rd_b = mk.sb("rd", [128, 256], F32)
            qr, qr_b = mk.sb("qr", [128, 1024], BF16)
            pj = [mk.ps("pj", [128, 512], F32) for _ in range(4)]
            qTp, qTp_b = mk.ps("qTp", [128, 8, 128], BF16)
            kTp, kTp_b = mk.ps("kTp", [128, 8, 128], BF16)
            pjn = [0]

            def proj_block(hT, hT_b, cb):
                ps, ps_b = pj[pjn[0] % 4]
                pjn[0] += 1
                for k in range(8):
                    op("pe", lambda p: p.matmul(ps[:], lhsT=hT[:, k, :], rhs=Win[:, k, cb * 512:(cb + 1) * 512],
                                                start=(k == 0), stop=(k == 7)),
                       reads=[hT_b, Win_b], writes=[ps_b], inc=(k == 7))
                return ps, ps_b

            def rope_block(ps, ps_b, cs, cs_b, dst, dst_b, c0):
                pv = ps[:].rearrange("p (h t d) -> p h t d", h=2, t=2)
                dv = dst[:, c0:c0 + 512].rearrange("p (h t d) -> p h t d", h=2, t=2)
                cosb = cs[:, 0:128].unsqueeze(1).to_broadcast([128, 2, 128])
                sinb = cs[:, 128:256].unsqueeze(1).to_broadcast([128, 2, 128])
                v3 = lambda t_: t_[:].rearrange("p (h d) -> p h d", h=2)
                op("dve", lambda v: v.tensor_tensor(out=v3(ra), in0=pv[:, :, 0, :], in1=cosb, op=ALU.mult),
                   reads=[ps_b, cs_b], writes=[ra_b])
                op("dve", lambda v: v.tensor_tensor(out=v3(rb), in0=pv[:, :, 1, :], in1=sinb, op=ALU.mult),
                   reads=[ps_b, cs_b], writes=[rb_b])
                op("dve", lambda v: v.tensor_tensor(out=v3(rc), in0=pv[:, :, 1, :], in1=cosb, op=ALU.mult),
                   reads=[ps_b, cs_b], writes=[rc_b])
                op("dve", lambda v: v.tensor_tensor(out=v3(rd), in0=pv[:, :, 0, :], in1=sinb, op=ALU.mult),
                   reads=[ps_b, cs_b], writes=[rd_b])
                op("dve", lambda g: g.tensor_tensor(out=dv[:, :, 0, :], in0=v3(ra), in1=v3(rb), op=ALU.subtract),
                   reads=[ra_b, rb_b], writes=[dst_b])
                op("dve", lambda g: g.tensor_tensor(out=dv[:, :, 1, :], in0=v3(rc), in1=v3(rd), op=ALU.add),
                   reads=[rc_b, rd_b], writes=[dst_b])

            mk.push()
            so = [mk.sb("so", [128, 7168], BF16) for _ in range(2)]
            dma(xt[0][0][:], xs[0:128, :], xt[0][1], reads=[xsb], writes=[xt[0][1]])
            dma(cst[0][0][:], rope_own[0:128, :], cst[0][1], reads=[wdr], writes=[cst[0][1]])
            for t in range(NT):
                par = t % 2
                if t + 1 < NT:
                    dma(xt[1 - par][0][:], xs[(t + 1) * 128:(t + 2) * 128, :], xt[1 - par][1], reads=[xsb], writes=[xt[1 - par][1]])
                    dma(cst[1 - par][0][:], rope_own[(t + 1) * 128:(t + 2) * 128, :], cst[1 - par][1], reads=[wdr],
                        writes=[cst[1 - par][1]])
                s = seq_of(t)
                gm, gm_b, sh, sh_b, _, _ = mods[s]
                x_t, x_b = xt[par]
                cs, cs_b = cst[par]
                o, o_b = so[par]
                hT, hT_b, _, _ = norm_mod_T(nb, par, x_t[:], x_b, gm, gm_b, sh, sh_b)
                for cb in range(2):
                    ps, ps_b = proj_block(hT, hT_b, cb)
                    rope_block(ps, ps_b, cs, cs_b, qr, qr_b, cb * 512)
                for k in range(8):
                    op("pe", lambda p: p.transpose(out=qTp[:, k, :], in_=qr[:, k * 128:(k + 1) * 128], identity=ident[:]),
                       reads=[qr_b, ident_b], writes=[qTp_b], inc=(k == 7))
                op("act", lambda a: a.copy(out=o[:, 0:1024].rearrange("p (k i) -> p k i", k=8), in_=qTp[:]),
                   reads=[qTp_b], writes=[o_b])
                for cb in range(2):
                    ps, ps_b = proj_block(hT, hT_b, 2 + cb)
                    rope_block(ps, ps_b, cs, cs_b, o, o_b, 2048 + cb * 512)
                for k in range(8):
                    op("pe", lambda p: p.transpose(out=kTp[:, k, :], in_=o[:, 2048 + k * 128:2048 + (k + 1) * 128],
                                                   identity=ident[:]),
                       reads=[o_b, ident_b], writes=[kTp_b], inc=(k == 7))
                op("act", lambda a: a.copy(out=o[:, 1024:2048].rearrange("p (k i) -> p k i", k=8), in_=kTp[:]),
                   reads=[kTp_b], writes=[o_b])
                for cb in range(4):
                    ps, ps_b = proj_block(hT, hT_b, 4 + cb)
                    op("act", lambda a: a.copy(out=o[:, 3072 + cb * 512:3072 + (cb + 1) * 512], in_=ps[:]),
                       reads=[ps_b], writes=[o_b])
                for cb in range(4):
                    ps, ps_b = proj_block(hT, hT_b, 8 + cb)
                    op("act", lambda a: a.activation(out=o[:, 5120 + cb * 512:5120 + (cb + 1) * 512], in_=ps[:], func=AF.Silu),
                       reads=[ps_b], writes=[o_b])
                dma(r1[t * 128:(t + 1) * 128, :], o[:], o_b, reads=[o_b], writes=[r1_b])
            mk.pop()

            check("R1")
            if NO > 0:
                mk.push()
                so2 = [mk.sb("so2", [128, 4096], BF16) for _ in range(2)]
                mt = [mk.sb("mt", [128, 4], F32) for _ in range(2)]
                sc8 = [mk.sb("sc8", [128, 8], F32) for _ in range(2)]
                kr, kr_b = mk.sb("kr", [128, 1024], F32)
                dma(xt[0][0][:], xo[0:128, :], xt[0][1], reads=[xob], writes=[xt[0][1]])
                dma(cst[0][0][:], rope_oth[0:128, :], cst[0][1], reads=[wdr], writes=[cst[0][1]])
                dma(mt[0][0][:], oth_meta[0:128, :], mt[0][1], reads=[wdr], writes=[mt[0][1]])
                gm, gm_b, sh, sh_b, _, _ = mods[1]
                for t in range(NO):
                    par = t % 2
                    if t + 1 < NO:
                        dma(xt[1 - par][0][:], xo[(t + 1) * 128:(t + 2) * 128, :], xt[1 - par][1], reads=[xob],
                            writes=[xt[1 - par][1]])
                        dma(cst[1 - par][0][:], rope_oth[(t + 1) * 128:(t + 2) * 128, :], cst[1 - par][1], reads=[wdr],
                            writes=[cst[1 - par][1]])
                        dma(mt[1 - par][0][:], oth_meta[(t + 1) * 128:(t + 2) * 128, :], mt[1 - par][1], reads=[wdr],
                            writes=[mt[1 - par][1]])
                    x_t, x_b = xt[par]
                    cs, cs_b = cst[par]
                    o, o_b = so2[par]
                    m, m_b = mt[par]
                    sc, sc_b = sc8[par]
                    hT, hT_b, _, _ = norm_mod_T(nb, par, x_t[:], x_b, gm, gm_b, sh, sh_b)
                    op("act", lambda a: a.activation(out=sc[:, 0:4], in_=lg[:, 0:4], func=AF.Exp, scale=m[:, 0:1]),
                       reads=[lg_b, m_b], writes=[sc_b])
                    op("act", lambda a: a.activation(out=sc[:, 4:8], in_=lg[:, 4:8], func=AF.Exp, scale=m[:, 2:3]),
                       reads=[lg_b, m_b], writes=[sc_b])
                    op("dve", lambda v: v.tensor_scalar(out=sc[:, 0:4], in0=sc[:, 0:4], scalar1=m[:, 1:2], scalar2=None,
                                                        op0=ALU.mult), reads=[sc_b, m_b], writes=[sc_b])
                    op("dve", lambda v: v.tensor_scalar(out=sc[:, 4:8], in0=sc[:, 4:8], scalar1=m[:, 3:4], scalar2=None,
                                                        op0=ALU.mult), reads=[sc_b, m_b], writes=[sc_b])
                    for cb in range(2):
                        ps, ps_b = proj_block(hT, hT_b, 2 + cb)
                        rope_block(ps, ps_b, cs, cs_b, kr, kr_b, cb * 512)
                    for h in range(4):
                        op("dve", lambda v: v.tensor_scalar(out=o[:, h * 256:(h + 1) * 256], in0=kr[:, h * 256:(h + 1) * 256],
                                                            scalar1=sc[:, h:h + 1], scalar2=None, op0=ALU.mult),
                           reads=[kr_b, sc_b], writes=[o_b])
                        op("dve", lambda g: g.tensor_scalar(out=o[:, 1024 + h * 256:1024 + (h + 1) * 256],
                                                             in0=kr[:, h * 256:(h + 1) * 256],
                                                             scalar1=sc[:, 4 + h:5 + h], scalar2=None, op0=ALU.mult),
                           reads=[kr_b, sc_b], writes=[o_b])
                    for cb in range(4):
                        ps, ps_b = proj_block(hT, hT_b, 4 + cb)
                        op("act", lambda a: a.copy(out=o[:, 2048 + cb * 512:2048 + (cb + 1) * 512], in_=ps[:]),
                           reads=[ps_b], writes=[o_b])
                    dma(osc[t * 128:(t + 1) * 128, :], o[:], o_b, reads=[o_b], writes=[osc_b])
                mk.pop()
            mk.pop()
            check("O")

            mk.push()
            DT, DT_b = mk.sb("DT", [128, 512], F32)
            qdF, qdF_b = mk.sb("qdF", [128, 8, 128], F32)
            qdB, qdB_b = mk.sb("qdB", [128, 8, 128], F32)
            kdec, kdec_b = mk.sb("kdec", [128, 8], F32)
            tmpd, tmpd_b = mk.sb("tmpd", [128, 128], F32)
            for h in range(4):
                op("act", lambda a: a.activation(out=tmpd[:], in_=ctab[:, CT_EF:CT_EF + 128], func=AF.Exp, scale=lg[:, h:h + 1]),
                   reads=[ctab_b, lg_b], writes=[tmpd_b])
                op("dve", lambda v: v.scalar_tensor_tensor(out=DT[:, h * 128:(h + 1) * 128], in0=tmpd[:], scalar=1.0 / 16,
                                                          in1=ctab[:, CT_MF:CT_MF + 128], op0=ALU.mult, op1=ALU.mult),
                   reads=[tmpd_b, ctab_b], writes=[DT_b])
                op("act", lambda a: a.activation(out=tmpd[:], in_=ctab[:, CT_EB:CT_EB + 128], func=AF.Exp,
                                                 scale=lg[:, 4 + h:5 + h]),
                   reads=[ctab_b, lg_b], writes=[tmpd_b])
                op("dve", lambda v: v.scalar_tensor_tensor(out=tmpd[:], in0=tmpd[:], scalar=1.0 / 16,
                                                          in1=ctab[:, CT_MB:CT_MB + 128], op0=ALU.mult, op1=ALU.mult),
                   reads=[tmpd_b, ctab_b], writes=[tmpd_b])
                op("dve", lambda v: v.tensor_tensor(out=DT[:, h * 128:(h + 1) * 128], in0=DT[:, h * 128:(h + 1) * 128],
                                                    in1=tmpd[:], op=ALU.add), reads=[tmpd_b, DT_b], writes=[DT_b])
                for dc in range(2):
                    op("act", lambda a: a.activation(out=qdF[:, h * 2 + dc, :], in_=ctab[:, CT_IP1:CT_IP1 + 128], func=AF.Exp,
                                                     scale=lg[:, h:h + 1]), reads=[ctab_b, lg_b], writes=[qdF_b])
                    op("act", lambda a: a.activation(out=qdB[:, h * 2 + dc, :], in_=ctab[:, CT_CMI:CT_CMI + 128], func=AF.Exp,
                                                     scale=lg[:, 4 + h:5 + h]), reads=[ctab_b, lg_b], writes=[qdB_b])
            op("dve", lambda v: v.tensor_scalar(out=qdF[:], in0=qdF[:], scalar1=1.0 / 16, scalar2=None, op0=ALU.mult),
               reads=[qdF_b], writes=[qdF_b])
            op("dve", lambda v: v.tensor_scalar(out=qdB[:], in0=qdB[:], scalar1=1.0 / 16, scalar2=None, op0=ALU.mult),
               reads=[qdB_b], writes=[qdB_b])
            op("act", lambda a: a.activation(out=kdec[:, 0:4], in_=lg[:, 0:4], func=AF.Exp, scale=ctab[:, CT_C127:CT_C127 + 1]),
               reads=[ctab_b, lg_b], writes=[kdec_b])
            op("act", lambda a: a.activation(out=kdec[:, 4:8], in_=lg[:, 4:8], func=AF.Exp, scale=ctab[:, CT_PIDX:CT_PIDX + 1]),
               reads=[ctab_b, lg_b], writes=[kdec_b])

            S32 = [mk.sb("S32", [128, 8, 512], F32) for _ in range(2)]
            Sbf, Sbf_b = mk.sb("Sbf", [128, 8, 512], BF16)
            Kt, Kt_b = mk.sb("Kt", [128, 1024], BF16)
            sps = [mk.ps("sps", [128, 512], F32) for _ in range(4)]
            spn = [0]

            sin_d, sin_d_b = mk.dram("sin_d", [256, 4096], F32)
            if NO > 0:
                mk.push()
                SinF, SinF_b = mk.sb("SinF", [128, 8, 512], F32)
                SinB, SinB_b = mk.sb("SinB", [128, 8, 512], F32)
                op("pool", lambda g: g.memset(SinF[:], 0.0), writes=[SinF_b])
                op("pool", lambda g: g.memset(SinB[:], 0.0), writes=[SinB_b])
                GSZ = 4 if NO % 4 == 0 else (2 if NO % 2 == 0 else 1)
                og = [mk.sb("og", [128, GSZ, 4096], BF16) for _ in range(2)]
                ngr = NO // GSZ
                def load_grp(gi):
                    g_, g_b = og[gi % 2]
                    dma(g_[:], osc[gi * GSZ * 128:(gi + 1) * GSZ * 128, :].rearrange("(i p) c -> p i c", p=128), g_b,
                        reads=[osc_b], writes=[g_b])
                load_grp(0)
                for gi in range(ngr):
                    if gi + 1 < ngr:
                        load_grp(gi + 1)
                    g_, g_b = og[gi % 2]
                    for d in range(2):
                        Sacc, Sacc_b = (SinF, SinF_b) if d == 0 else (SinB, SinB_b)
                        for h in range(4):
                            for dc in range(2):
                                ps, ps_b = sps[spn[0] % 4]
                                spn[0] += 1
                                for i in range(GSZ):
                                    kc0 = d * 1024 + h * 256 + dc * 128
                                    op("pe", lambda p: p.matmul(ps[:], lhsT=g_[:, i, kc0:kc0 + 128],
                                                                rhs=g_[:, i, 2048 + h * 512:2048 + (h + 1) * 512],
                                                                start=(i == 0), stop=(i == GSZ - 1)),
                                       reads=[g_b], writes=[ps_b], inc=(i == GSZ - 1))
                                op("dve", lambda v: v.tensor_tensor(out=Sacc[:, h * 2 + dc, :], in0=Sacc[:, h * 2 + dc, :],
                                                                    in1=ps[:], op=ALU.add),
                                   reads=[ps_b, Sacc_b], writes=[Sacc_b])
                dma(sin_d[0:128, :], SinF[:].rearrange("p a b -> p (a b)"), SinF_b, reads=[SinF_b], writes=[sin_d_b])
                dma(sin_d[128:256, :], SinB[:].rearrange("p a b -> p (a b)"), SinB_b, reads=[SinB_b], writes=[sin_d_b])
                mk.pop()

            def state_update(d, K_ap, K_b, V_ap, V_b):
                S, S_b = S32[d]
                for h in range(4):
                    eng = "dve"
                    op(eng, lambda v: v.tensor_scalar(out=Kt[:, h * 256:(h + 1) * 256], in0=K_ap[:, h * 256:(h + 1) * 256],
                                                      scalar1=kdec[:, d * 4 + h:d * 4 + h + 1], scalar2=None, op0=ALU.mult),
                       reads=[K_b, kdec_b], writes=[Kt_b])
                for h in range(4):
                    for dc in range(2):
                        ps, ps_b = sps[spn[0] % 4]
                        spn[0] += 1
                        op("pe", lambda p: p.matmul(ps[:], lhsT=Kt[:, h * 256 + dc * 128:h * 256 + (dc + 1) * 128],
                                                    rhs=V_ap[:, h * 512:(h + 1) * 512], start=True, stop=True),
                           reads=[Kt_b, V_b], writes=[ps_b])
                        op("dve", lambda v: v.scalar_tensor_tensor(out=S[:, h * 2 + dc, :], in0=S[:, h * 2 + dc, :],
                                                                  scalar=cdec[:, d * 4 + h:d * 4 + h + 1], in1=ps[:],
                                                                  op0=ALU.mult, op1=ALU.add),
                           reads=[ps_b, S_b, cdec_b], writes=[S_b])

            seqs = [(0, TS), (TS, NT)]

            check("O2")
            mk.push()
            kv = [mk.sb("kv", [128, 3072], BF16) for _ in range(2)]
            sst = [mk.sb("sst", [128, 8, 512], BF16) for _ in range(2)]
            order = []
            for si, (t0, t1) in enumerate(seqs):
                order += [(si, t) for t in range(t1 - 1, t0 - 1, -1)]
            def load_kv(i):
                _, t = order[i]
                b_, b_b = kv[i % 2]
                dma(b_[:], r1[t * 128:(t + 1) * 128, 2048:5120], b_b, reads=[r1_b], writes=[b_b])
            if order:
                load_kv(0)
            for i, (si, t) in enumerate(order):
                if i + 1 < len(order):
                    load_kv(i + 1)
                S, S_b = S32[1]
                if t == seqs[si][1] - 1:
                    if si == 0 or NO == 0:
                        op("pool", lambda g: g.memset(S[:], 0.0), writes=[S_b])
                    else:
                        dma(S[:].rearrange("p a b -> p (a b)"), sin_d[128:256, :], S_b, reads=[sin_d_b], writes=[S_b])
                st_, st_b = sst[i % 2]
                op("act", lambda a: a.copy(out=st_[:], in_=S[:]), reads=[S_b], writes=[st_b])
                dma(sbst[t * 128:(t + 1) * 128, :], st_[:].rearrange("p a b -> p (a b)"), st_b, reads=[st_b], writes=[sbst_b])
                b_, b_b = kv[i % 2]
                state_update(1, b_[:, 0:1024], b_b, b_[:, 1024:3072], b_b)
            mk.pop()

            check("R2")
            mk.push()
            Wo, Wo_b = mk.sb("Wo", [128, 16, 1024], BF16)
            mk.push()
            stg = [mk.sb("stg", [128, 1024], F32) for _ in range(2)]
            load_weight_bf16(Wo, Wo_b, ret_w_out, 2048, 1024, stg, 1024)
            mk.pop()
            mods = make_mod(0, 0, True, want_norm=False)
            gng, gng_b = mk.sb("gng", [128, 2048], F32)
            load_bc(gng[:], gng_b, ret_gn_g[0:1, :])
            rt = [mk.sb("rt", [128, 7168], BF16) for _ in range(2)]
            sbt = [mk.sb("sbt", [128, 8, 512], BF16) for _ in range(2)]
            xt = [mk.sb("xt", [128, 1024], F32) for _ in range(2)]
            QfT, QfT_b = mk.sb("QfT", [128, 8, 128], BF16)
            QbT, QbT_b = mk.sb("QbT", [128, 8, 128], BF16)
            sTm, sTm_b = mk.sb("sTm", [128, 512], BF16)
            on, on_b = mk.sb("on", [128, 2048], F32)
            ogb, ogb_b = mk.sb("ogb", [128, 2048], BF16)
            ogT, ogT_b = mk.sb("ogT", [128, 16, 128], BF16)
            st6, st6_b = mk.sb("st6", [128, 4, 6], F32)
            mv, mv_b = mk.sb("mv", [128, 4, 2], F32)
            rstd, rstd_b = mk.sb("rstd", [128, 4], F32)
            xo_t = [mk.sb("xo_t", [128, 1024], F32) for _ in range(1)]
            psS, psS_b = mk.ps("psS", [128, 512], F32)
            psO = [mk.ps("psO", [128, 512], F32) for _ in range(2)]
            psT, psT_b = mk.ps("psT", [128, 8, 128], BF16)

            order = [(si, t) for si, (t0, t1) in enumerate(seqs) for t in range(t0, t1)]
            def load_r3(i):
                _, t = order[i]
                a_, a_b = rt[i % 2]
                b_, b_b = sbt[i % 2]
                c_, c_b = xt[i % 2]
                dma(a_[:], r1[t * 128:(t + 1) * 128, :], a_b, reads=[r1_b], writes=[a_b])
                dma(b_[:].rearrange("p a b -> p (a b)"), sbst[t * 128:(t + 1) * 128, :], b_b, reads=[sbst_b], writes=[b_b])
                dma(c_[:], xs[t * 128:(t + 1) * 128, :], c_b, reads=[xsb], writes=[c_b])
            if order:
                load_r3(0)
            for i, (si, t) in enumerate(order):
                if i + 1 < len(order):
                    load_r3(i + 1)
                S, S_b = S32[0]
                if t == seqs[si][0]:
                    if si == 0 or NO == 0:
                        op("pool", lambda g: g.memset(S[:], 0.0), writes=[S_b])
                    else:
                        dma(S[:].rearrange("p a b -> p (a b)"), sin_d[0:128, :], S_b, reads=[sin_d_b], writes=[S_b])
                a_, a_b = rt[i % 2]
                sb_, sb_b = sbt[i % 2]
                x_t, x_b = xt[i % 2]
                _, _, _, _, gt, gt_b = mods[si]
                QT = a_[:, 0:1024].rearrange("p (k i) -> p k i", k=8)
                KT = a_[:, 1024:2048].rearrange("p (k i) -> p k i", k=8)
                op("act", lambda a: a.copy(out=Sbf[:], in_=S[:]), reads=[S_b], writes=[Sbf_b])
                for h in range(4):
                    for dc in range(2):
                        op("pe", lambda p: p.matmul(psS[:, h * 128:(h + 1) * 128], lhsT=KT[:, h * 2 + dc, :],
                                                    rhs=QT[:, h * 2 + dc, :], start=(dc == 0), stop=(dc == 1)),
                           reads=[a_b], writes=[psS_b], inc=(h == 3 and dc == 1))
                op("dve", lambda v: v.tensor_tensor(out=sTm[:], in0=psS[:], in1=DT[:], op=ALU.mult),
                   reads=[psS_b, DT_b], writes=[sTm_b])
                op("dve", lambda g: g.tensor_tensor(out=QfT[:], in0=QT, in1=qdF[:], op=ALU.mult),
                   reads=[a_b, qdF_b], writes=[QfT_b])
                op("dve", lambda g: g.tensor_tensor(out=QbT[:], in0=QT, in1=qdB[:], op=ALU.mult),
                   reads=[a_b, qdB_b], writes=[QbT_b])
                for h in range(4):
                    po, po_b = psO[h % 2]
                    op("pe", lambda p: p.matmul(po[:], lhsT=sTm[:, h * 128:(h + 1) * 128],
                                                rhs=a_[:, 3072 + h * 512:3072 + (h + 1) * 512], start=True, stop=False),
                       reads=[sTm_b, a_b], writes=[po_b], inc=False)
                    for dc in range(2):
                        op("pe", lambda p: p.matmul(po[:], lhsT=QfT[:, h * 2 + dc, :], rhs=Sbf[:, h * 2 + dc, :],
                                                    start=False, stop=False),
                           reads=[QfT_b, Sbf_b], writes=[po_b], inc=False)
                    for dc in range(2):
                        op("pe", lambda p: p.matmul(po[:], lhsT=QbT[:, h * 2 + dc, :], rhs=sb_[:, h * 2 + dc, :],
                                                    start=False, stop=(dc == 1)),
                           reads=[QbT_b, sb_b], writes=[po_b], inc=(dc == 1))
                    op("dve", lambda v: v.bn_stats(out=st6[:, h, :], in_=po[:]), reads=[po_b], writes=[st6_b])
                    op("dve", lambda v: v.bn_aggr(out=mv[:, h, :], in_=st6[:, h, :]), reads=[st6_b], writes=[mv_b])
                    op("dve", lambda v: v.tensor_scalar(out=rstd[:, h:h + 1], in0=mv[:, h, 1:2], scalar1=EPS, scalar2=None,
                                                        op0=ALU.add), reads=[mv_b], writes=[rstd_b])
                    op("act", lambda a: a.sqrt(out=rstd[:, h:h + 1], in_=rstd[:, h:h + 1]), reads=[rstd_b], writes=[rstd_b])
                    op("dve", lambda v: v.reciprocal(out=rstd[:, h:h + 1], in_=rstd[:, h:h + 1]), reads=[rstd_b], writes=[rstd_b])
                    op("dve", lambda v: v.tensor_scalar(out=on[:, h * 512:(h + 1) * 512], in0=po[:], scalar1=mv[:, h, 0:1],
                                                        scalar2=rstd[:, h:h + 1], op0=ALU.subtract, op1=ALU.mult),
                       reads=[po_b, mv_b, rstd_b], writes=[on_b])
                op("dve", lambda g: g.tensor_tensor(out=on[:], in0=on[:], in1=gng[:], op=ALU.mult),
                   reads=[on_b, gng_b], writes=[on_b])
                op("dve", lambda g: g.tensor_tensor(out=ogb[:], in0=on[:], in1=a_[:, 5120:7168], op=ALU.mult),
                   reads=[on_b, a_b], writes=[ogb_b])
                for half in range(2):
                    for k in range(8):
                        kk = half * 8 + k
                        op("pe", lambda p: p.transpose(out=psT[:, k, :], in_=ogb[:, kk * 128:(kk + 1) * 128], identity=ident[:]),
                           reads=[ogb_b, ident_b], writes=[psT_b], inc=(k == 7))
                    op("act", lambda a: a.copy(out=ogT[:, half * 8:(half + 1) * 8, :], in_=psT[:]),
                       reads=[psT_b], writes=[ogT_b])
                xo_, xo_b = xo_t[0]
                for cb in range(2):
                    po, po_b = psO[cb]
                    for k in range(16):
                        op("pe", lambda p: p.matmul(po[:], lhsT=ogT[:, k, :], rhs=Wo[:, k, cb * 512:(cb + 1) * 512],
                                                    start=(k == 0), stop=(k == 15)),
                           reads=[ogT_b, Wo_b], writes=[po_b], inc=(k == 15))
                    op("dve", lambda v: v.tensor_tensor(out=xo_[:, cb * 512:(cb + 1) * 512], in0=po[:],
                                                        in1=gt[:, cb * 512:(cb + 1) * 512], op=ALU.mult),
                       reads=[po_b, gt_b], writes=[xo_b])
                op("dve", lambda g: g.tensor_tensor(out=x_t[:], in0=xo_[:], in1=x_t[:], op=ALU.add),
                   reads=[xo_b, x_b], writes=[x_b])
                dma(xa[t * 128:(t + 1) * 128, :], x_t[:], x_b, reads=[x_b], writes=[xa_b])
                state_update(0, a_[:, 2048:3072], a_b, a_[:, 3072:5120], a_b)
            mk.pop()
            mk.pop()
            check("R3")

            NBM = NT * 4 + NE
            U32 = mybir.dt.uint32
            I32 = mybir.dt.int32
            xg_d, xg_b = mk.dram("xg_d", [NBM * 128, 1024], BF16)
            yg_d, yg_b = mk.dram("yg_d", [NBM * 128, 1024], F32)

            bnd_reg = nc.gpsimd.alloc_register("bnd")
            nc.gpsimd.reg_mov(bnd_reg, 2 * NE * 128 - 1)

            def moe_layer(l, xin, xin_b, xout, xout_b):
                mk.push()
                Rall, Rall_b = mk.sb("Rall", [128, NT, NE], F32)
                I4all, I4all_b = mk.sb("I4all", [128, NT, 4], F32)
                G4all, G4all_b = mk.sb("G4all", [128, NT, 4], F32)
                Dall, Dall_b = mk.sb("Dall", [128, NT, 4], U32)
                off, off_b = mk.sb("off", [128, NE], F32)
                ebf, ebf_b = mk.sb("ebf", [128, NBM], F32)
                idxW, idxW_b = mk.sb("idxW", [128, NBM], U32)
                pst, pst_b = mk.sb("pst", [128, NE], F32)
                op("pool", lambda g: g.memset(off[:], 0.0), writes=[off_b])
                mk.push()
                mods = make_mod(l, 1, False)
                nb = NormBufs()
                xt = [mk.sb("xt", [128, 1024], F32) for _ in range(2)]
                wr32, wr32_b = mk.sb("wr32", [128, 8, NE], F32)
                wr, wr_b = mk.sb("wr", [128, 8, NE], BF16)
                brb, brb_b = mk.sb("brb", [128, NE], F32)
                lgt, lgt_b = mk.sb("lgt", [128, NE], F32)
                mskb, mskb_b = mk.sb("mskb", [128, NE], BF16)
                top, top_b = mk.sb("top", [128, 8], F32)
                idx8, idx8_b = mk.sb("idx8", [128, 8], U32)
                e4, e4_b = mk.sb("e4", [128, 4], F32)
                nm, nm_b = mk.sb("nm", [128, 1], F32)
                sm, sm_b = mk.sb("sm", [128, 1], F32)
                Ubf, Ubf_b = mk.sb("Ubf", [128, 128], BF16)
                onesb, onesb_b = mk.sb("onesb", [128, 128], BF16)
                lps, lps_b = mk.ps("lps", [128, NE], F32)
                rps, rps_b = mk.ps("rps", [128, NE], F32)
                cps, cps_b = mk.ps("cps", [128, NE], F32)
                op("pool", lambda g: g.tensor_copy(out=Ubf[:], in_=ctab[:, CT_U:CT_U + 128]), reads=[ctab_b], writes=[Ubf_b])
                op("pool", lambda g: g.memset(onesb[:], 1.0), writes=[onesb_b])
                dma(wr32[:], moe_w_r[l * 1024:(l + 1) * 1024, :].rearrange("(k p) e -> p k e", p=128), wr32_b,
                    reads=[wdr], writes=[wr32_b])
                op("pool", lambda g: g.tensor_copy(out=wr[:], in_=wr32[:]), reads=[wr32_b], writes=[wr_b])
                load_bc(brb[:], brb_b, moe_b_r[l:l + 1, :])
                dma(xt[0][0][:], xin[0:128, :], xt[0][1], reads=[xin_b], writes=[xt[0][1]])
                for t in range(NT):
                    par = t % 2
                    if t + 1 < NT:
                        dma(xt[1 - par][0][:], xin[(t + 1) * 128:(t + 2) * 128, :], xt[1 - par][1], reads=[xin_b],
                            writes=[xt[1 - par][1]])
                    gm, gm_b, sh, sh_b, _, _ = mods[seq_of(t)]
                    x_t, x_b = xt[par]
                    hT, hT_b, hh, hh_b = norm_mod_T(nb, par, x_t[:], x_b, gm, gm_b, sh, sh_b)
                    dma(h2d[t * 128:(t + 1) * 128, :], hh[:], hh_b, reads=[hh_b], writes=[h2d_b])
                    for k in range(8):
                        op("pe", lambda p: p.matmul(lps[:], lhsT=hT[:, k, :], rhs=wr[:, k, :], start=(k == 0), stop=(k == 7)),
                           reads=[hT_b, wr_b], writes=[lps_b], inc=(k == 7))
                    op("dve", lambda v: v.tensor_tensor(out=lgt[:], in0=lps[:], in1=brb[:], op=ALU.add),
                       reads=[lps_b, brb_b], writes=[lgt_b])
                    op("dve", lambda v: v.max(out=top[:], in_=lgt[:]), reads=[lgt_b], writes=[top_b])
                    op("dve", lambda v: v.max_index(out=idx8[:], in_max=top[:], in_values=lgt[:]),
                       reads=[lgt_b, top_b], writes=[idx8_b])
                    op("dve", lambda v: v.tensor_copy(out=I4all[:, t, :], in_=idx8[:, 0:4]), reads=[idx8_b], writes=[I4all_b])
                    op("dve", lambda v: v.tensor_scalar(out=mskb[:], in0=lgt[:], scalar1=top[:, 3:4], scalar2=None, op0=ALU.is_ge),
                       reads=[lgt_b, top_b], writes=[mskb_b])
                    op("dve", lambda v: v.tensor_scalar(out=nm[:], in0=top[:, 0:1], scalar1=-1.0, scalar2=None, op0=ALU.mult),
                       reads=[top_b], writes=[nm_b])
                    op("act", lambda a: a.activation(out=e4[:], in_=top[:, 0:4], func=AF.Exp, bias=nm[:, 0:1]),
                       reads=[top_b, nm_b], writes=[e4_b])
                    op("dve", lambda v: v.reduce_sum(out=sm[:], in_=e4[:], axis=mybir.AxisListType.X),
                       reads=[e4_b], writes=[sm_b])
                    op("dve", lambda v: v.reciprocal(out=sm[:], in_=sm[:]), reads=[sm_b], writes=[sm_b])
                    op("dve", lambda v: v.tensor_scalar(out=G4all[:, t, :], in0=e4[:], scalar1=sm[:, 0:1], scalar2=None,
                                                        op0=ALU.mult), reads=[e4_b, sm_b], writes=[G4all_b])
                    op("pe", lambda p: p.matmul(rps[:], lhsT=Ubf[:], rhs=mskb[:], start=True, stop=True),
                       reads=[Ubf_b, mskb_b], writes=[rps_b])
                    op("pe", lambda p: p.matmul(cps[:], lhsT=onesb[:], rhs=mskb[:], start=True, stop=True),
                       reads=[onesb_b, mskb_b], writes=[cps_b])
                    op("dve", lambda v: v.tensor_tensor(out=Rall[:, t, :], in0=rps[:], in1=off[:], op=ALU.add),
                       reads=[rps_b, off_b], writes=[Rall_b])
                    op("dve", lambda v: v.tensor_tensor(out=off[:], in0=cps[:], in1=off[:], op=ALU.add),
                       reads=[cps_b, off_b], writes=[off_b])
                mk.pop()

                mk.push()
                thr, thr_b = mk.sb("thr", [128, 128], F32)
                cmpt, cmpt_b = mk.sb("cmpt", [128, NE * NT], F32)
                nbk, nbk_b = mk.sb("nbk", [128, NE], F32)
                ca, ca_b = mk.sb("ca", [128, NE], F32)
                cb_, cb_b = mk.sb("cb_", [128, NE], F32)
                io_i, io_i_b = mk.sb("io_i", [128, NBM], I32)
                thb, thb_b = mk.sb("thb", [128, NBM], F32)
                CH = 64
                cmpb, cmpb_b = mk.sb("cmpb", [128, CH * NE], F32)
                sk, sk_b = mk.sb("sk", [128, NBM], F32)
                rowf, rowf_b = mk.sb("rowf", [128, NBM], F32)
                pcol, pcol_b = mk.sb("pcol", [128, 1], F32)
                op("dve", lambda v: v.tensor_scalar(out=thr[:], in0=ctab[:, CT_IO:CT_IO + 128], scalar1=128.0, scalar2=None,
                                                    op0=ALU.mult), reads=[ctab_b], writes=[thr_b])
                op("dve", lambda v: v.tensor_tensor(out=cmpt[:].rearrange("p (e m) -> p e m", e=NE),
                                                    in0=off[:].unsqueeze(2).to_broadcast([128, NE, NT]),
                                                    in1=thr[:, 0:NT].unsqueeze(1).to_broadcast([128, NE, NT]), op=ALU.is_gt),
                   reads=[off_b, thr_b], writes=[cmpt_b])
                op("dve", lambda v: v.reduce_sum(out=nbk[:], in_=cmpt[:].rearrange("p (e m) -> p e m", e=NE),
                                                 axis=mybir.AxisListType.X), reads=[cmpt_b], writes=[nbk_b])
                op("dve", lambda v: v.tensor_scalar(out=ca[:], in0=nbk[:], scalar1=128.0, scalar2=None, op0=ALU.mult),
                   reads=[nbk_b], writes=[ca_b])
                op("dve", lambda v: v.tensor_copy(out=pst[:], in_=ca[:]), reads=[ca_b], writes=[pst_b])
                cur, cur_b, nxt, nxt_b = ca, ca_b, cb_, cb_b
                s_ = 1
                while s_ < NE:
                    sft = s_
                    op("dve", lambda v: v.tensor_copy(out=nxt[:, 0:sft], in_=cur[:, 0:sft]), reads=[cur_b], writes=[nxt_b])
                    op("dve", lambda v: v.tensor_tensor(out=nxt[:, sft:NE], in0=cur[:, sft:NE], in1=cur[:, 0:NE - sft], op=ALU.add),
                       reads=[cur_b], writes=[nxt_b])
                    cur, cur_b, nxt, nxt_b = nxt, nxt_b, cur, cur_b
                    s_ *= 2
                pend, pend_b = cur, cur_b
                op("dve", lambda v: v.tensor_tensor(out=pst[:], in0=pend[:], in1=pst[:], op=ALU.subtract),
                   reads=[pend_b, pst_b], writes=[pst_b])
                op("pool", lambda g: g.iota(io_i[:], pattern=[[1, NBM]], base=0, channel_multiplier=0), writes=[io_i_b])
                op("dve", lambda v: v.tensor_copy(out=thb[:], in_=io_i[:]), reads=[io_i_b], writes=[thb_b])
                op("dve", lambda v: v.tensor_scalar(out=thb[:], in0=thb[:], scalar1=128.0, scalar2=None, op0=ALU.mult),
                   reads=[thb_b], writes=[thb_b])
                for c0 in range(0, NBM, CH):
                    cw = min(CH, NBM - c0)
                    op("dve", lambda v: v.tensor_tensor(out=cmpb[:, 0:cw * NE].rearrange("p (b e) -> p b e", e=NE),
                                                        in0=pend[:].unsqueeze(1).to_broadcast([128, cw, NE]),
                                                        in1=thb[:, c0:c0 + cw].unsqueeze(2).to_broadcast([128, cw, NE]), op=ALU.is_le),
                       reads=[pend_b, thb_b], writes=[cmpb_b])
                    op("dve", lambda v: v.reduce_sum(out=ebf[:, c0:c0 + cw], in_=cmpb[:, 0:cw * NE].rearrange("p (b e) -> p b e", e=NE),
                                                     axis=mybir.AxisListType.X), reads=[cmpb_b], writes=[ebf_b])
                op("dve", lambda v: v.tensor_scalar(out=ebf[:], in0=ebf[:], scalar1=float(NE - 1), scalar2=None, op0=ALU.min),
                   reads=[ebf_b], writes=[ebf_b])
                op("dve", lambda v: v.tensor_scalar(out=pcol[:], in0=ctab[:, CT_PIDX:CT_PIDX + 1], scalar1=float(l * NE * 128),
                                                    scalar2=None, op0=ALU.add), reads=[ctab_b], writes=[pcol_b])
                op("pool", lambda g: g.memset(sk[:], 0.0), writes=[sk_b])
                op("dve", lambda v: v.tensor_tensor(out=sk[:, 2:NBM], in0=ebf[:, 2:NBM], in1=ebf[:, 0:NBM - 2], op=ALU.is_equal),
                   reads=[ebf_b, sk_b], writes=[sk_b])
                op("dve", lambda v: v.tensor_scalar(out=rowf[:], in0=ebf[:], scalar1=128.0, scalar2=pcol[:, 0:1],
                                                    op0=ALU.mult, op1=ALU.add), reads=[ebf_b, pcol_b], writes=[rowf_b])
                op("dve", lambda v: v.scalar_tensor_tensor(out=rowf[:], in0=sk[:], scalar=float(1 << 22), in1=rowf[:],
                                                          op0=ALU.mult, op1=ALU.add), reads=[sk_b, rowf_b], writes=[rowf_b])
                op("dve", lambda v: v.tensor_copy(out=idxW[:], in_=rowf[:]), reads=[rowf_b], writes=[idxW_b])

                tmpR, tmpR_b = mk.sb("tmpR", [128, NE], F32)
                jk, jk_b = mk.sb("jk", [128, NE], F32)
                d4f, d4f_b = mk.sb("d4f", [128, 4], F32)
                hx = [mk.sb("hx", [128, 1024], BF16) for _ in range(2)]
                for t in range(NT):
                    h_, h_b = hx[t % 2]
                    dma(h_[:], h2d[t * 128:(t + 1) * 128, :], h_b, reads=[h2d_b], writes=[h_b])
                    op("dve", lambda v: v.tensor_tensor(out=tmpR[:], in0=Rall[:, t, :], in1=pst[:], op=ALU.add),
                       reads=[Rall_b, pst_b], writes=[tmpR_b])
                    for k in range(4):
                        op("dve", lambda v: v.scalar_tensor_tensor(out=jk[:], in0=ctab[:, CT_IO:CT_IO + NE], scalar=I4all[:, t, k:k + 1],
                                                                  in1=tmpR[:], op0=ALU.is_equal, op1=ALU.mult),
                           reads=[ctab_b, I4all_b, tmpR_b], writes=[jk_b])
                        op("dve", lambda v: v.reduce_sum(out=d4f[:, k:k + 1], in_=jk[:], axis=mybir.AxisListType.X),
                           reads=[jk_b], writes=[d4f_b])
                    op("dve", lambda v: v.tensor_copy(out=Dall[:, t, :], in_=d4f[:]), reads=[d4f_b], writes=[Dall_b])
                    for k in range(4):
                        dma(None, None, h_b, reads=[h_b, Dall_b], writes=[xg_b], q="pool",
                            fn=lambda g: g.indirect_dma_start(out=xg_d[:, :],
                                                              out_offset=bass.IndirectOffsetOnAxis(ap=Dall[:, t, k:k + 1], axis=0),
                                                              in_=h_[:], in_offset=None))
                mk.pop()

                mk.push()
                Wgu = [mk.sb("Wgu", [128, 8, 2 * DE], BF16) for _ in range(2)]
                Wdn = [mk.sb("Wdn", [128, KE, 1024], BF16) for _ in range(2)]
                bguA, bguA_b = mk.sb("bguA", [NE, 2 * DE], BF16)
                bdnA, bdnA_b = mk.sb("bdnA", [NE, 1024], BF16)
                ohs = [mk.sb("oh", [NE, 128], BF16) for _ in range(2)]
                xgs = [mk.sb("xgs", [128, 1024], BF16) for _ in range(2)]
                xTs = [mk.sb("xTs", [128, 8, 128], BF16) for _ in range(2)]
                gl, gl_b = mk.sb("gl", [128, DE], F32)
                sg, sg_b = mk.sb("sg", [128, DE], F32)
                ln, ln_b = mk.sb("ln", [128, DE], F32)
                actb = [mk.sb("actb", [128, DE], BF16) for _ in range(2)]
                actT = [mk.sb("actT", [128, KE, 128], BF16) for _ in range(2)]
                ygs = [mk.sb("ygs", [128, 1024], F32) for _ in range(2)]
                hps = [mk.ps("hps", [128, 512], F32) for _ in range(4)]
                yps = [mk.ps("yps", [128, 512], F32) for _ in range(2)]
                tps, tps_b = mk.ps("tps", [128, KE, 128], BF16)
                tpx, tpx_b = mk.ps("tpx", [128, 8, 128], BF16)
                dma(bguA[:], moe_b_gu[l * NE:(l + 1) * NE, :], bguA_b, reads=[wdr], writes=[bguA_b], q="pool")
                dma(bdnA[:], moe_b_dn[l * NE:(l + 1) * NE, :], bdnA_b, reads=[wdr], writes=[bdnA_b], q="pool")
                ROWS = 2 * NE * 128

                def load_wgu(b):
                    if b >= NBM:
                        return
                    wg, wg_b = Wgu[b % 2]
                    dma(None, None, wg_b, reads=[wdr, idxW_b], writes=[wg_b], q="pool",
                        fn=lambda g: g.indirect_dma_start(out=wg[:].rearrange("p k n -> p (k n)"), out_offset=None, in_=moe_w_gu[:, :],
                                                          in_offset=bass.IndirectOffsetOnAxis(ap=idxW[:, b:b + 1], axis=0),
                                                          bounds_check=bnd_reg, oob_is_err=False))

                def load_wdn(b):
                    if b >= NBM:
                        return
                    wd, wd_b = Wdn[b % 2]
                    dma(None, None, wd_b, reads=[wdr, idxW_b], writes=[wd_b], q="pool",
                        fn=lambda g: g.indirect_dma_start(out=wd[:].rearrange("p k n -> p (k n)"), out_offset=None, in_=moe_w_dn[:, :],
                                                          in_offset=bass.IndirectOffsetOnAxis(ap=idxW[:, b:b + 1], axis=0),
                                                          bounds_check=bnd_reg, oob_is_err=False))

                def load_x(b):
                    if b >= NBM:
                        return
                    x_, x_b_ = xgs[b % 2]
                    dma(x_[:], xg_d[b * 128:(b + 1) * 128, :], x_b_, reads=[xg_b], writes=[x_b_])

                def front(b):
                    par = b % 2
                    wg, wg_b = Wgu[par]
                    oh, oh_b = ohs[par]
                    x_, x_b_ = xgs[par]
                    xT, xT_b = xTs[par]
                    ab, ab_b = actb[par]
                    op("dve", lambda v: v.tensor_scalar(out=oh[:], in0=ebf[0:NE, b:b + 1].to_broadcast([NE, 128]),
                                                        scalar1=ctab[0:NE, CT_PIDX:CT_PIDX + 1], scalar2=None, op0=ALU.is_equal),
                       reads=[ebf_b, ctab_b], writes=[oh_b])
                    for k in range(8):
                        op("pe", lambda p: p.transpose(out=tpx[:, k, :], in_=x_[:, k * 128:(k + 1) * 128], identity=ident[:]),
                           reads=[x_b_, ident_b], writes=[tpx_b], inc=(k == 7))
                    op("act", lambda a: a.copy(out=xT[:], in_=tpx[:]), reads=[tpx_b], writes=[xT_b])
                    for half in range(2):
                        for q in range(GH):
                            cb = half * GH + q
                            hp, hp_b = hps[cb % 4]
                            op("pe", lambda p: p.matmul(hp[:], lhsT=oh[:], rhs=bguA[:, cb * 512:(cb + 1) * 512], start=True, stop=False),
                               reads=[oh_b, bguA_b], writes=[hp_b], inc=False)
                            for k in range(8):
                                op("pe", lambda p: p.matmul(hp[:], lhsT=xT[:, k, :], rhs=wg[:, k, cb * 512:(cb + 1) * 512],
                                                            start=False, stop=(k == 7)),
                                   reads=[xT_b, wg_b], writes=[hp_b], inc=(k == 7))
                            if half == 0:
                                op("dve", lambda v: v.tensor_scalar(out=gl[:, q * 512:(q + 1) * 512], in0=hp[:], scalar1=7.0,
                                                                    scalar2=None, op0=ALU.min), reads=[hp_b], writes=[gl_b])
                            else:
                                op("dve", lambda v: v.tensor_scalar(out=ln[:, q * 512:(q + 1) * 512], in0=hp[:], scalar1=7.0,
                                                                    scalar2=-7.0, op0=ALU.min, op1=ALU.max),
                                   reads=[hp_b], writes=[ln_b])
                    op("act", lambda a: a.activation(out=sg[:], in_=gl[:], func=AF.Sigmoid, scale=1.702),
                       reads=[gl_b], writes=[sg_b])
                    op("dve", lambda v: v.scalar_tensor_tensor(out=ln[:], in0=ln[:], scalar=1.0, in1=gl[:],
                                                              op0=ALU.add, op1=ALU.mult),
                       reads=[ln_b, gl_b], writes=[ln_b])
                    op("dve", lambda v: v.tensor_tensor(out=ab[:], in0=ln[:], in1=sg[:], op=ALU.mult),
                       reads=[ln_b, sg_b], writes=[ab_b])

                def back(b):
                    par = b % 2
                    wd, wd_b = Wdn[par]
                    oh, oh_b = ohs[par]
                    ab, ab_b = actb[par]
                    aT, aT_b = actT[par]
                    yg, yg_bb = ygs[par]
                    for k in range(KE):
                        op("pe", lambda p: p.transpose(out=tps[:, k, :], in_=ab[:, k * 128:(k + 1) * 128], identity=ident[:]),
                           reads=[ab_b, ident_b], writes=[tps_b], inc=(k == KE - 1))
                    op("act", lambda a: a.copy(out=aT[:], in_=tps[:]), reads=[tps_b], writes=[aT_b])
                    for cb in range(2):
                        yp, yp_b = yps[cb]
                        op("pe", lambda p: p.matmul(yp[:], lhsT=oh[:], rhs=bdnA[:, cb * 512:(cb + 1) * 512], start=True, stop=False),
                           reads=[oh_b, bdnA_b], writes=[yp_b], inc=False)
                        for k in range(KE):
                            op("pe", lambda p: p.matmul(yp[:], lhsT=aT[:, k, :], rhs=wd[:, k, cb * 512:(cb + 1) * 512],
                                                        start=False, stop=(k == KE - 1)),
                               reads=[aT_b, wd_b], writes=[yp_b], inc=(k == KE - 1))
                        op("act", lambda a: a.copy(out=yg[:, cb * 512:(cb + 1) * 512], in_=yp[:]), reads=[yp_b], writes=[yg_bb])
                    dma(yg_d[b * 128:(b + 1) * 128, :], yg[:], yg_bb, reads=[yg_bb], writes=[yg_b])

                for b0 in (0, 1):
                    load_wgu(b0)
                    load_wdn(b0)
                    load_x(b0)
                front(0)
                for b in range(NBM):
                    if b + 1 < NBM:
                        front(b + 1)
                    back(b)
                    load_wgu(b + 2)
                    load_wdn(b + 2)
                    load_x(b + 2)
                mk.pop()

                mk.push()
                gts = make_mod(l, 1, True, want_norm=False)
                yk = [[mk.sb("yk", [128, 1024], F32) for _ in range(4)] for _ in range(2)]
                acc, acc_b = mk.sb("acc", [128, 1024], F32)
                xt = [mk.sb("xt", [128, 1024], F32) for _ in range(2)]

                def load_c(t):
                    for k in range(4):
                        y_, y_b = yk[t % 2][k]
                        dma(None, None, y_b, reads=[yg_b, Dall_b], writes=[y_b], q="pool",
                            fn=lambda g: g.indirect_dma_start(out=y_[:], out_offset=None, in_=yg_d[:, :],
                                                              in_offset=bass.IndirectOffsetOnAxis(ap=Dall[:, t, k:k + 1], axis=0)))
                    x_t, x_b = xt[t % 2]
                    dma(x_t[:], xin[t * 128:(t + 1) * 128, :], x_b, reads=[xin_b], writes=[x_b])

                load_c(0)
                for t in range(NT):
                    if t + 1 < NT:
                        load_c(t + 1)
                    x_t, x_b = xt[t % 2]
                    _, _, _, _, gt, gt_b = gts[seq_of(t)]
                    y0, y0_b = yk[t % 2][0]
                    op("dve", lambda v: v.tensor_scalar(out=acc[:], in0=y0[:], scalar1=G4all[:, t, 0:1], scalar2=None, op0=ALU.mult),
                       reads=[y0_b, G4all_b], writes=[acc_b])
                    for k in range(1, 4):
                        y_, y_b = yk[t % 2][k]
                        op("dve", lambda v: v.scalar_tensor_tensor(out=acc[:], in0=y_[:], scalar=G4all[:, t, k:k + 1], in1=acc[:],
                                                                  op0=ALU.mult, op1=ALU.add),
                           reads=[y_b, G4all_b, acc_b], writes=[acc_b])
                    op("dve", lambda g: g.tensor_tensor(out=acc[:], in0=acc[:], in1=gt[:], op=ALU.mult),
                       reads=[acc_b, gt_b], writes=[acc_b])
                    op("dve", lambda g: g.tensor_tensor(out=x_t[:], in0=x_t[:], in1=acc[:], op=ALU.add),
                       reads=[acc_b, x_b], writes=[x_b])
                    dma(xout[t * 128:(t + 1) * 128, :], x_t[:], x_b, reads=[x_b], writes=[xout_b])
                mk.pop()
                mk.pop()

            moe_layer(0, xa, xa_b, xb, xb_b)
            check("E0")

            mk.push()
            Wg, Wg_b = mk.sb("Wg", [128, 8, 4096], BF16)
            Wgo, Wgo_b = mk.sb("Wgo", [128, 16, 1024], BF16)
            wsT, wsT_b = mk.sb("wsT", [128, 4, 128], BF16)
            bb16, bb16_b = mk.sb("bb16", [1, 5120], BF16)
            mk.push()
            stg = [mk.sb("stg", [128, 2048], F32) for _ in range(2)]
            load_weight_bf16(Wg, Wg_b, gm_w_in, 1024, 4096, stg, 2048)
            load_weight_bf16(Wgo, Wgo_b, gm_w_out, 2048, 1024, stg, 1024)
            s_, s_b = stg[0]
            dma(s_[:, 0:512].rearrange("p (g i) -> p g i", g=4), gm_w_sT[:, :].rearrange("(g p) i -> p g i", p=128), s_b,
                reads=[wdr], writes=[s_b])
            op("pool", lambda g: g.tensor_copy(out=wsT[:], in_=s_[:, 0:512].rearrange("p (g i) -> p g i", g=4)),
               reads=[s_b], writes=[wsT_b])
            b32, b32_b = mk.sb("b32", [1, 5120], F32)
            dma(b32[:, 0:4096], gm_b_in[0:1, :], b32_b, reads=[wdr], writes=[b32_b])
            dma(b32[:, 4096:5120], gm_b_out[0:1, :], b32_b, reads=[wdr], writes=[b32_b])
            op("pool", lambda g: g.tensor_copy(out=bb16[:], in_=b32[:]), reads=[b32_b], writes=[bb16_b])
            mk.barrier()
            mk.pop()
            mods = make_mod(1, 0, True)
            nb = NormBufs()
            vng, vng_b = mk.sb("vng", [128, 2048], F32)
            vnb, vnb_b = mk.sb("vnb", [128, 2048], F32)
            bsT, bsT_b = mk.sb("bsT", [128, 4], F32)
            load_bc(vng[:], vng_b, gm_vn_g[0:1, :])
            load_bc(vnb[:], vnb_b, gm_vn_b[0:1, :])
            dma(bsT[:], gm_b_sT[:, :], bsT_b, reads=[wdr], writes=[bsT_b])
            xt = [mk.sb("xt", [128, 1024], F32) for _ in range(2)]
            uu, uu_b = mk.sb("uu", [128, 2048], BF16)
            vv, vv_b = mk.sb("vv", [128, 2048], F32)
            vn, vn_b = mk.sb("vn", [128, 2048], BF16)
            pp, pp_b = mk.sb("pp", [128, 2048], BF16)
            ppT, ppT_b = mk.sb("ppT", [128, 16, 128], BF16)
            st6, st6_b = mk.sb("st6", [128, 4, 6], F32)
            mv, mv_b = mk.sb("mv", [128, 2], F32)
            rstd, rstd_b = mk.sb("rstd", [128, 1], F32)
            xo_t = [mk.sb("xo_t", [128, 1024], F32) for _ in range(1)]
            zps = [mk.ps("zps", [128, 512], F32) for _ in range(4)]
            mps_ = [mk.ps("mps2", [128, 512], F32) for _ in range(2)]
            psT, psT_b = mk.ps("psT", [128, 8, 128], BF16)
            dma(xt[0][0][:], xb[0:128, :], xt[0][1], reads=[xb_b], writes=[xt[0][1]])
            for t in range(NT):
                par = t % 2
                if t + 1 < NT:
                    dma(xt[1 - par][0][:], xb[(t + 1) * 128:(t + 2) * 128, :], xt[1 - par][1], reads=[xb_b],
                        writes=[xt[1 - par][1]])
                gm, gm_b, sh, sh_b, gt, gt_b = mods[seq_of(t)]
                x_t, x_b = xt[par]
                hT, hT_b, _, _ = norm_mod_T(nb, par, x_t[:], x_b, gm, gm_b, sh, sh_b)
                for cb in range(8):
                    zp, zp_b = zps[cb % 4]
                    op("pe", lambda p: p.matmul(zp[:], lhsT=ones1[0:1, :], rhs=bb16[0:1, cb * 512:(cb + 1) * 512],
                                                start=True, stop=False), reads=[ones1_b, bb16_b], writes=[zp_b], inc=False)
                    for k in range(8):
                        op("pe", lambda p: p.matmul(zp[:], lhsT=hT[:, k, :], rhs=Wg[:, k, cb * 512:(cb + 1) * 512],
                                                    start=False, stop=(k == 7)),
                           reads=[hT_b, Wg_b], writes=[zp_b], inc=(k == 7))
                    if cb < 4:
                        op("act", lambda a: a.activation(out=uu[:, cb * 512:(cb + 1) * 512], in_=zp[:], func=AF.Gelu),
                           reads=[zp_b], writes=[uu_b])
                    else:
                        c = cb - 4
                        op("act", lambda a: a.activation(out=vv[:, c * 512:(c + 1) * 512], in_=zp[:], func=AF.Gelu),
                           reads=[zp_b], writes=[vv_b])
                        op("dve", lambda v: v.bn_stats(out=st6[:, c, :], in_=vv[:, c * 512:(c + 1) * 512]),
                           reads=[vv_b], writes=[st6_b])
                op("dve", lambda v: v.bn_aggr(out=mv[:], in_=st6[:]), reads=[st6_b], writes=[mv_b])
                op("dve", lambda v: v.tensor_scalar(out=rstd[:], in0=mv[:, 1:2], scalar1=EPS, scalar2=None, op0=ALU.add),
                   reads=[mv_b], writes=[rstd_b])
                op("act", lambda a: a.sqrt(out=rstd[:], in_=rstd[:]), reads=[rstd_b], writes=[rstd_b])
                op("dve", lambda v: v.reciprocal(out=rstd[:], in_=rstd[:]), reads=[rstd_b], writes=[rstd_b])
                op("dve", lambda v: v.tensor_scalar(out=vv[:], in0=vv[:], scalar1=mv[:, 0:1], scalar2=rstd[:, 0:1],
                                                    op0=ALU.subtract, op1=ALU.mult), reads=[vv_b, mv_b, rstd_b], writes=[vv_b])
                op("dve", lambda g: g.tensor_tensor(out=vv[:], in0=vv[:], in1=vng[:], op=ALU.mult),
                   reads=[vv_b, vng_b], writes=[vv_b])
                op("dve", lambda g: g.tensor_tensor(out=vn[:], in0=vv[:], in1=vnb[:], op=ALU.add),
                   reads=[vv_b, vnb_b], writes=[vn_b])
                for g_ in range(4):
                    mp, mp_b = mps_[g_ % 2]
                    op("pe", lambda p: p.matmul(mp[:], lhsT=wsT[:, g_, :], rhs=vn[:, g_ * 512:(g_ + 1) * 512], start=True, stop=True),
                       reads=[wsT_b, vn_b], writes=[mp_b])
                    op("dve", lambda v: v.scalar_tensor_tensor(out=pp[:, g_ * 512:(g_ + 1) * 512], in0=mp[:], scalar=bsT[:, g_:g_ + 1],
                                                              in1=uu[:, g_ * 512:(g_ + 1) * 512], op0=ALU.add, op1=ALU.mult),
                       reads=[mp_b, bsT_b, uu_b], writes=[pp_b])
                for half in range(2):
                    for k in range(8):
                        kk = half * 8 + k
                        op("pe", lambda p: p.transpose(out=psT[:, k, :], in_=pp[:, kk * 128:(kk + 1) * 128], identity=ident[:]),
                           reads=[pp_b, ident_b], writes=[psT_b], inc=(k == 7))
                    op("act", lambda a: a.copy(out=ppT[:, half * 8:(half + 1) * 8, :], in_=psT[:]), reads=[psT_b], writes=[ppT_b])
                xo_, xo_b = xo_t[0]
                for cb in range(2):
                    mp, mp_b = mps_[cb]
                    op("pe", lambda p: p.matmul(mp[:], lhsT=ones1[0:1, :], rhs=bb16[0:1, 4096 + cb * 512:4096 + (cb + 1) * 512],
                                                start=True, stop=False), reads=[ones1_b, bb16_b], writes=[mp_b], inc=False)
                    for k in range(16):
                        op("pe", lambda p: p.matmul(mp[:], lhsT=ppT[:, k, :], rhs=Wgo[:, k, cb * 512:(cb + 1) * 512],
                                                    start=False, stop=(k == 15)),
                           reads=[ppT_b, Wgo_b], writes=[mp_b], inc=(k == 15))
                    op("dve", lambda v: v.tensor_tensor(out=xo_[:, cb * 512:(cb + 1) * 512], in0=mp[:],
                                                        in1=gt[:, cb * 512:(cb + 1) * 512], op=ALU.mult),
                       reads=[mp_b, gt_b], writes=[xo_b])
                op("dve", lambda g: g.tensor_tensor(out=x_t[:], in0=xo_[:], in1=x_t[:], op=ALU.add),
                   reads=[xo_b, x_b], writes=[x_b])
                dma(xa[t * 128:(t + 1) * 128, :], x_t[:], x_b, reads=[x_b], writes=[xa_b])
            mk.pop()

            check("G")
            moe_layer(1, xa, xa_b, xb, xb_b)
            check("E1")

            mk.push()
            fg, fg_b = mk.sb("fg", [128, 1024], F32)
            load_bc(fg[:], fg_b, final_g[0:1, :])
            xt = [mk.sb("xt", [128, 1024], F32) for _ in range(2)]
            yt = [mk.sb("yt", [128, 1024], F32) for _ in range(2)]
            junk, junk_b = mk.sb("junk", [128, 1024], F32)
            ss2 = [mk.sb("ss", [128, 1], F32) for _ in range(2)]
            dma(xt[0][0][:], xb[0:128, :], xt[0][1], reads=[xb_b], writes=[xt[0][1]])
            for t in range(NT):
                par = t % 2
                if t + 1 < NT:
                    dma(xt[1 - par][0][:], xb[(t + 1) * 128:(t + 2) * 128, :], xt[1 - par][1], reads=[xb_b],
                        writes=[xt[1 - par][1]])
                x_t, x_b = xt[par]
                y_t, y_b = yt[par]
                ss, ss_b = ss2[par]
                op("act", lambda a: a.activation(out=junk[:], in_=x_t[:], func=AF.Square, accum_out=ss[:]),
                   reads=[x_b], writes=[junk_b, ss_b])
                op("dve", lambda v: v.tensor_scalar(out=ss[:], in0=ss[:], scalar1=1.0 / D, scalar2=EPS,
                                                    op0=ALU.mult, op1=ALU.add), reads=[ss_b], writes=[ss_b])
                op("act", lambda a: a.sqrt(out=ss[:], in_=ss[:]), reads=[ss_b], writes=[ss_b])
                op("dve", lambda v: v.reciprocal(out=ss[:], in_=ss[:]), reads=[ss_b], writes=[ss_b])
                op("dve", lambda v: v.scalar_tensor_tensor(out=y_t[:], in0=x_t[:], scalar=ss[:, 0:1], in1=fg[:],
                                                          op0=ALU.mult, op1=ALU.mult),
                   reads=[x_b, ss_b, fg_b], writes=[y_b])
                dma(y_out[t * 128:(t + 1) * 128, :], y_t[:], y_b, reads=[y_b], writes=[yb_])
            mk.pop()

        except StopBuild:
            while len(mk.stacks) > 1:
                mk.pop()
        mk.finish()
        build.stats = (mk.n_inst, mk.n_wait)
    return nc


def prepare(cfg, inp):
    NC, TS, TP, NE, DE = cfg.NC, cfg.TS, cfg.TP, cfg.NE, cfg.DE
    f = lambda a: np.ascontiguousarray(np.asarray(a, dtype=np.float32))
    xp = f(inp["x_prompt"])[0]
    xsm = f(inp["x_sample"])
    cp = f(inp["c_prompt"])[0]
    csm = f(inp["c_sample"])
    PT = TP * 128
    S = xp.shape[0]
    perm = np.concatenate([np.arange(0, 2 * DE, 2), np.arange(1, 2 * DE, 2)])
    shared = {
        "ctab": make_ctab(),
        "ada_w": f(inp["ada_w"]).reshape(2 * 1024, 6144),
        "ada_b": f(inp["ada_b"]),
        "norm1_g": f(inp["norm1_g"]), "norm2_g": f(inp["norm2_g"]),
        "ret_w_in": f(inp["ret_w_in"])[0],
        "ret_ld": f(inp["ret_log_decay"]).reshape(1, 8),
        "ret_gn_g": f(inp["ret_gn_g"]).reshape(1, 2048),
        "ret_w_out": f(inp["ret_w_out"])[0],
        "gm_w_in": f(inp["gm_w_in"])[0],
        "gm_b_in": f(inp["gm_b_in"]).reshape(1, 4096),
        "gm_vn_g": f(inp["gm_vn_g"]).reshape(1, 2048),
        "gm_vn_b": f(inp["gm_vn_b"]).reshape(1, 2048),
        "gm_w_sT": f(np.transpose(f(inp["gm_w_s"])[0], (0, 2, 1))).reshape(4 * 128, 128),
        "gm_b_sT": f(f(inp["gm_b_s"])[0].T),
        "gm_w_out": f(inp["gm_w_out"])[0],
        "gm_b_out": f(inp["gm_b_out"]).reshape(1, 1024),
        "moe_w_r": f(inp["moe_w_r"]).reshape(2 * 1024, NE),
        "moe_b_r": f(inp["moe_b_r"]),
        "moe_w_gu": f(f(inp["moe_w_gu"])[..., perm].reshape(2, NE, 8, 128, 2 * DE).transpose(0, 1, 3, 2, 4)).reshape(2 * NE * 128, 8 * 2 * DE),
        "moe_b_gu": f(f(inp["moe_b_gu"])[..., perm]).reshape(2 * NE, 2 * DE),
        "moe_w_dn": f(f(inp["moe_w_dn"]).reshape(2, NE, DE // 128, 128, 1024).transpose(0, 1, 3, 2, 4)).reshape(2 * NE * 128, (DE // 128) * 1024),
        "moe_b_dn": f(inp["moe_b_dn"]).reshape(2 * NE, 1024),
        "final_g": f(inp["final_g"]).reshape(1, 1024),
    }
    pos_s = np.arange(TS * 128)
    in_maps = []
    for c in range(NC):
        a, b = c * PT, (c + 1) * PT
        m = dict(shared)
        m["xs"] = np.concatenate([xsm[c], xp[a:b]], axis=0)
        if NC > 1:
            m["xo"] = np.concatenate([xp[:a], xp[b:]], axis=0)
            pos_o = np.concatenate([np.arange(0, a), np.arange(b, S)])
            meta = np.zeros((pos_o.shape[0], 4), np.float32)
            bef = pos_o < a
            meta[bef, 0] = (a - 1 - pos_o[bef])
            meta[bef, 1] = 1.0
            meta[~bef, 2] = (pos_o[~bef] - b)
            meta[~bef, 3] = 1.0
        else:
            m["xo"] = np.zeros((128, 1024), np.float32)
            pos_o = np.zeros(128)
            meta = np.zeros((128, 4), np.float32)
        m["oth_meta"] = meta
        m["rope_oth"] = rope_table(pos_o)
        m["rope_own"] = rope_table(np.concatenate([pos_s, np.arange(a, b)]))
        cc = np.stack([csm[c], cp], axis=-1)
        m["ccol"] = f(cc.reshape(8, 128, 2).transpose(1, 0, 2).reshape(128, 16))
        in_maps.append(m)
    return in_maps


def assemble(cfg, results):
    TS, TP = cfg.TS, cfg.TP
    ys = [np.asarray(r["y"], dtype=np.float32) for r in results]
    y_sample = np.stack([y[:TS * 128] for y in ys], axis=0)
    y_prompt = np.concatenate([y[TS * 128:] for y in ys], axis=0)[None]
    return y_prompt, y_sample


_CACHE = {}


def run(cfg, inputs):
    key = (cfg.NC, cfg.TS, cfg.TP, cfg.NE, cfg.DE)
    if key not in _CACHE:
        _CACHE[key] = build(cfg)
    nc = _CACHE[key]
    in_maps = prepare(cfg, inputs)
    res = run_bass_kernel_spmd(nc, in_maps, core_ids=list(range(cfg.NC)))
    return assemble(cfg, res.results)


def kernel(**inputs):
    cfg = Cfg()
    return run(cfg, inputs)
```
